# Optimizing a Trainium2 kernel written in Bass

```python
import jax, jax.numpy as jnp
from jax import lax
import numpy as np

D_MODEL = 1024
BATCH = 8
SEQ = 4096
DEPTH = 2

MOBA_HEADS = 8
MOBA_HEAD_DIM = 64
MOBA_BLOCK = 256
MOBA_TOPK = 3
MOBA_Q_CHUNK = 32
RET_HEADS = 4
RET_DK = 64
RET_DV = 128
RET_CHUNK = 256
SSD_D_INNER = 512
SSD_HEAD_DIM = 64
SSD_HEADS = SSD_D_INNER // SSD_HEAD_DIM
SSD_GROUPS = 2
SSD_STATE = 128
SSD_CONV = 4
SSD_CHUNK = 256
SSD_XBC = SSD_D_INNER + 2 * SSD_GROUPS * SSD_STATE
SB_HEADS = 8
SB_HEAD_DIM = 64
SB_Q_BLOCK = 128

MOBA_W = MOBA_HEADS * MOBA_HEAD_DIM
RET_W = RET_HEADS * RET_DV
SB_W = SB_HEADS * SB_HEAD_DIM
N_BRANCH = 4
BRANCH_W = 512
IN_SIZES = (MOBA_W, MOBA_W, MOBA_W,
            RET_HEADS * RET_DK, RET_HEADS * RET_DK, RET_W, RET_W,
            SSD_D_INNER, SSD_XBC, SSD_HEADS,
            SB_W, SB_W, SB_W,
            N_BRANCH * D_MODEL)
IN_WIDTH = int(sum(IN_SIZES))
SPLIT_POINTS = tuple(int(p) for p in np.cumsum(IN_SIZES)[:-1])

D_FF = 2816
N_EXPERTS = 8
TOP_K = 2
D_FF_EXPERT = 3584
N_DENSE = (DEPTH + 1) // 2
N_MOE = DEPTH // 2

DEEPNORM_ALPHA = (2.0 * DEPTH) ** 0.25
DEEPNORM_BETA = (8.0 * DEPTH) ** -0.25
LN_EPS = 1e-5
NORM_EPS = 1e-6
NEG_INF = -1e30

kernel_name = 'hybrid_moba_retnet_ssd_stickbreak_moe'


def _pad_seq(t, s_pad):
    pad = [(0, 0)] * t.ndim
    pad[1] = (0, s_pad - t.shape[1])
    return jnp.pad(t, pad)


def _layer_norm(x, g, b):
    xf = x.astype(jnp.float32)
    mu = jnp.mean(xf, axis=-1, keepdims=True)
    var = jnp.mean(jnp.square(xf - mu), axis=-1, keepdims=True)
    return ((xf - mu) * lax.rsqrt(var + LN_EPS) * g + b).astype(x.dtype)


def _alibi_slopes(n_heads):
    return jnp.asarray(2.0 ** (-8.0 * np.arange(1, n_heads + 1) / n_heads), dtype=jnp.float32)


def _moba_attention(q, k, v):
    bsz, s, h, dh = q.shape
    s_pad = -(-s // MOBA_BLOCK) * MOBA_BLOCK
    nb = s_pad // MOBA_BLOCK
    k_sel = min(MOBA_TOPK, max(nb - 1, 1))
    q, k, v = (_pad_seq(t, s_pad) for t in (q, k, v))
    qh = q.transpose(0, 2, 1, 3)
    kb = k.transpose(0, 2, 1, 3).reshape(bsz, h, nb, MOBA_BLOCK, dh)
    vb = v.transpose(0, 2, 1, 3).reshape(bsz, h, nb, MOBA_BLOCK, dh)
    kmean = jnp.mean(kb, axis=3)
    slopes = _alibi_slopes(h)[None, :, None, None]
    scale = dh ** -0.5
    bi = jnp.arange(bsz)[:, None, None, None]
    hi = jnp.arange(h)[None, :, None, None]
    offs = jnp.arange(MOBA_BLOCK)
    n_sel = k_sel * MOBA_BLOCK

    def chunk(ci):
        q0 = ci * MOBA_Q_CHUNK
        qc = lax.dynamic_slice_in_dim(qh, q0, MOBA_Q_CHUNK, axis=2)
        qpos = q0 + jnp.arange(MOBA_Q_CHUNK)
        own = q0 // MOBA_BLOCK
        gate = jnp.einsum('bhqd,bhnd->bhqn', qc, kmean).astype(jnp.float32)
        past = jnp.arange(nb) < own
        gate = jnp.where(past, gate, NEG_INF)
        _, sel = lax.top_k(gate, k_sel)
        sel_ok = past[sel]
        ksel = kb[bi, hi, sel]
        vsel = vb[bi, hi, sel]
        s_sel = jnp.einsum('bhqd,bhqjkd->bhqjk', qc, ksel).astype(jnp.float32) * scale
        kpos_sel = sel[..., None] * MOBA_BLOCK + offs
        s_sel = s_sel - slopes[..., None] * (qpos[:, None, None] - kpos_sel)
        s_sel = jnp.where(sel_ok[..., None], s_sel, NEG_INF)
        kown = lax.dynamic_index_in_dim(kb, own, axis=2, keepdims=False)
        vown = lax.dynamic_index_in_dim(vb, own, axis=2, keepdims=False)
        kpos_own = own * MOBA_BLOCK + offs
        s_own = jnp.einsum('bhqd,bhkd->bhqk', qc, kown).astype(jnp.float32) * scale
        s_own = s_own - slopes * (qpos[:, None] - kpos_own[None, :])
        s_own = jnp.where(kpos_own[None, :] <= qpos[:, None], s_own, NEG_INF)
        scores = jnp.concatenate([s_sel.reshape(bsz, h, MOBA_Q_CHUNK, n_sel), s_own], axis=-1)
        p = jax.nn.softmax(scores, axis=-1).astype(v.dtype)
        p_sel = p[..., :n_sel].reshape(bsz, h, MOBA_Q_CHUNK, k_sel, MOBA_BLOCK)
        p_own = p[..., n_sel:]
        return (jnp.einsum('bhqjk,bhqjkd->bhqd', p_sel, vsel)
                + jnp.einsum('bhqk,bhkd->bhqd', p_own, vown))

    outs = lax.map(chunk, jnp.arange(s_pad // MOBA_Q_CHUNK))
    out = outs.transpose(1, 2, 0, 3, 4).reshape(bsz, h, s_pad, dh)[:, :, :s]
    return out.transpose(0, 2, 1, 3)


def _retention(q, k, v, g, gn_g, gn_b):
    bsz, s, h, dk = q.shape
    dv = v.shape[-1]
    s_pad = -(-s // RET_CHUNK) * RET_CHUNK
    n = s_pad // RET_CHUNK
    log_g = jnp.log(1.0 - 2.0 ** (-5.0 - jnp.arange(h, dtype=jnp.float32)))
    q = _pad_seq(q.astype(jnp.float32) * dk ** -0.5, s_pad).reshape(bsz, n, RET_CHUNK, h, dk)
    k = _pad_seq(k.astype(jnp.float32), s_pad).reshape(bsz, n, RET_CHUNK, h, dk)
    v = _pad_seq(v.astype(jnp.float32), s_pad).reshape(bsz, n, RET_CHUNK, h, dv)
    idx = jnp.arange(RET_CHUNK, dtype=jnp.float32)
    diff = idx[:, None] - idx[None, :]
    decay = jnp.where(diff >= 0, jnp.exp(jnp.maximum(diff, 0.0)[None] * log_g[:, None, None]), 0.0)
    qk = jnp.einsum('bnihd,bnjhd->bnhij', q, k) * decay
    o_intra = jnp.einsum('bnhij,bnjhe->bnihe', qk, v)
    k_dec = jnp.exp((RET_CHUNK - 1.0 - idx)[None, :] * log_g[:, None])
    states = jnp.einsum('bnjhd,hj,bnjhe->nbhde', k, k_dec, v)
    chunk_dec = jnp.exp(RET_CHUNK * log_g)[None, :, None, None]

    def step(st, s_n):
        return chunk_dec * st + s_n, st

    _, prev = lax.scan(step, jnp.zeros_like(states[0]), states)
    q_dec = jnp.exp((idx + 1.0)[None, :] * log_g[:, None])
    o_cross = jnp.einsum('bnihd,hi,nbhde->bnihe', q, q_dec, prev)
    o = (o_intra + o_cross).reshape(bsz, s_pad, h, dv)[:, :s]
    mu = jnp.mean(o, axis=-1, keepdims=True)
    var = jnp.mean(jnp.square(o - mu), axis=-1, keepdims=True)
    o = ((o - mu) * lax.rsqrt(var + NORM_EPS)).reshape(bsz, s, h * dv) * gn_g + gn_b
    return (jax.nn.silu(g.astype(jnp.float32)) * o).astype(g.dtype)


def _causal_dwconv(x, w, b):
    kw, ch = w.shape
    out = lax.conv_general_dilated(x, w[:, None, :].astype(x.dtype), window_strides=(1,),
                                   padding=[(kw - 1, 0)],
                                   dimension_numbers=('NWC', 'WIO', 'NWC'),
                                   feature_group_count=ch)
    return out + b


def _ssd_chunked(x, dt, a, bm, cm):
    bsz, s = x.shape[0], x.shape[1]
    g, r, p = x.shape[2], x.shape[3], x.shape[4]
    s_pad = -(-s // SSD_CHUNK) * SSD_CHUNK
    n = s_pad // SSD_CHUNK
    x, dt, bm, cm = (_pad_seq(t, s_pad) for t in (x, dt, bm, cm))
    x = x.reshape(bsz, n, SSD_CHUNK, g, r, p)
    dt = dt.reshape(bsz, n, SSD_CHUNK, g, r)
    bm = bm.reshape(bsz, n, SSD_CHUNK, g, SSD_STATE)
    cm = cm.reshape(bsz, n, SSD_CHUNK, g, SSD_STATE)
    cum = jnp.cumsum(dt * a, axis=2)
    idx = jnp.arange(SSD_CHUNK)
    causal = (idx[:, None] >= idx[None, :])[:, :, None, None]
    seg = cum[:, :, :, None] - cum[:, :, None, :]
    lmat = jnp.exp(jnp.where(causal, seg, NEG_INF))
    cb = jnp.einsum('bntgk,bnsgk->bntsg', cm, bm)
    w = cb[..., None] * lmat * dt[:, :, None]
    y_diag = jnp.einsum('bntsgr,bnsgrp->bntgrp', w, x)
    decay_states = jnp.exp(cum[:, :, -1:] - cum) * dt
    states = jnp.einsum('bnsgk,bnsgr,bnsgrp->nbgrpk', bm, decay_states, x)
    chunk_decay = jnp.exp(cum[:, :, -1]).transpose(1, 0, 2, 3)

    def step(hs, inp):
        st, dec = inp
        return dec[..., None, None] * hs + st, hs

    _, prev = lax.scan(step, jnp.zeros_like(states[0]), (states, chunk_decay))
    y_off = jnp.einsum('bntgk,nbgrpk->bntgrp', cm, prev) * jnp.exp(cum)[..., None]
    y = (y_diag + y_off).reshape(bsz, s_pad, g, r, p)
    return y[:, :s]


def _ssd_mixer(z, xbc, dt_raw, conv_w, conv_b, dt_bias, a_log, d_skip, norm_g):
    bsz, s, _ = xbc.shape
    r = SSD_HEADS // SSD_GROUPS
    xbc = jax.nn.silu(_causal_dwconv(xbc, conv_w, conv_b)).astype(jnp.float32)
    xs, bm, cm = jnp.split(xbc, [SSD_D_INNER, SSD_D_INNER + SSD_GROUPS * SSD_STATE], axis=-1)
    x = xs.reshape(bsz, s, SSD_GROUPS, r, SSD_HEAD_DIM)
    bm = bm.reshape(bsz, s, SSD_GROUPS, SSD_STATE)
    cm = cm.reshape(bsz, s, SSD_GROUPS, SSD_STATE)
    dt = jax.nn.softplus(dt_raw.astype(jnp.float32) + dt_bias).reshape(bsz, s, SSD_GROUPS, r)
    a = -jnp.exp(a_log.astype(jnp.float32)).reshape(SSD_GROUPS, r)
    y = _ssd_chunked(x, dt, a, bm, cm)
    y = y + d_skip.reshape(SSD_GROUPS, r)[:, :, None] * x
    y = y.reshape(bsz, s, SSD_D_INNER) * jax.nn.silu(z.astype(jnp.float32))
    yg = y.reshape(bsz, s, SSD_GROUPS, SSD_D_INNER // SSD_GROUPS)
    yg = yg * lax.rsqrt(jnp.mean(jnp.square(yg), axis=-1, keepdims=True) + LN_EPS)
    return (yg.reshape(bsz, s, SSD_D_INNER) * norm_g).astype(z.dtype)


def _stick_breaking(q, k, v):
    bsz, s, h, dh = q.shape
    scale = dh ** -0.5
    kpos = jnp.arange(s)

    def block(i):
        q0 = i * SB_Q_BLOCK
        qb = lax.dynamic_slice_in_dim(q, q0, SB_Q_BLOCK, axis=1)
        z = jnp.einsum('bqhd,bkhd->bhqk', qb, k).astype(jnp.float32) * scale
        qpos = q0 + jnp.arange(SB_Q_BLOCK)
        causal = kpos[None, :] < qpos[:, None]
        log_beta = jax.nn.log_sigmoid(z)
        log_1mb = jnp.where(causal, jax.nn.log_sigmoid(-z), 0.0)
        rem = lax.cumsum(log_1mb, axis=3, reverse=True) - log_1mb
        w = jnp.where(causal, jnp.exp(log_beta + rem), 0.0).astype(v.dtype)
        return jnp.einsum('bhqk,bkhd->bqhd', w, v)

    outs = lax.map(block, jnp.arange(s // SB_Q_BLOCK))
    return outs.transpose(1, 0, 2, 3, 4).reshape(bsz, s, h, dh)


def _hybrid_mixer(h, w_in, conv_w, conv_b, dt_bias, a_log, d_skip, ssm_norm_g,
                  ret_gn_g, ret_gn_b, w_br, w_out):
    bsz, s, _ = h.shape
    proj = jnp.einsum('bsd,de->bse', h, w_in)
    (mq, mk, mv, rq, rk, rv, rg, sz, sxbc, sdt, bq, bk, bv, gates) = jnp.split(proj, SPLIT_POINTS, axis=-1)
    o_a = _moba_attention(mq.reshape(bsz, s, MOBA_HEADS, MOBA_HEAD_DIM),
                          mk.reshape(bsz, s, MOBA_HEADS, MOBA_HEAD_DIM),
                          mv.reshape(bsz, s, MOBA_HEADS, MOBA_HEAD_DIM)).reshape(bsz, s, MOBA_W)
    o_b = _retention(rq.reshape(bsz, s, RET_HEADS, RET_DK), rk.reshape(bsz, s, RET_HEADS, RET_DK),
                     rv.reshape(bsz, s, RET_HEADS, RET_DV), rg, ret_gn_g, ret_gn_b)
    o_c = _ssd_mixer(sz, sxbc, sdt, conv_w, conv_b, dt_bias, a_log, d_skip, ssm_norm_g)
    o_d = _stick_breaking(bq.reshape(bsz, s, SB_HEADS, SB_HEAD_DIM),
                          bk.reshape(bsz, s, SB_HEADS, SB_HEAD_DIM),
                          bv.reshape(bsz, s, SB_HEADS, SB_HEAD_DIM)).reshape(bsz, s, SB_W)
    o = jnp.stack([o_a.astype(h.dtype), o_b.astype(h.dtype), o_c.astype(h.dtype), o_d.astype(h.dtype)], axis=2)
    y = jnp.einsum('bsnc,ncd->bsnd', o, w_br)
    gate = jax.nn.sigmoid(gates.reshape(bsz, s, N_BRANCH, D_MODEL))
    merged = jnp.einsum('bsnd,bsnd->bsd', gate, y)
    return jnp.einsum('bsd,de->bse', merged, w_out)


def _swiglu(h, w_gu, w_down):
    gu = jnp.einsum('bsd,df->bsf', h, w_gu)
    g, u = jnp.split(gu, 2, axis=-1)
    return jnp.einsum('bsf,fd->bsd', jax.nn.silu(g) * u, w_down)


def _moe_swiglu(h, router_w, router_b, w_gu, w_down):
    logits = (jnp.einsum('bsd,de->bse', h, router_w) + router_b).astype(jnp.float32)
    top_val, top_idx = lax.top_k(logits, TOP_K)
    top_w = jax.nn.softmax(top_val, axis=-1)
    combine = jnp.einsum('bsk,bske->bse', top_w,
                         jax.nn.one_hot(top_idx, N_EXPERTS, dtype=jnp.float32)).astype(h.dtype)
    y = jnp.zeros_like(h)
    for e in range(N_EXPERTS):
        y = y + combine[..., e:e + 1] * _swiglu(h, w_gu[e], w_down[e])
    return y


def setup_inputs(seed: int = 0) -> dict:
    key = jax.random.key(seed)
    ks = jax.random.split(key, 26)
    f32 = jnp.float32

    def nrm(k, shape, scale):
        return jax.random.normal(k, shape, f32) * scale

    dt0 = jnp.exp(jax.random.uniform(ks[7], (DEPTH, SSD_HEADS), f32, np.log(1e-3), np.log(1e-1)))
    return {
        'x': nrm(ks[0], (BATCH, SEQ, D_MODEL), 1.0),
        'c': nrm(ks[1], (BATCH, D_MODEL), 1.0),
        'w_ada': nrm(ks[2], (DEPTH, D_MODEL, 6 * D_MODEL), 0.5 * D_MODEL ** -0.5),
        'b_ada': nrm(ks[3], (DEPTH, 6 * D_MODEL), 0.02),
        'w_in': nrm(ks[4], (DEPTH, D_MODEL, IN_WIDTH), D_MODEL ** -0.5),
        'conv_w': nrm(ks[5], (DEPTH, SSD_CONV, SSD_XBC), SSD_CONV ** -0.5),
        'conv_b': nrm(ks[6], (DEPTH, SSD_XBC), 0.02),
        'dt_bias': dt0 + jnp.log(-jnp.expm1(-dt0)),
        'a_log': jnp.log(jax.random.uniform(ks[8], (DEPTH, SSD_HEADS), f32, 1.0, 16.0)),
        'd_skip': 1.0 + nrm(ks[9], (DEPTH, SSD_HEADS), 0.1),
        'ssm_norm_g': 1.0 + nrm(ks[10], (DEPTH, SSD_D_INNER), 0.05),
        'ret_gn_g': 1.0 + nrm(ks[11], (DEPTH, RET_W), 0.05),
        'ret_gn_b': nrm(ks[12], (DEPTH, RET_W), 0.02),
        'w_br': nrm(ks[13], (DEPTH, N_BRANCH, BRANCH_W, D_MODEL), BRANCH_W ** -0.5),
        'w_out': nrm(ks[14], (DEPTH, D_MODEL, D_MODEL), DEEPNORM_BETA * D_MODEL ** -0.5),
        'ln1_g': 1.0 + nrm(ks[15], (DEPTH, D_MODEL), 0.05),
        'ln1_b': nrm(ks[16], (DEPTH, D_MODEL), 0.02),
        'ln2_g': 1.0 + nrm(ks[17], (DEPTH, D_MODEL), 0.05),
        'ln2_b': nrm(ks[18], (DEPTH, D_MODEL), 0.02),
        'ffn_w_gu': nrm(ks[19], (N_DENSE, D_MODEL, 2 * D_FF), D_MODEL ** -0.5),
        'ffn_w_down': nrm(ks[20], (N_DENSE, D_FF, D_MODEL), DEEPNORM_BETA * D_FF ** -0.5),
        'router_w': nrm(ks[21], (N_MOE, D_MODEL, N_EXPERTS), D_MODEL ** -0.5),
        'router_b': nrm(ks[22], (N_MOE, N_EXPERTS), 0.01),
        'expert_w_gu': nrm(ks[23], (N_MOE, N_EXPERTS, D_MODEL, 2 * D_FF_EXPERT), D_MODEL ** -0.5),
        'expert_w_down': nrm(ks[24], (N_MOE, N_EXPERTS, D_FF_EXPERT, D_MODEL), DEEPNORM_BETA * D_FF_EXPERT ** -0.5),
    }


def reference(x, c, w_ada, b_ada, w_in, conv_w, conv_b, dt_bias, a_log, d_skip, ssm_norm_g,
              ret_gn_g, ret_gn_b, w_br, w_out, ln1_g, ln1_b, ln2_g, ln2_b,
              ffn_w_gu, ffn_w_down, router_w, router_b, expert_w_gu, expert_w_down):
    c_act = jax.nn.silu(c)
    for l in range(DEPTH):
        mod = jnp.einsum('bd,de->be', c_act, w_ada[l]) + b_ada[l]
        sh1, sc1, g1, sh2, sc2, g2 = jnp.split(mod[:, None, :], 6, axis=-1)
        h = x * (1.0 + sc1) + sh1
        mix = _hybrid_mixer(h, w_in[l], conv_w[l], conv_b[l], dt_bias[l], a_log[l], d_skip[l],
                            ssm_norm_g[l], ret_gn_g[l], ret_gn_b[l], w_br[l], w_out[l])
        x = _layer_norm(DEEPNORM_ALPHA * x + g1 * mix, ln1_g[l], ln1_b[l])
        h = x * (1.0 + sc2) + sh2
        if l % 2 == 0:
            f = _swiglu(h, ffn_w_gu[l // 2], ffn_w_down[l // 2])
        else:
            f = _moe_swiglu(h, router_w[l // 2], router_b[l // 2], expert_w_gu[l // 2], expert_w_down[l // 2])
        x = _layer_norm(DEEPNORM_ALPHA * x + g2 * f, ln2_g[l], ln2_b[l])
    return x
```

```python
import contextlib
import math
import numpy as np
import concourse.bass as bass
import concourse.mybir as mybir
from concourse.bass_utils import run_bass_kernel_spmd

F32 = mybir.dt.float32
BF16 = mybir.dt.bfloat16
AF = mybir.ActivationFunctionType
ALU = mybir.AluOpType

S = 4096
D = 1024
NT = 32
IN_W = 10248
ALPHA = 4.0 ** 0.25
LN_EPS = 1e-5
NORM_EPS = 1e-6
D_FF = 2816
D_FFE = 3584


class Builder:
    ENGS = ('pe', 'act', 'dve', 'pool', 'sp')

    def __init__(self):
        self.nc = bass.Bass("TRN2", target_bir_lowering=False)
        self.stack = contextlib.ExitStack()
        self.q = {e: [] for e in self.ENGS}
        self.seq = {e: 0 for e in self.ENGS}
        self.known = {e: {} for e in self.ENGS}
        self.last_w = {}
        self.readers = {}
        self.slots = {}
        self.sems = {}
        for e in self.ENGS:
            self.sems[('e', e)] = self.stack.enter_context(self.nc.semaphore("c_" + e))
        self._uid = 0

    def sbuf(self, shape, dtype, name=None):
        self._uid += 1
        return self.stack.enter_context(self.nc.sbuf_tensor(name or f"sb{self._uid}", list(shape), dtype))

    def psum(self, shape, dtype, name=None):
        self._uid += 1
        return self.stack.enter_context(self.nc.psum_tensor(name or f"ps{self._uid}", list(shape), dtype))

    def dram(self, name, shape, dtype, kind="Internal"):
        return self.nc.dram_tensor(name, list(shape), dtype, kind=kind).ap()

    def _deps(self, eng, r, w, skip_same_w=False):
        deps = {}

        def add(tok):
            s, v = tok
            if deps.get(s, 0) < v:
                deps[s] = v
        for k in r:
            t = self.last_w.get(k)
            if t is not None:
                add(t)
        for k in w:
            t = self.last_w.get(k)
            if t is not None and not (skip_same_w and t[0] == ('e', eng)):
                add(t)
            for t in self.readers.get(k, ()):
                add(t)
        waits = []
        kn = self.known[eng]
        for s, v in deps.items():
            if kn.get(s, 0) >= v:
                continue
            kn[s] = v
            waits.append((s, v))
        return waits

    def _record(self, tok, r, w):
        for k in w:
            self.last_w[k] = tok
            self.readers[k] = []
        for k in r:
            self.readers.setdefault(k, []).append(tok)

    def op(self, eng, fn, r=(), w=()):
        waits = self._deps(eng, r, w, skip_same_w=(eng == 'pe'))
        self.seq[eng] += 1
        tok = (('e', eng), self.seq[eng])
        self.q[eng].append((fn, waits, (('e', eng), 1)))
        self._record(tok, r, w)
        return tok

    def dma(self, out, in_, r=(), w=(), slot=None, q='sp'):
        if slot not in self.slots:
            self.sems[('d', slot)] = self.stack.enter_context(self.nc.semaphore("d%d" % len(self.slots)))
            self.slots[slot] = [('d', slot), 0]
        waits = self._deps(q, r, w)
        ent = self.slots[slot]
        ent[1] += 16
        tok = (ent[0], ent[1])
        self.q[q].append((lambda e, o=out, i=in_: e.dma_start(out=o, in_=i), waits, (ent[0], 16)))
        self._record(tok, r, w)
        return tok

    def barrier(self):
        toks = []
        for e in self.ENGS:
            if self.seq[e] > 0:
                toks.append((('e', e), self.seq[e]))
        for k, ent in self.slots.items():
            if ent[1] > 0:
                toks.append((ent[0], ent[1]))
        for e in self.ENGS:
            kn = self.known[e]
            waits = []
            for s, v in toks:
                if kn.get(s, 0) >= v:
                    continue
                kn[s] = v
                waits.append((s, v))
            if waits:
                self.q[e].append((None, waits, None))
        self.last_w.clear()
        self.readers.clear()

    def finish(self):
        self.barrier()
        nc = self.nc
        sems = self.sems

        def run(eng_obj, lst):
            for fn, waits, inc in lst:
                for s, v in waits:
                    eng_obj.wait_ge(sems[s], v)
                if fn is not None:
                    fn(eng_obj).then_inc(sems[inc[0]], inc[1])
        with nc.Block() as block:
            @block.tensor
            def _(e):
                run(e, self.q['pe'])

            @block.scalar
            def _(e):
                run(e, self.q['act'])

            @block.vector
            def _(e):
                run(e, self.q['dve'])

            @block.gpsimd
            def _(e):
                run(e, self.q['pool'])

            @block.sync
            def _(e):
                run(e, self.q['sp'])
        self.stack.close()
        return nc


def _r3(ap, c):
    return ap.rearrange("p (c n) -> p c n", c=c)


class MK(Builder):
    AW = 50 * 1024

    def __init__(self, dbg=()):
        super().__init__()
        self.dbg = set(dbg)
        self.arena = self.sbuf([128, self.AW], F32, "arena")
        self.aoff = 0
        self.ps = self.psum([128, 8, 512], F32, "psum")
        self.pcur = 0
        self.ktag = 0

    def alloc(self, words):
        a = self.aoff
        self.aoff += words
        assert self.aoff <= self.AW, (self.aoff, self.AW)
        return self.arena[:, a:a + words]

    def allocb(self, n):
        assert n % 2 == 0
        return self.alloc(n // 2).bitcast(BF16)

    def key(self, name):
        self.ktag += 1
        return (name, self.ktag)

    def bank(self):
        i = self.pcur
        self.pcur = (self.pcur + 1) % 8
        return i

    def bank2(self):
        if self.pcur % 2:
            self.pcur = (self.pcur + 1) % 8
        i = self.pcur
        self.pcur = (self.pcur + 2) % 8
        return i

    def scr(self, name, shape, dtype):
        return self.dram(name, shape, dtype, kind="ExternalOutput" if name in self.dbg else "Internal")

    def mm(self, out, lhsT, rhs, start, stop, r, w):
        self.op('pe', lambda e: e.matmul(out, lhsT=lhsT, rhs=rhs, start=start, stop=stop), r=r, w=w)

    def tr(self, out, in_, ident, r, w):
        self.op('pe', lambda e: e.transpose(out=out, in_=in_, identity=ident), r=r, w=w)

    def act(self, out, in_, func, r, w, bias=None, scale=None):
        kw = {}
        if bias is not None:
            kw['bias'] = bias
        if scale is not None:
            kw['scale'] = scale
        self.op('act', lambda e: e.activation(out=out, in_=in_, func=func, **kw), r=r, w=w)

    def tt(self, eng, out, in0, in1, op, r, w):
        self.op(eng, lambda e: e.tensor_tensor(out=out, in0=in0, in1=in1, op=op), r=r, w=w)

    def ts(self, eng, out, in0, s1, op0, r, w, s2=None, op1=None):
        if op1 is None:
            self.op(eng, lambda e: e.tensor_scalar(out=out, in0=in0, scalar1=s1, scalar2=None, op0=op0), r=r, w=w)
        else:
            self.op(eng, lambda e: e.tensor_scalar(out=out, in0=in0, scalar1=s1, scalar2=s2, op0=op0, op1=op1), r=r, w=w)

    def stt(self, out, in0, scalar, in1, op0, op1, r, w):
        self.op('dve', lambda e: e.scalar_tensor_tensor(out=out, in0=in0, scalar=scalar, in1=in1, op0=op0, op1=op1), r=r, w=w)

    def cp(self, eng, out, in_, r, w):
        if eng == 'act':
            self.op('act', lambda e: e.copy(out=out, in_=in_), r=r, w=w)
        else:
            self.op(eng, lambda e: e.tensor_copy(out=out, in_=in_), r=r, w=w)

    def memset(self, eng, ap, val, w):
        self.op(eng, lambda e: e.memset(ap, val), w=w)

    def asel(self, out, in_, pattern, cmp, fill, base, cm, r, w):
        self.op('pool', lambda e: e.affine_select(out=out, in_=in_, pattern=pattern, compare_op=cmp, fill=fill,
                                                  base=base, channel_multiplier=cm), r=r, w=w)

    def init_wbufs(self, nslots, words):
        self.wst = [self.alloc(words) for _ in range(nslots)]
        self.wbf = [self.alloc(words // 2).bitcast(BF16) for _ in range(nslots)]
        self.wi = 0
        self.wn = nslots

    def load_w(self, src, kc, n, cast=True):
        i = self.wi
        self.wi = (self.wi + 1) % self.wn
        st = _r3(self.wst[i][:, 0:kc * n], kc)
        self.dma(st, src.rearrange("(c p) n -> p c n", p=128), w=[('wst', i)], slot=('wst', i))
        if not cast:
            return st, ('wst', i)
        bf = _r3(self.wbf[i][:, 0:kc * n], kc)
        self.cp('pool', bf, st, r=[('wst', i)], w=[('wbf', i)])
        return bf, ('wbf', i)


def build(dbg=(), stop_after=None, layers=(0, 1), MIX='abcd', skip=(), x_first=False):
    m = MK(dbg)
    nc = m.nc

    SHAPES = dict(x=[S, D], c=[128, 8], w_ada=[2, D, 6 * D], b_ada=[2, 1, 6 * D], w_in=[2, D, IN_W],
                  conv_w=[2, 128, 8, 4], conv_b=[2, 128, 8], dt_bias=[2, 8, 1], a_log=[2, 8, 1], d_skip=[2, 1, 8],
                  ssm_norm_g=[2, 1, 512], ret_gn_g=[2, 1, 512], ret_gn_b=[2, 1, 512], w_br=[2, 4, 512, D],
                  w_out=[2, D, D], ln1_g=[2, 1, D], ln1_b=[2, 1, D], ln2_g=[2, 1, D], ln2_b=[2, 1, D],
                  ffn_w_gu=[1, D, 2 * D_FF], ffn_w_down=[1, D_FF, D], router_w=[1, D, 8], router_b=[1, 1, 8],
                  expert_w_gu=[1, 8, D, 2 * D_FFE], expert_w_down=[1, 8, D_FFE, D])
    _ins = {}

    def IN(name):
        if name not in _ins:
            _ins[name] = m.dram(name, SHAPES[name], F32, kind="ExternalInput")
        return _ins[name]
    m.used_inputs = _ins
    y_out = m.dram("y", [S, D], F32, kind="ExternalOutput")

    qT = {n: m.scr(n, [512, S], BF16) for n in ("mqT", "mkT", "rqkT", "bqT", "bkT", "sBCT")}
    mv65 = m.scr("mv65", [S, 520], BF16)
    tokm = {n: m.scr(n, [S, 512], BF16) for n in ("rv", "rg", "sz", "bv", "sx")}
    scumT = m.scr("scumT", [8, S], F32)
    sdtok = m.scr("sdtok", [128, NT * 16], F32)
    oT = m.scr("oT", [4, 512, S], BF16)
    mergedT = m.scr("mergedT", [D, S], BF16)
    x1_d = m.scr("x1", [S, D], F32)
    x2_d = m.scr("x2", [S, D], F32)

    ident = m.alloc(128)
    identb = m.allocb(128)
    ones_row = m.alloc(128)
    one11 = ones_row[0:1, 0:1]
    modT = m.alloc(48)
    scp = m.alloc(16)
    gbc = m.alloc(2 * D)
    lnbc = m.alloc(4 * D)
    small = m.alloc(256)
    persist_end = m.aoff

    m.memset('pool', ident, 1.0, w=['ident'])
    m.asel(ident, ident, [[-1, 128]], ALU.is_equal, 0.0, 0, 1, r=['ident'], w=['ident'])
    m.cp('pool', identb, ident, r=['ident'], w=['identb'])
    m.memset('pool', ones_row, 1.0, w=['ones'])
    m.barrier()

    def ln_epilogue(ps2, xt, xkey, g_ap, lg, lb, out_t, okey, tmp, tkey, st):
        pk, stk = ps2[1], ('st', st)
        m.tt('dve', tmp, ps2[0], g_ap, ALU.mult, r=list(pk) + ['gbc'], w=[tkey])
        m.stt(tmp, xt, ALPHA, tmp, ALU.mult, ALU.add, r=[xkey, tkey], w=[tkey])
        sm = small[:, st * 32:(st + 1) * 32]
        m.op('dve', lambda e: e.bn_stats(out=sm[:, 0:6], in_=tmp[:, 0:512]), r=[tkey], w=[stk])
        m.op('dve', lambda e: e.bn_stats(out=sm[:, 6:12], in_=tmp[:, 512:1024]), r=[tkey], w=[stk])
        m.op('dve', lambda e: e.bn_aggr(out=sm[:, 12:14], in_=sm[:, 0:12]), r=[stk], w=[stk])
        m.ts('dve', sm[:, 14:15], sm[:, 13:14], LN_EPS, ALU.add, r=[stk], w=[stk])
        m.act(sm[:, 15:16], sm[:, 14:15], AF.Sqrt, r=[stk], w=[stk])
        m.op('dve', lambda e: e.reciprocal(out=sm[:, 16:17], in_=sm[:, 15:16]), r=[stk], w=[stk])
        m.ts('dve', tmp, tmp, sm[:, 12:13], ALU.subtract, r=[tkey, stk], w=[tkey], s2=sm[:, 16:17], op1=ALU.mult)
        m.tt('pool', tmp, tmp, lg, ALU.mult, r=[tkey, 'lnbc'], w=[tkey])
        m.tt('pool', out_t, tmp, lb, ALU.add, r=[tkey, 'lnbc'], w=[okey])

    for l in layers:
        x_src = IN("x") if (l == 0 or x_first) else x2_d
        x_dst = x2_d if l == 0 else y_out
        m.aoff = persist_end
        cT = m.alloc(8)
        cact = m.alloc(8)
        modrow = m.alloc(6 * D)
        brow = m.alloc(6 * D)
        m.init_wbufs(2, 8 * 512)
        m.dma(cT, IN("c"), w=['cT'], slot='misc0')
        m.dma(brow[0:1, :], IN("b_ada")[l], w=['brow'], slot='misc1')
        for i, k in enumerate(("ln1_g", "ln1_b", "ln2_g", "ln2_b")):
            m.dma(lnbc[:, i * D:(i + 1) * D], IN(k)[l].partition_broadcast(128), w=['lnbc'], slot=('lnbc', i))
        m.act(cact, cT, AF.Silu, r=['cT'], w=['cact'])
        for j in range(12):
            wb, wk = m.load_w(IN("w_ada")[l][:, j * 512:(j + 1) * 512], 8, 512, cast=False)
            b = m.bank()
            for k in range(8):
                m.mm(m.ps[0:1, b, :], cact[:, k:k + 1], wb[:, k, :], k == 0, k == 7, r=['cact', wk], w=[('ps', b)])
            m.tt('dve', modrow[0:1, j * 512:(j + 1) * 512], m.ps[0:1, b, :], brow[0:1, j * 512:(j + 1) * 512], ALU.add,
                 r=[('ps', b), 'brow'], w=['modrow'])
        b = m.bank()
        for j in range(48):
            m.mm(m.ps[:, b, j:j + 1], modrow[0:1, j * 128:(j + 1) * 128], one11, True, True, r=['modrow', 'ones'], w=[('ps', b)])
        m.cp('dve', modT, m.ps[:, b, 0:48], r=[('ps', b)], w=['modT'])
        m.ts('dve', scp[:, 0:8], modT[:, 8:16], 1.0, ALU.add, r=['modT'], w=['scp'])
        m.ts('dve', scp[:, 8:16], modT[:, 32:40], 1.0, ALU.add, r=['modT'], w=['scp'])
        for gi, c0 in enumerate((2 * D, 5 * D)):
            for hh in range(2):
                b = m.bank()
                m.mm(m.ps[:, b, :], ones_row[0:1, 0:128], modrow[0:1, c0 + hh * 512:c0 + (hh + 1) * 512], True, True,
                     r=['modrow', 'ones'], w=[('ps', b)])
                m.cp('act', gbc[:, gi * D + hh * 512:gi * D + (hh + 1) * 512], m.ps[:, b, :], r=[('ps', b)], w=['gbc'])
        m.barrier()
        if 'modT' in m.dbg:
            dd = m.scr("modT", [128, 48], F32)
            m.dma(dd, modT, r=['modT'], slot='dbg')
        if stop_after == ('mod', l):
            break

        m.aoff = persist_end
        hT = _r3(m.allocb(8 * S), 8)
        p1_end = m.aoff
        xts = [m.alloc(D) for _ in range(3)]

        def make_hT(dst, src_d, sc_ap, sh_ap, tts, dst_off=0):
            for tt_ in tts:
                s_ = tt_ % 3
                m.dma(xts[s_], src_d[tt_ * 128:(tt_ + 1) * 128, :], w=[('xt', s_)], slot=('xt', s_))
                b2 = m.bank2()
                for c in range(8):
                    bb, cc = b2 + c // 4, (c % 4) * 128
                    m.tr(m.ps[:, bb, cc:cc + 128], xts[s_][:, c * 128:(c + 1) * 128], ident, r=[('xt', s_), 'ident'], w=[('ps', bb)])
                for c in range(8):
                    bb, cc = b2 + c // 4, (c % 4) * 128
                    o = (tt_ - dst_off) * 128
                    m.act(dst[:, c, o:o + 128], m.ps[:, bb, cc:cc + 128], AF.Identity, r=[('ps', bb), 'scp', 'modT'],
                          w=[('hT', tt_)], scale=sc_ap[:, c:c + 1], bias=sh_ap[:, c:c + 1])
        make_hT(hT, x_src, scp[:, 0:8], modT[:, 0:8], range(NT))
        m.barrier()
        if 'hT' in m.dbg:
            dd = m.scr("hT", [128, 8 * S], BF16)
            m.dma(dd, hT.rearrange("p c n -> p (c n)"), slot='dbg')
            m.barrier()
        if stop_after == ('hT', l):
            break

        m.aoff = p1_end
        m.init_wbufs(2, 8 * 512)
        rowbuf = [m.allocb(S) for _ in range(2)]
        tokbuf = [m.allocb(8 * 520) for _ in range(2)]
        ri = [0]
        ti = [0]
        for tb_ in tokbuf:
            m.memset('pool', tb_, 1.0, w=[])
        m.barrier()
        HK = [('hT', t) for t in range(NT)]

        def proj_T(col0, dst_rows_list, evac_eng=('act', 'dve')):
            wb, wk = m.load_w(IN("w_in")[l][:, col0:col0 + 512], 8, 512)
            for j in range(4):
                if dst_rows_list[j] is None:
                    continue
                rb = rowbuf[ri[0] % 2]
                rk = ('row', ri[0] % 2)
                ri[0] += 1
                for tb in range(8):
                    b = m.bank()
                    for k in range(8):
                        m.mm(m.ps[:, b, :], wb[:, k, j * 128:(j + 1) * 128], hT[:, k, tb * 512:(tb + 1) * 512], k == 0, k == 7,
                             r=[wk] + HK[tb * 4:tb * 4 + 4], w=[('ps', b)])
                    m.cp(evac_eng[tb % 2], rb[:, tb * 512:(tb + 1) * 512], m.ps[:, b, :], r=[('ps', b)], w=[rk])
                m.dma(dst_rows_list[j], rb, r=[rk], slot=rk)

        def proj_N(col0, dst, width=512):
            wb, wk = m.load_w(IN("w_in")[l][:, col0:col0 + 512], 8, 512)
            for g in range(4):
                tb_ = tokbuf[ti[0] % 2]
                tk = ('tok', ti[0] % 2)
                ti[0] += 1
                t3 = tb_.rearrange("p (t n) -> p t n", t=8)
                for q in range(8):
                    tt_ = g * 8 + q
                    b = m.bank()
                    for k in range(8):
                        m.mm(m.ps[:, b, :], hT[:, k, tt_ * 128:(tt_ + 1) * 128], wb[:, k, :], k == 0, k == 7,
                             r=[wk, HK[tt_]], w=[('ps', b)])
                    if width == 520:
                        o = t3[:, q, :].rearrange("p (h e) -> p h e", e=65)[:, :, 0:64]
                        i_ = m.ps[:, b, :].rearrange("p (h e) -> p h e", e=64)
                    else:
                        o = t3[:, q, 0:512]
                        i_ = m.ps[:, b, :]
                    m.cp(('act', 'dve')[q % 2], o, i_, r=[('ps', b)], w=[tk])
                m.dma(dst[g * 1024:(g + 1) * 1024, :].rearrange("(t p) n -> p t n", p=128), t3[:, :, 0:width], r=[tk], slot=tk)

        def rows(name, j):
            return qT[name][j * 128:(j + 1) * 128, :]
        proj_T(0, [rows("mqT", j) for j in range(4)])
        proj_T(512, [rows("mkT", j) for j in range(4)])
        proj_N(1024, mv65, 520)
        proj_T(1536, [rows("rqkT", j) for j in range(4)])
        proj_N(2048, tokm["rv"])
        proj_N(2560, tokm["rg"])
        proj_N(3072, tokm["sz"])
        proj_T(4616, [rows("bqT", j) for j in range(4)])
        proj_T(5128, [rows("bkT", j) for j in range(4)])
        proj_N(5640, tokm["bv"])
        m.barrier()
        if stop_after == ('proj', l):
            break

        m.aoff = p1_end
        m.init_wbufs(1, 8 * 512)
        xc = m.alloc(S + 4)
        accb = m.alloc(S)
        sc2 = m.alloc(S)
        outb = sc2[:, 0:S // 2].bitcast(BF16)
        xtok = sc2[:, S // 2:S].bitcast(BF16)
        cwt = m.alloc(32)
        cbt = m.alloc(8)
        dtb = m.alloc(1)
        acol = m.alloc(1)
        m.dma(cwt, IN("conv_w")[l].rearrange("p c j -> p (c j)"), w=['cwt'], slot='misc0')
        m.dma(cbt, IN("conv_b")[l], w=['cbt'], slot='misc1')
        m.dma(dtb[0:8, :], IN("dt_bias")[l], w=['dtb'], slot='misc2')
        m.dma(acol[0:8, :], IN("a_log")[l], w=['acol'], slot='misc3')
        m.act(acol[0:8, :], acol[0:8, :], AF.Exp, r=['acol'], w=['acol'])
        m.ts('dve', acol[0:8, :], acol[0:8, :], -1.0, ALU.mult, r=['acol'], w=['acol'])
        m.memset('pool', accb[0:8, :], 1.0, w=['accb'])
        m.memset('pool', xc[:, 0:3], 0.0, w=['xc'])
        wb, wk = m.load_w(IN("w_in")[l][:, 4608:4616], 8, 8)
        dtT = xc[0:8, 4:4 + S]
        cum = sc2[0:8, :]
        for tb in range(8):
            b = m.bank()
            for k in range(8):
                m.mm(m.ps[0:8, b, :], wb[:, k, 0:8], hT[:, k, tb * 512:(tb + 1) * 512], k == 0, k == 7,
                     r=[wk] + HK[tb * 4:tb * 4 + 4], w=[('ps', b)])
            m.act(dtT[:, tb * 512:(tb + 1) * 512], m.ps[0:8, b, :], AF.Exp, r=[('ps', b), 'dtb'], w=['dtT'], bias=dtb[0:8, :])
        m.act(dtT, dtT, AF.Ln, r=['dtT'], w=['dtT'], bias=1.0)
        m.ts('dve', cum, dtT, acol[0:8, :], ALU.mult, r=['dtT', 'acol'], w=['cum'])
        m.op('dve', lambda e: e.tensor_tensor_scan(out=cum, data0=accb[0:8, :], data1=cum, initial=0.0, op0=ALU.mult, op1=ALU.add),
             r=['cum', 'accb'], w=['cum'])
        m.dma(scumT, cum, r=['cum'], slot='misc4')
        b = m.bank()
        for tt_ in range(NT):
            m.tr(m.ps[:, b, tt_ * 16:tt_ * 16 + 8], dtT[:, tt_ * 128:(tt_ + 1) * 128], ident[0:8, 0:8], r=['dtT', 'ident'], w=[('ps', b)])
            m.tr(m.ps[:, b, tt_ * 16 + 8:tt_ * 16 + 16], cum[:, tt_ * 128:(tt_ + 1) * 128], ident[0:8, 0:8], r=['cum', 'ident'], w=[('ps', b)])
        sdt_sb = accb[:, 512:1024]
        m.cp('dve', sdt_sb, m.ps[:, b, :], r=[('ps', b), 'accb'], w=['sdt_sb'])
        ncv = sdt_sb.rearrange("p (t e) -> p t e", e=16)[:, :, 8:16]
        m.ts('dve', ncv, ncv, -1.0, ALU.mult, r=['sdt_sb'], w=['sdt_sb'])
        m.dma(sdtok, sdt_sb, r=['sdt_sb'], slot='misc5')
        m.barrier()
        for blk in range(2):
            wb, wk = m.load_w(IN("w_in")[l][:, 3584 + blk * 512:3584 + (blk + 1) * 512], 8, 512)
            for jj in range(4):
                j = blk * 4 + jj
                for tb in range(8):
                    b = m.bank()
                    for k in range(8):
                        m.mm(m.ps[:, b, :], wb[:, k, jj * 128:(jj + 1) * 128], hT[:, k, tb * 512:(tb + 1) * 512], k == 0, k == 7,
                             r=[wk] + HK[tb * 4:tb * 4 + 4], w=[('ps', b)])
                    m.cp(('act', 'dve')[tb % 2], xc[:, 3 + tb * 512:3 + (tb + 1) * 512], m.ps[:, b, :], r=[('ps', b)], w=['xc'])
                m.act(accb, xc[:, 3:3 + S], AF.Identity, r=['xc', 'cwt', 'cbt'], w=['accb'], scale=cwt[:, j * 4 + 3:j * 4 + 4], bias=cbt[:, j:j + 1])
                for tap in (2, 1, 0):
                    m.stt(accb, xc[:, tap:tap + S], cwt[:, j * 4 + tap:j * 4 + tap + 1], accb, ALU.mult, ALU.add, r=['xc', 'cwt', 'accb'], w=['accb'])
                m.act(outb, accb, AF.Silu, r=['accb'], w=['outb'])
                if j >= 4:
                    m.dma(qT["sBCT"][(j - 4) * 128:(j - 3) * 128, :], outb, r=['outb'], slot='misc6')
                else:
                    for g in range(4):
                        b = m.bank()
                        pb = m.ps[:, b, :].bitcast(BF16)
                        for q in range(8):
                            tt_ = g * 8 + q
                            m.tr(pb[:, q * 128:(q + 1) * 128], outb[:, tt_ * 128:(tt_ + 1) * 128], identb, r=['outb', 'identb'], w=[('ps', b)])
                        m.cp(('act', 'dve')[g % 2], xtok[:, g * 1024:(g + 1) * 1024], pb, r=[('ps', b)], w=['xtok'])
                    m.dma(tokm["sx"][:, j * 128:(j + 1) * 128].rearrange("(t p) n -> p t n", p=128),
                          xtok.rearrange("p (t n) -> p t n", n=128), r=['xtok'], slot='misc7')
        m.barrier()
        if stop_after == ('ssdpre', l):
            break

        m.aoff = persist_end
        O_all = m.allocb(NT * 512).rearrange("p (t n) -> p t n", n=512)
        qkraw = m.alloc(4 * S // 2)
        qk = [[qkraw[:, (2 * s_ + i_) * 2048:(2 * s_ + i_ + 1) * 2048].bitcast(BF16) for i_ in range(2)] for s_ in range(2)]
        rowb = m.allocb(S)
        Ei = m.alloc(512)
        Ef = m.alloc(512)
        mix_base = m.aoff
        m.op('pool', lambda e: e.iota(Ei.bitcast(mybir.dt.int32), pattern=[[1, 512]], base=0, channel_multiplier=-1), w=['Ei'])
        m.cp('dve', Ef, Ei.bitcast(mybir.dt.int32), r=['Ei'], w=['Ef'])
        qki = [0]

        def load_qk(qname, qrow, kname, krow):
            s_ = qki[0] % 2
            qki[0] += 1
            m.dma(qk[s_][0][0:64, :], qT[qname][qrow:qrow + 64, :], w=[('qk', s_)], slot=('qk', s_, 0))
            m.dma(qk[s_][1][0:64, :], qT[kname][krow:krow + 64, :], r=[('qk', s_)], w=[('qk', s_)], slot=('qk', s_, 1))
            return qk[s_][0][0:64, :], qk[s_][1][0:64, :], ('qk', s_)

        def finalize_branch(n):
            for c in range(4):
                for g in range(4):
                    b = m.bank()
                    pb = m.ps[:, b, :].bitcast(BF16)
                    for q in range(8):
                        tt_ = g * 8 + q
                        m.tr(pb[:, q * 128:(q + 1) * 128], O_all[:, tt_, c * 128:(c + 1) * 128], identb, r=[('O', tt_), 'identb'], w=[('ps', b)])
                    m.cp(('act', 'dve')[g % 2], rowb[:, g * 1024:(g + 1) * 1024], pb, r=[('ps', b)], w=['rowb'])
                m.dma(oT[n][c * 128:(c + 1) * 128, :], rowb, r=['rowb'], slot='rowb')
            m.barrier()

        def mk_decay(dst_full, dst_mask, coef, width, strict=False):
            m.act(dst_full[:, 0:width], Ef[:, 0:width], AF.Exp, r=['Ef'], w=['dfull'], scale=coef)
            m.asel(dst_mask[:, 0:width], dst_full[:, 0:width], [[1, width]], ALU.is_ge, 0.0, -1 if strict else 0, -1, r=['dfull'], w=['dmask'])

        def diag_mask(ap, r, w, strict):
            m.asel(ap, ap, [[1, 128]], ALU.is_ge, 0.0, -1 if strict else 0, -1, r=r, w=w)

        if 'a' in MIX:
            m.aoff = mix_base
            V = m.allocb(NT * 520).rearrange("p (t h e) -> p t h e", h=8, e=65)
            m.dma(V.rearrange("p t h e -> p t (h e)"), mv65.rearrange("(t p) n -> p t n", p=128), w=['V'], slot='V')
            Dfull = m.alloc(256)
            Dmask = m.alloc(256)
            ksum = m.alloc(16)
            ktmp = m.alloc(16)
            khl = m.allocb(32)
            gate = m.alloc(512)
            sel = m.alloc(512)
            m8 = m.alloc(8)
            accs = [m.alloc(130) for _ in range(2)]
            rec = m.alloc(2)
            pes = [m.alloc(256) for _ in range(2)]
            pts = [m.allocb(256) for _ in range(4)]
            cnt = 0
            for h in range(8):
                slope = 2.0 ** (-(h + 1))
                qh, kh, qkk = load_qk("mqT", h * 64, "mkT", h * 64)
                mk_decay(Dfull, Dmask, -slope, 256)
                m.op('dve', lambda e, kh=kh: e.tensor_reduce(out=ksum[0:64, :], in_=kh.rearrange("p (n j) -> p n j", j=256), axis=mybir.AxisListType.X, op=ALU.add),
                     r=[qkk], w=['ksum'])
                m.cp('dve', khl[0:64, 0:16], ksum[0:64, :], r=['ksum'], w=['khl'])
                m.tt('dve', ktmp[0:64, :], ksum[0:64, :], khl[0:64, 0:16], ALU.subtract, r=['ksum', 'khl'], w=['ktmp'])
                m.cp('dve', khl[0:64, 16:32], ktmp[0:64, :], r=['ktmp'], w=['khl'])
                b = m.bank()
                for tt_ in range(NT):
                    m.mm(m.ps[:, b, tt_ * 16:(tt_ + 1) * 16], qh[:, tt_ * 128:(tt_ + 1) * 128], khl[0:64, 0:16], True, False, r=[qkk, 'khl'], w=[('ps', b)])
                    m.mm(m.ps[:, b, tt_ * 16:(tt_ + 1) * 16], qh[:, tt_ * 128:(tt_ + 1) * 128], khl[0:64, 16:32], False, True, r=[qkk, 'khl'], w=[('ps', b)])
                m.cp('dve', gate, m.ps[:, b, :], r=[('ps', b)], w=['gate'])
                m.asel(gate.rearrange("p (t n) -> p t n", n=16), gate.rearrange("p (t n) -> p t n", n=16), [[1, 32], [-2, 16]], ALU.is_ge, -1e30, -2, 0,
                       r=['gate'], w=['gate'])
                for tt_ in range(NT):
                    own = tt_ // 2
                    g_ = gate[:, tt_ * 16:(tt_ + 1) * 16]
                    if own >= 4:
                        m.op('dve', lambda e, g_=g_: e.max(out=m8, in_=g_), r=['gate'], w=['m8'])
                        m.ts('dve', sel[:, tt_ * 16:(tt_ + 1) * 16], g_, m8[:, 2:3], ALU.is_ge, r=['gate', 'm8'], w=['sel'])
                    else:
                        m.ts('dve', sel[:, tt_ * 16:(tt_ + 1) * 16], g_, -1e29, ALU.is_gt, r=['gate'], w=['sel'])
                for qb in range(16):
                    q0 = qb * 256
                    for a_ in accs:
                        m.memset('pool', a_, 0.0, w=[('acc', id(a_))])
                    for n in range(qb + 1):
                        diag = (n == qb)
                        ptl = []
                        for kt2 in range(2):
                            kt = 2 * n + kt2
                            c0 = 128 if (diag and kt2 == 1) else 0
                            b = m.bank()
                            m.mm(m.ps[:, b, c0:256], kh[:, kt * 128:(kt + 1) * 128], qh[:, q0 + c0:q0 + 256], True, True, r=[qkk], w=[('ps', b)])
                            pe_ = pes[cnt % 2]
                            pek = ('pe', cnt % 2)
                            pt = pts[cnt % 4]
                            ptk = ('pt', cnt % 4)
                            cnt += 1
                            m.act(pe_[:, c0:256], m.ps[:, b, c0:256], AF.Exp, r=[('ps', b)], w=[pek], scale=0.125)
                            if diag:
                                m.tt('dve', pt[:, c0:256], pe_[:, c0:256], Dmask[:, 0:256 - c0], ALU.mult, r=[pek, 'dmask'], w=[ptk])
                            else:
                                sc = math.exp(-slope * (q0 - kt * 128))
                                m.stt(pt[:, 0:256], pe_[:, 0:256], sc, Dfull[:, 0:256], ALU.mult, ALU.mult, r=[pek, 'dfull'], w=[ptk])
                            ptl.append((pt, ptk, kt, c0))
                        for qt2 in range(2):
                            tt_ = 2 * qb + qt2
                            use = [(pt, ptk, kt) for (pt, ptk, kt, c0) in ptl if c0 <= qt2 * 128]
                            b = m.bank()
                            for i_, (pt, ptk, kt) in enumerate(use):
                                m.mm(m.ps[:, b, 0:65], pt[:, qt2 * 128:(qt2 + 1) * 128], V[:, kt, h, :], i_ == 0, i_ == len(use) - 1,
                                     r=[ptk, 'V'], w=[('ps', b)])
                            ak = ('acc', id(accs[qt2]))
                            scal = 1.0 if diag else sel[:, tt_ * 16 + n:tt_ * 16 + n + 1]
                            m.stt(accs[qt2][:, 0:65], m.ps[:, b, 0:65], scal, accs[qt2][:, 0:65], ALU.mult, ALU.add, r=[('ps', b), 'sel', ak], w=[ak])
                    for qt2 in range(2):
                        tt_ = 2 * qb + qt2
                        ak = ('acc', id(accs[qt2]))
                        m.op('dve', lambda e, qt2=qt2: e.reciprocal(out=rec[:, qt2:qt2 + 1], in_=accs[qt2][:, 64:65]), r=[ak], w=[('rec', qt2)])
                        m.ts('dve', O_all[:, tt_, h * 64:(h + 1) * 64], accs[qt2][:, 0:64], rec[:, qt2:qt2 + 1], ALU.mult, r=[ak, ('rec', qt2)], w=[('O', tt_)])
            finalize_branch(0)

        if 'b' in MIX:
            m.aoff = mix_base
            V = m.allocb(NT * 512).rearrange("p (t n) -> p t n", n=512)
            G = m.allocb(NT * 512).rearrange("p (t n) -> p t n", n=512)
            m.dma(V, tokm["rv"].rearrange("(t p) n -> p t n", p=128), w=['V'], slot='V')
            m.dma(G, tokm["rg"].rearrange("(t p) n -> p t n", p=128), w=['G'], slot='G')
            gng = m.alloc(512)
            gnb = m.alloc(512)
            m.dma(gng, IN("ret_gn_g")[l].partition_broadcast(128), w=['gng'], slot='misc0')
            m.dma(gnb, IN("ret_gn_b")[l].partition_broadcast(128), w=['gnb'], slot='misc1')
            Dfull = m.alloc(512)
            Dmask = m.alloc(512)
            pts = [m.allocb(512) for _ in range(3)]
            tmpo = [m.alloc(128) for _ in range(2)]
            tmps = [m.alloc(128) for _ in range(2)]
            cnt = 0
            ecnt = 0
            for h in range(4):
                lg = math.log(1.0 - 2.0 ** (-5.0 - h))
                qh, kh, qkk = load_qk("rqkT", h * 64, "rqkT", 256 + h * 64)
                mk_decay(Dfull, Dmask, lg, 512)
                for qb in range(8):
                    q0 = qb * 512
                    bo = m.bank()
                    first = True
                    for kt in range(4 * qb + 4):
                        mdg = kt - 4 * qb
                        c0 = 128 * max(mdg, 0)
                        b = m.bank()
                        if b == bo:
                            b = m.bank()
                        m.mm(m.ps[:, b, c0:512], kh[:, kt * 128:(kt + 1) * 128], qh[:, q0 + c0:q0 + 512], True, True, r=[qkk], w=[('ps', b)])
                        pt = pts[cnt % 3]
                        ptk = ('pt', cnt % 3)
                        cnt += 1
                        if mdg >= 0:
                            m.stt(pt[:, c0:512], m.ps[:, b, c0:512], 0.125, Dmask[:, 0:512 - c0], ALU.mult, ALU.mult, r=[('ps', b), 'dmask'], w=[ptk])
                        else:
                            sc = 0.125 * math.exp(lg * (q0 - kt * 128))
                            m.stt(pt, m.ps[:, b, :], sc, Dfull, ALU.mult, ALU.mult, r=[('ps', b), 'dfull'], w=[ptk])
                        for qt in range(max(mdg, 0), 4):
                            m.mm(m.ps[:, bo, qt * 128:(qt + 1) * 128], pt[:, qt * 128:(qt + 1) * 128], V[:, kt, h * 128:(h + 1) * 128], first, False,
                                 r=[ptk, 'V'], w=[('ps', bo)])
                            first = False
                    for qt in range(4):
                        tt_ = 4 * qb + qt
                        e_ = ecnt % 2
                        ecnt += 1
                        sm = small[:, 64 + e_ * 32:64 + (e_ + 1) * 32]
                        smk = ('sm', e_)
                        o_ = m.ps[:, bo, qt * 128:(qt + 1) * 128]
                        m.op('dve', lambda e, sm=sm, o_=o_: e.bn_stats(out=sm[:, 0:6], in_=o_), r=[('ps', bo)], w=[smk])
                        m.op('dve', lambda e, sm=sm: e.bn_aggr(out=sm[:, 6:8], in_=sm[:, 0:6]), r=[smk], w=[smk])
                        m.ts('dve', sm[:, 8:9], sm[:, 7:8], NORM_EPS, ALU.add, r=[smk], w=[smk])
                        m.act(sm[:, 9:10], sm[:, 8:9], AF.Sqrt, r=[smk], w=[smk])
                        m.op('dve', lambda e, sm=sm: e.reciprocal(out=sm[:, 10:11], in_=sm[:, 9:10]), r=[smk], w=[smk])
                        to = tmpo[e_]
                        tk = ('tmpo', e_)
                        m.ts('dve', to, o_, sm[:, 6:7], ALU.subtract, r=[('ps', bo), smk], w=[tk], s2=sm[:, 10:11], op1=ALU.mult)
                        m.tt('pool', to, to, gng[:, h * 128:(h + 1) * 128], ALU.mult, r=[tk, 'gng'], w=[tk])
                        m.tt('pool', to, to, gnb[:, h * 128:(h + 1) * 128], ALU.add, r=[tk, 'gnb'], w=[tk])
                        m.act(tmps[e_], G[:, tt_, h * 128:(h + 1) * 128], AF.Silu, r=['G'], w=[('tmps', e_)])
                        m.tt('pool', O_all[:, tt_, h * 128:(h + 1) * 128], to, tmps[e_], ALU.mult, r=[tk, ('tmps', e_)], w=[('O', tt_)])
            finalize_branch(1)

        if 'c' in MIX:
            m.aoff = mix_base
            X = m.allocb(NT * 512).rearrange("p (t n) -> p t n", n=512)
            Z = m.allocb(NT * 512).rearrange("p (t n) -> p t n", n=512)
            m.dma(X, tokm["sx"].rearrange("(t p) n -> p t n", p=128), w=['X'], slot='V')
            m.dma(Z, tokm["sz"].rearrange("(t p) n -> p t n", p=128), w=['Z'], slot='G')
            sdt = m.alloc(512)
            m.dma(sdt, sdtok, w=['sdt'], slot='misc0')
            dsk = m.alloc(8)
            m.dma(dsk, IN("d_skip")[l].partition_broadcast(128), w=['dsk'], slot='misc1')
            ngb = m.alloc(512)
            m.dma(ngb, IN("ssm_norm_g")[l].partition_broadcast(128), w=['ngb'], slot='misc2')
            BT = qk[0][0]
            CT = qk[0][1]
            Gbc = qkraw[:, 4096:8192]
            xdt = m.allocb(NT * 64).rearrange("p (t n) -> p t n", n=64)
            ss = m.alloc(NT * 8)
            rstd = m.alloc(NT * 2)
            Ls = [m.alloc(512) for _ in range(2)]
            wts = [m.allocb(512) for _ in range(3)]
            ytmp = [m.alloc(64) for _ in range(2)]
            ztmp = [m.alloc(64) for _ in range(2)]
            junk = m.alloc(64)
            sdt3 = sdt.rearrange("p (t e) -> p t e", e=16)
            cnt = 0
            ecnt = 0
            for g in range(2):
                m.dma(BT, qT["sBCT"][g * 128:(g + 1) * 128, :], w=['BT'], slot='misc3')
                m.dma(CT, qT["sBCT"][256 + g * 128:256 + (g + 1) * 128, :], w=['CT'], slot='misc4')
                for r_ in range(4):
                    h = 4 * g + r_
                    m.dma(Gbc, scumT[h:h + 1, :].partition_broadcast(128), w=['Gbc'], slot='misc5')
                    for tt_ in range(NT):
                        m.ts('pool', xdt[:, tt_, :], X[:, tt_, h * 64:(h + 1) * 64], sdt3[:, tt_, h:h + 1], ALU.mult, r=['X', 'sdt'], w=['xdt'])
                    for tb in range(8):
                        t0 = tb * 512
                        by = m.bank()
                        first = True
                        for st in range(4 * tb + 4):
                            mdg = st - 4 * tb
                            c0 = 128 * max(mdg, 0)
                            b = m.bank()
                            if b == by:
                                b = m.bank()
                            m.mm(m.ps[:, b, c0:512], BT[:, st * 128:(st + 1) * 128], CT[:, t0 + c0:t0 + 512], True, True, r=['BT', 'CT'], w=[('ps', b)])
                            L = Ls[cnt % 2]
                            lk = ('L', cnt % 2)
                            wt = wts[cnt % 3]
                            wk_ = ('wt', cnt % 3)
                            cnt += 1
                            m.act(L[:, c0:512], Gbc[:, t0 + c0:t0 + 512], AF.Exp, r=['Gbc', 'sdt'], w=[lk], bias=sdt3[:, st, 8 + h:9 + h])
                            if mdg >= 0:
                                diag_mask(L[:, c0:c0 + 128], r=[lk], w=[lk], strict=False)
                            m.tt('dve', wt[:, c0:512], L[:, c0:512], m.ps[:, b, c0:512], ALU.mult, r=[lk, ('ps', b)], w=[wk_])
                            for qt in range(max(mdg, 0), 4):
                                m.mm(m.ps[:, by, qt * 64:(qt + 1) * 64], wt[:, qt * 128:(qt + 1) * 128], xdt[:, st, :], first, False,
                                     r=[wk_, 'xdt'], w=[('ps', by)])
                                first = False
                        for qt in range(4):
                            tt_ = 4 * tb + qt
                            e_ = ecnt % 2
                            ecnt += 1
                            yk = ('ytmp', e_)
                            m.stt(ytmp[e_], X[:, tt_, h * 64:(h + 1) * 64], dsk[:, h:h + 1], m.ps[:, by, qt * 64:(qt + 1) * 64], ALU.mult, ALU.add,
                                  r=['X', 'dsk', ('ps', by)], w=[yk])
                            m.act(ztmp[e_], Z[:, tt_, h * 64:(h + 1) * 64], AF.Silu, r=['Z'], w=[('ztmp', e_)])
                            m.tt('dve', ytmp[e_], ytmp[e_], ztmp[e_], ALU.mult, r=[yk, ('ztmp', e_)], w=[yk])
                            m.op('act', lambda e, e_=e_, tt_=tt_, h=h: e.activation(out=junk, in_=ytmp[e_], func=AF.Square, accum_out=ss[:, tt_ * 8 + h:tt_ * 8 + h + 1]),
                                 r=[yk], w=['junk', ('ss', tt_)])
                            m.cp('pool', O_all[:, tt_, h * 64:(h + 1) * 64], ytmp[e_], r=[yk], w=[('O', tt_)])
                ssv = ss.rearrange("p (t e) -> p t e", e=8)[:, :, 4 * g:4 * g + 4]
                rg_ = rstd[:, g * NT:(g + 1) * NT]
                m.op('dve', lambda e, ssv=ssv, rg_=rg_: e.tensor_reduce(out=rg_, in_=ssv, axis=mybir.AxisListType.X, op=ALU.add),
                     r=[('ss', t) for t in range(NT)], w=[('rstd', g)])
                m.ts('dve', rg_, rg_, 1.0 / 256.0, ALU.mult, r=[('rstd', g)], w=[('rstd', g)], s2=LN_EPS, op1=ALU.add)
                m.act(rg_, rg_, AF.Sqrt, r=[('rstd', g)], w=[('rstd', g)])
                m.op('dve', lambda e, rg_=rg_: e.reciprocal(out=rg_, in_=rg_), r=[('rstd', g)], w=[('rstd', g)])
                for tt_ in range(NT):
                    o_ = O_all[:, tt_, g * 256:(g + 1) * 256]
                    m.stt(o_, o_, rg_[:, tt_:tt_ + 1], ngb[:, g * 256:(g + 1) * 256], ALU.mult, ALU.mult, r=[('O', tt_), ('rstd', g), 'ngb'], w=[('O', tt_)])
            finalize_branch(2)

        if 'd' in MIX:
            m.aoff = mix_base
            V = m.allocb(NT * 512).rearrange("p (t n) -> p t n", n=512)
            m.dma(V, tokm["bv"].rearrange("(t p) n -> p t n", p=128), w=['V'], slot='V')
            triU = m.allocb(128)
            triL = m.allocb(128)
            onesb = m.allocb(128)
            m.memset('pool', onesb, 1.0, w=['onesb'])
            m.asel(triU, onesb, [[-1, 128]], ALU.is_ge, 0.0, -1, 1, r=['onesb'], w=['triU'])
            m.asel(triL, onesb, [[1, 128]], ALU.is_ge, 0.0, 0, -1, r=['onesb'], w=['triL'])
            NS = 3
            es = [m.alloc(512) for _ in range(NS)]
            sps = [m.alloc(512) for _ in range(NS)]
            spbs = [m.allocb(512) for _ in range(NS)]
            ts_ = [m.alloc(512) for _ in range(NS)]
            wts = [m.allocb(512) for _ in range(NS)]
            BACC, BO = 6, 7
            zb = [0]

            def zbank():
                i = zb[0]
                zb[0] = (zb[0] + 1) % 6
                return i
            cnt = 0
            import os
            for h in range(int(os.environ.get('SB_H', 8))):
                qh, kh, qkk = load_qk("bqT", h * 64, "bkT", h * 64)
                for qb in range(int(os.environ.get('SB_QB', 8))):
                    q0 = qb * 512
                    kts = list(range(4 * qb + 3, -1, -1))
                    st_ = {}

                    def stageA(kt):
                        nonlocal cnt
                        i = cnt % NS
                        cnt += 1
                        mdg = kt - 4 * qb
                        c0 = 128 * max(mdg, 0)
                        b = zbank()
                        m.mm(m.ps[:, b, c0:512], kh[:, kt * 128:(kt + 1) * 128], qh[:, q0 + c0:q0 + 512], True, True, r=[qkk], w=[('ps', b)])
                        m.act(es[i][:, c0:512], m.ps[:, b, c0:512], AF.Exp, r=[('ps', b)], w=[('es', i)], scale=0.125)
                        m.act(sps[i][:, c0:512], es[i][:, c0:512], AF.Ln, r=[('es', i)], w=[('sps', i)], bias=1.0)
                        m.cp('pool', spbs[i][:, c0:512], sps[i][:, c0:512], r=[('sps', i)], w=[('spb', i)])
                        if mdg >= 0:
                            diag_mask(spbs[i][:, c0:c0 + 128], r=[('spb', i)], w=[('spb', i)], strict=True)
                        m.stt(ts_[i][:, c0:512], m.ps[:, b, c0:512], 0.125, sps[i][:, c0:512], ALU.mult, ALU.subtract, r=[('ps', b), ('sps', i)], w=[('ts', i)])
                        st_[kt] = (i, c0, mdg)

                    def stageB(kt, first):
                        i, c0, mdg = st_[kt]
                        m.mm(m.ps[:, BACC, c0:512], triU, spbs[i][:, c0:512], first, False, r=['triU', ('spb', i)], w=[('ps', BACC)])
                        m.tt('dve', ts_[i][:, c0:512], ts_[i][:, c0:512], m.ps[:, BACC, c0:512], ALU.subtract, r=[('ts', i), ('ps', BACC)], w=[('ts', i)])
                        m.mm(m.ps[:, BACC, c0:512], triL, spbs[i][:, c0:512], False, False, r=['triL', ('spb', i)], w=[('ps', BACC)])
                        m.act(wts[i][:, c0:512], ts_[i][:, c0:512], AF.Exp, r=[('ts', i)], w=[('wt', i)])
                        if mdg >= 0:
                            diag_mask(wts[i][:, c0:c0 + 128], r=[('wt', i)], w=[('wt', i)], strict=True)

                    def stageC(kt, first):
                        i, c0, mdg = st_[kt]
                        for qt in range(max(mdg, 0), 4):
                            m.mm(m.ps[:, BO, qt * 64:(qt + 1) * 64], wts[i][:, qt * 128:(qt + 1) * 128], V[:, kt, h * 64:(h + 1) * 64], first and qt == max(mdg, 0), False,
                                 r=[('wt', i), 'V'], w=[('ps', BO)])
                    n_ = len(kts)
                    stageA(kts[0])
                    for i_ in range(n_):
                        if i_ + 1 < n_:
                            stageA(kts[i_ + 1])
                        stageB(kts[i_], i_ == 0)
                        if i_ >= 1:
                            stageC(kts[i_ - 1], i_ - 1 == 0)
                    stageC(kts[n_ - 1], n_ - 1 == 0)
                    for qt in range(4):
                        tt_ = 4 * qb + qt
                        m.cp('dve', O_all[:, tt_, h * 64:(h + 1) * 64], m.ps[:, BO, qt * 64:(qt + 1) * 64], r=[('ps', BO)], w=[('O', tt_)])
            finalize_branch(3)
        m.barrier()
        if stop_after == ('mix', l):
            break

        m.aoff = persist_end
        hT = _r3(m.allocb(8 * S), 8)
        p1_end = m.aoff
        xts = [m.alloc(D) for _ in range(3)]
        make_hT(hT, x_src, scp[:, 0:8], modT[:, 0:8], range(NT))
        m.barrier()
        m.aoff = p1_end
        wst4 = [m.alloc(8 * 512)] * 2
        wg4 = [m.allocb(8 * 512) for _ in range(2)]
        wb4 = [m.allocb(4 * 512) for _ in range(2)]
        obs = [m.allocb(16 * 512) for _ in range(2)]
        rowbs = [m.allocb(S) for _ in range(2)]
        sigs = [m.alloc(512) for _ in range(2)]
        accs4 = [m.alloc(512) for _ in range(2)]
        tmps4 = [m.alloc(512) for _ in range(2)]
        oc = 0
        sc_ = 0
        for dc in range(8):
            i = dc % 2
            st_g = wst4[0].rearrange("p (c n j) -> p c n j", c=8, n=4)
            for n in range(4):
                c0 = 6152 + n * 1024 + dc * 128
                m.dma(st_g[:, :, n, :], IN("w_in")[l][:, c0:c0 + 128].rearrange("(c p) j -> p c j", p=128), w=[('wst4', 0)], r=[('wst4', 0)], slot=('wst4', 0, n))
            wg = wg4[i].rearrange("p (c n j) -> p c n j", c=8, n=4)
            m.cp('pool', wg4[i], wst4[i], r=[('wst4', 0)], w=[('wg4', i)])
            st_b = wst4[i][:, 0:2048].rearrange("p (n c j) -> p n c j", n=4, c=4)
            for n in range(4):
                m.dma(st_b[:, n, :, :], IN("w_br")[l][n][:, dc * 128:(dc + 1) * 128].rearrange("(c p) j -> p c j", p=128), w=[('wst4', 0)], r=[('wst4', 0)], slot=('wst4', 0, n))
            wbr = wb4[i].rearrange("p (n c j) -> p n c j", n=4, c=4)
            m.cp('pool', wb4[i], wst4[i][:, 0:2048], r=[('wst4', 0)], w=[('wb4', i)])
            rb = rowbs[dc % 2]
            rk = ('rowb4', dc % 2)
            for tb in range(8):
                o_i = oc % 2
                oc += 1
                ob = obs[o_i].rearrange("p (n c t) -> p n c t", n=4, c=4)
                for n in range(4):
                    m.dma(ob[:, n, :, :], oT[n][:, tb * 512:(tb + 1) * 512].rearrange("(c p) t -> p c t", p=128), w=[('ob', o_i, n)], slot=('ob', o_i, n))
                a_i = tb % 2
                for n in range(4):
                    bg = m.bank()
                    for k in range(8):
                        m.mm(m.ps[:, bg, :], wg[:, k, n, :], hT[:, k, tb * 512:(tb + 1) * 512], k == 0, k == 7, r=[('wg4', i)] + HK[tb * 4:tb * 4 + 4], w=[('ps', bg)])
                    by = m.bank()
                    for c in range(4):
                        m.mm(m.ps[:, by, :], wbr[:, n, c, :], ob[:, n, c, :], c == 0, c == 3, r=[('wb4', i), ('ob', o_i, n)], w=[('ps', by)])
                    s_i = sc_ % 2
                    sc_ += 1
                    m.act(sigs[s_i], m.ps[:, bg, :], AF.Sigmoid, r=[('ps', bg)], w=[('sig', s_i)])
                    if n == 0:
                        m.tt('dve', accs4[a_i], sigs[s_i], m.ps[:, by, :], ALU.mult, r=[('sig', s_i), ('ps', by)], w=[('acc4', a_i)])
                    else:
                        m.tt('dve', tmps4[s_i], sigs[s_i], m.ps[:, by, :], ALU.mult, r=[('sig', s_i), ('ps', by)], w=[('tmp4', s_i)])
                        if n < 3:
                            m.tt('pool', accs4[a_i], accs4[a_i], tmps4[s_i], ALU.add, r=[('acc4', a_i), ('tmp4', s_i)], w=[('acc4', a_i)])
                        else:
                            m.tt('pool', rb[:, tb * 512:(tb + 1) * 512], accs4[a_i], tmps4[s_i], ALU.add, r=[('acc4', a_i), ('tmp4', s_i)], w=[rk])
            m.dma(mergedT[dc * 128:(dc + 1) * 128, :], rb, r=[rk], slot=rk)
        m.barrier()
        if stop_after == ('merge', l):
            break

        m.aoff = persist_end
        m.init_wbufs(2, 8 * 512)
        wo = [m.load_w(IN("w_out")[l][:, hh * 512:(hh + 1) * 512], 8, 512) for hh in range(2)]
        mts = [m.allocb(8 * 128) for _ in range(2)]
        xts = [m.alloc(D) for _ in range(3)]
        tmp5 = [m.alloc(D) for _ in range(2)]
        out5 = [m.alloc(D) for _ in range(2)]
        for tt_ in range(NT):
            i = tt_ % 2
            mt = _r3(mts[i], 8)
            m.dma(mt, mergedT[:, tt_ * 128:(tt_ + 1) * 128].rearrange("(c p) t -> p c t", p=128), w=[('mt', i)], slot=('mt', i))
            xs = tt_ % 3
            m.dma(xts[xs], x_src[tt_ * 128:(tt_ + 1) * 128, :], w=[('xt', xs)], slot=('xt', xs))
            b2 = m.bank2()
            for hh in range(2):
                for k in range(8):
                    m.mm(m.ps[:, b2 + hh, :], mt[:, k, :], wo[hh][0][:, k, :], k == 0, k == 7, r=[('mt', i), wo[hh][1]], w=[('ps', b2 + hh)])
            ps2 = (m.ps[:, b2:b2 + 2, :].rearrange("p a n -> p (a n)"), [('ps', b2), ('ps', b2 + 1)])
            ln_epilogue(ps2, xts[xs], ('xt', xs), gbc[:, 0:D], lnbc[:, 0:D], lnbc[:, D:2 * D], out5[i], ('out5', i), tmp5[i], ('tmp5', i), i)
            m.dma(x1_d[tt_ * 128:(tt_ + 1) * 128, :], out5[i], r=[('out5', i)], slot=('out5', i))
        m.barrier()
        if stop_after == ('ln1', l):
            break

        m.aoff = persist_end
        moe = (l % 2 == 1)
        TBK = 1024
        h2T = _r3(m.allocb(8 * TBK), 8)
        yacc = _r3(m.alloc(8 * TBK), 8)
        aTs = [m.allocb(4 * TBK).rearrange("p (c t) -> p c t", c=4) for _ in range(2)]
        wst6 = [m.alloc(8 * 512) for _ in range(2)]
        wbf6 = [m.allocb(8 * 512) for _ in range(4)]
        xts = [m.alloc(D) for _ in range(2)]
        tmp6 = [m.alloc(D) for _ in range(1)]
        out6 = [m.alloc(D) for _ in range(2)]
        sil = [m.alloc(512) for _ in range(2)]
        tmy = [m.alloc(512) for _ in range(2)]
        wsi = [0]
        wbi = [0]

        def load6(src_r3, kc, n):
            si = wsi[0] % 2
            wsi[0] += 1
            bi = wbi[0] % 4
            wbi[0] += 1
            st = _r3(wst6[si][:, 0:kc * n], kc)
            m.dma(st, src_r3, w=[('wst6', si)], slot=('wst6', si))
            bf = _r3(wbf6[bi][:, 0:kc * n], kc)
            m.cp('pool', bf, st, r=[('wst6', si)], w=[('wbf6', bi)])
            return bf, ('wbf6', bi)
        if moe:
            rw = m.alloc(64)
            rbb = m.alloc(8)
            selE = m.alloc(8 * 128)
            combT = m.alloc(TBK)
            cbt_ = m.alloc(TBK)
            hf = [m.alloc(8 * 128)] * 2
            rs = m.alloc(64)
            cpad = m.alloc(128)
            m.memset('pool', cpad, 0.0, w=['cpad'])
            m.dma(_r3(rw, 8), IN("router_w")[0].rearrange("(c p) e -> p c e", p=128), w=['rw'], slot='misc0')
            m.dma(rbb, IN("router_b")[0].partition_broadcast(128), w=['rbb'], slot='misc1')
            m.memset('pool', selE[0:8, :], 1.0, w=['selE'])
            for e_ in range(8):
                m.ts('dve', selE[0:8, e_ * 128:(e_ + 1) * 128], selE[0:8, e_ * 128:(e_ + 1) * 128], ident[0:8, e_:e_ + 1], ALU.mult, r=['selE', 'ident'], w=['selE'])
            experts = [(IN("expert_w_gu")[0][e], IN("expert_w_down")[0][e], D_FFE, e) for e in range(8)]
        else:
            experts = [(IN("ffn_w_gu")[l // 2], IN("ffn_w_down")[l // 2], D_FF, None)]
        for tbk in range(S // TBK):
            tts = list(range(tbk * 8, tbk * 8 + 8))
            for tt_ in tts:
                s_ = tt_ % 2
                m.dma(xts[s_], x1_d[tt_ * 128:(tt_ + 1) * 128, :], w=[('xt', s_)], slot=('xt', s_))
                b2 = m.bank2()
                for c in range(8):
                    bb, cc = b2 + c // 4, (c % 4) * 128
                    m.tr(m.ps[:, bb, cc:cc + 128], xts[s_][:, c * 128:(c + 1) * 128], ident, r=[('xt', s_), 'ident'], w=[('ps', bb)])
                o = (tt_ - tbk * 8) * 128
                import os
                MA = int(os.environ.get('MOE_A', 9))
                if not moe:
                    for c in range(8):
                        bb, cc = b2 + c // 4, (c % 4) * 128
                        m.act(h2T[:, c, o:o + 128], m.ps[:, bb, cc:cc + 128], AF.Identity, r=[('ps', bb), 'scp', 'modT'],
                              w=[('h2T', tt_ % 8)], scale=scp[:, 8 + c:9 + c], bias=modT[:, 24 + c:25 + c])
                else:
                    hfi = hf[0]
                    hk = ('hf', 0)
                    for c in range(8):
                        bb, cc = b2 + c // 4, (c % 4) * 128
                        m.act(hfi[:, c * 128:(c + 1) * 128], m.ps[:, bb, cc:cc + 128], AF.Identity, r=[('ps', bb), 'scp', 'modT'],
                              w=[hk], scale=scp[:, 8 + c:9 + c], bias=modT[:, 24 + c:25 + c])
                    m.cp('pool', h2T[:, :, o:o + 128], _r3(hfi, 8), r=[hk], w=[('h2T', tt_ % 8)])
                if moe and MA >= 1:
                    bl = m.bank()
                    for c in range(8):
                        m.mm(m.ps[:, bl, 0:8], hfi[:, c * 128:(c + 1) * 128], rw[:, c * 8:(c + 1) * 8], c == 0, c == 7, r=[hk, 'rw'], w=[('ps', bl)])
                    m.tt('dve', rs[:, 0:8], m.ps[:, bl, 0:8], rbb, ALU.add, r=[('ps', bl), 'rbb'], w=['rs'])
                if moe and MA >= 2:
                    m.op('dve', lambda e: e.max(out=rs[:, 8:16], in_=rs[:, 0:8]), r=['rs'], w=['rs'])
                    m.ts('dve', rs[:, 16:24], rs[:, 0:8], rs[:, 9:10], ALU.is_ge, r=['rs'], w=['rs'])
                    m.ts('dve', rs[:, 24:25], rs[:, 8:9], -1.0, ALU.mult, r=['rs'], w=['rs'])
                    m.act(rs[:, 32:40], rs[:, 0:8], AF.Exp, r=['rs'], w=['rs'], bias=rs[:, 24:25])
                    m.act(rs[:, 25:26], rs[:, 9:10], AF.Exp, r=['rs'], w=['rs'], bias=rs[:, 24:25])
                    m.ts('dve', rs[:, 25:26], rs[:, 25:26], 1.0, ALU.add, r=['rs'], w=['rs'])
                    m.op('dve', lambda e: e.reciprocal(out=rs[:, 26:27], in_=rs[:, 25:26]), r=['rs'], w=['rs'])
                    m.tt('dve', rs[:, 40:48], rs[:, 32:40], rs[:, 16:24], ALU.mult, r=['rs'], w=['rs'])
                    m.ts('dve', rs[:, 40:48], rs[:, 40:48], rs[:, 26:27], ALU.mult, r=['rs'], w=['rs'])
                if moe and MA >= 3:
                    bt = m.bank()
                    m.cp('dve', cpad[:, 0:8], rs[:, 40:48], r=['rs'], w=['cpad'])
                    m.tr(m.ps[:, bt, 0:128], cpad, ident, r=['cpad', 'ident'], w=[('ps', bt)])
                    m.cp('dve', combT[0:8, o:o + 128], m.ps[0:8, bt, 0:128], r=[('ps', bt)], w=['combT'])
            import os
            MD = os.environ.get('MOE_DBG', 'ABC')
            m.memset('pool', yacc, 0.0, w=['yacc'])
            H2K = [('h2T', i_) for i_ in range(8)]
            gi = 0
            for (wgu, wdn, F, e) in (experts if 'B' in MD else []):
                if e is not None:
                    for half in range(2):
                        bc_ = m.bank()
                        m.mm(m.ps[:, bc_, :], selE[0:8, e * 128:(e + 1) * 128], combT[0:8, half * 512:(half + 1) * 512], True, True, r=['selE', 'combT'], w=[('ps', bc_)])
                        m.cp('act', cbt_[:, half * 512:(half + 1) * 512], m.ps[:, bc_, :], r=[('ps', bc_)], w=['cbt_'])
                nfc = F // 128
                for f0 in range(0, nfc, 4):
                    ncq = min(4, nfc - f0)
                    ncol = ncq * 128
                    gw, gk = load6(wgu[:, f0 * 128:f0 * 128 + ncol].rearrange("(c p) n -> p c n", p=128), 8, ncol)
                    uw, uk = load6(wgu[:, F + f0 * 128:F + f0 * 128 + ncol].rearrange("(c p) n -> p c n", p=128), 8, ncol)
                    dw, dk = load6(wdn[f0 * 128:f0 * 128 + ncol, :].rearrange("(c p) n -> p c n", p=128), ncq, D)
                    aT = aTs[gi % 2]
                    ak = ('aT', gi % 2)
                    gi += 1
                    for fc in range(ncq):
                        for half in range(2):
                            pg = m.bank()
                            for k in range(8):
                                m.mm(m.ps[:, pg, :], gw[:, k, fc * 128:(fc + 1) * 128], h2T[:, k, half * 512:(half + 1) * 512], k == 0, k == 7,
                                     r=[gk] + H2K[half * 4:half * 4 + 4], w=[('ps', pg)])
                            pu = m.bank()
                            for k in range(8):
                                m.mm(m.ps[:, pu, :], uw[:, k, fc * 128:(fc + 1) * 128], h2T[:, k, half * 512:(half + 1) * 512], k == 0, k == 7,
                                     r=[uk] + H2K[half * 4:half * 4 + 4], w=[('ps', pu)])
                            s_i = (fc * 2 + half) % 2
                            m.act(sil[s_i], m.ps[:, pg, :], AF.Silu, r=[('ps', pg)], w=[('sil', s_i)])
                            m.tt('dve', aT[:, fc, half * 512:(half + 1) * 512], sil[s_i], m.ps[:, pu, :], ALU.mult, r=[('sil', s_i), ('ps', pu)], w=[ak])
                    for dc in range(8):
                        for half in range(2):
                            py = m.bank()
                            for fc in range(ncq):
                                m.mm(m.ps[:, py, :], dw[:, fc, dc * 128:(dc + 1) * 128], aT[:, fc, half * 512:(half + 1) * 512], fc == 0, fc == ncq - 1,
                                     r=[dk, ak], w=[('ps', py)])
                            ya = yacc[:, dc, half * 512:(half + 1) * 512]
                            yk = ('yacc', dc, half)
                            if e is not None:
                                t_i = (dc * 2 + half) % 2
                                m.tt('dve', tmy[t_i], m.ps[:, py, :], cbt_[:, half * 512:(half + 1) * 512], ALU.mult, r=[('ps', py), 'cbt_'], w=[('tmy', t_i)])
                                m.tt('pool', ya, ya, tmy[t_i], ALU.add, r=['yacc', yk, ('tmy', t_i)], w=[yk])
                            else:
                                m.tt('dve', ya, ya, m.ps[:, py, :], ALU.add, r=['yacc', yk, ('ps', py)], w=[yk])
            YK = [('yacc', dc, half) for dc in range(8) for half in range(2)] + ['yacc']
            for tt_ in (tts if 'C' in MD else []):
                s_ = tt_ % 2
                o = (tt_ - tbk * 8) * 128
                m.dma(xts[s_], x1_d[tt_ * 128:(tt_ + 1) * 128, :], w=[('xt', s_)], slot=('xt', s_))
                b2 = m.bank2()
                for c in range(8):
                    bb, cc = b2 + c // 4, (c % 4) * 128
                    m.tr(m.ps[:, bb, cc:cc + 128], yacc[:, c, o:o + 128], ident, r=YK + ['ident'], w=[('ps', bb)])
                ps2 = (m.ps[:, b2:b2 + 2, :].rearrange("p a n -> p (a n)"), [('ps', b2), ('ps', b2 + 1)])
                ln_epilogue(ps2, xts[s_], ('xt', s_), gbc[:, D:2 * D], lnbc[:, 2 * D:3 * D], lnbc[:, 3 * D:4 * D], out6[s_], ('out6', s_), tmp6[0], ('tmp6', 0), s_)
                m.dma(x_dst[tt_ * 128:(tt_ + 1) * 128, :], out6[s_], r=[('out6', s_)], slot=('out6', s_))
        m.barrier()
        if stop_after == ('ffn', l):
            break
    m.barrier()
    nc_ = m.finish()
    nc_.used_inputs = list(_ins.keys())
    return nc_


_CACHE = {}


def _prep_inputs(inputs, b):
    f = lambda a: np.ascontiguousarray(a, dtype=np.float32)
    R = {
        "x": lambda a: a[b],
        "c": lambda a: np.asarray(a[b]).reshape(8, 128).T,
        "b_ada": lambda a: np.asarray(a).reshape(2, 1, 6 * D),
        "conv_w": lambda a: np.asarray(a).reshape(2, 4, 8, 128).transpose(0, 3, 2, 1),
        "conv_b": lambda a: np.asarray(a).reshape(2, 8, 128).transpose(0, 2, 1),
        "dt_bias": lambda a: np.asarray(a).reshape(2, 8, 1),
        "a_log": lambda a: np.asarray(a).reshape(2, 8, 1),
        "d_skip": lambda a: np.asarray(a).reshape(2, 1, 8),
        "ssm_norm_g": lambda a: np.asarray(a).reshape(2, 1, 512),
        "ret_gn_g": lambda a: np.asarray(a).reshape(2, 1, 512),
        "ret_gn_b": lambda a: np.asarray(a).reshape(2, 1, 512),
        "ln1_g": lambda a: np.asarray(a).reshape(2, 1, D),
        "ln1_b": lambda a: np.asarray(a).reshape(2, 1, D),
        "ln2_g": lambda a: np.asarray(a).reshape(2, 1, D),
        "ln2_b": lambda a: np.asarray(a).reshape(2, 1, D),
        "router_b": lambda a: np.asarray(a).reshape(1, 1, 8),
    }
    return {k: f(R[k](v) if k in R else v) for k, v in inputs.items()}


def kernel(**inputs):
    nc = build()
    shared = _prep_inputs(inputs, 0)
    in_maps = []
    for b in range(8):
        d = dict(shared)
        d["x"] = np.ascontiguousarray(inputs["x"][b], dtype=np.float32)
        d["c"] = np.ascontiguousarray(np.asarray(inputs["c"][b]).reshape(8, 128).T, dtype=np.float32)
        in_maps.append(d)
    res = run_bass_kernel_spmd(nc, in_maps, core_ids=list(range(8)))
    return np.stack([np.asarray(r["y"], dtype=np.float32) for r in res.results], axis=0)
```

```python
import contextlib
import math
import numpy as np
import concourse.bass as bass
import concourse.mybir as mybir
from concourse.bass_utils import run_bass_kernel_spmd

F32 = mybir.dt.float32
BF16 = mybir.dt.bfloat16
AF = mybir.ActivationFunctionType
ALU = mybir.AluOpType

S = 4096
D = 1024
NT = 32
IN_W = 10248
ALPHA = 4.0 ** 0.25
LN_EPS = 1e-5
NORM_EPS = 1e-6
D_FF = 2816
D_FFE = 3584


class Builder:
    ENGS = ('pe', 'act', 'dve', 'pool', 'sp')

    def __init__(self):
        self.nc = bass.Bass("TRN2", target_bir_lowering=False)
        self.stack = contextlib.ExitStack()
        self.q = {e: [] for e in self.ENGS}
        self.seq = {e: 0 for e in self.ENGS}
        self.known = {e: {} for e in self.ENGS}
        self.last_w = {}
        self.readers = {}
        self.slots = {}
        self.sems = {}
        for e in self.ENGS:
            self.sems[('e', e)] = self.stack.enter_context(self.nc.semaphore("c_" + e))
        self._uid = 0

    def sbuf(self, shape, dtype, name=None):
        self._uid += 1
        return self.stack.enter_context(self.nc.sbuf_tensor(name or f"sb{self._uid}", list(shape), dtype))

    def psum(self, shape, dtype, name=None):
        self._uid += 1
        return self.stack.enter_context(self.nc.psum_tensor(name or f"ps{self._uid}", list(shape), dtype))

    def dram(self, name, shape, dtype, kind="Internal"):
        return self.nc.dram_tensor(name, list(shape), dtype, kind=kind).ap()

    def _deps(self, eng, r, w, skip_same_w=False):
        deps = {}

        def add(tok):
            s, v = tok
            if deps.get(s, 0) < v:
                deps[s] = v
        for k in r:
            t = self.last_w.get(k)
            if t is not None:
                add(t)
        for k in w:
            t = self.last_w.get(k)
            if t is not None and not (skip_same_w and t[0] == ('e', eng)):
                add(t)
            for t in self.readers.get(k, ()):
                add(t)
        waits = []
        kn = self.known[eng]
        for s, v in deps.items():
            if kn.get(s, 0) >= v:
                continue
            kn[s] = v
            waits.append((s, v))
        return waits

    def _record(self, tok, r, w):
        for k in w:
            self.last_w[k] = tok
            self.readers[k] = []
        for k in r:
            self.readers.setdefault(k, []).append(tok)

    def op(self, eng, fn, r=(), w=()):
        waits = self._deps(eng, r, w, skip_same_w=(eng == 'pe'))
        self.seq[eng] += 1
        tok = (('e', eng), self.seq[eng])
        self.q[eng].append((fn, waits, (('e', eng), 1)))
        self._record(tok, r, w)
        return tok

    def dma(self, out, in_, r=(), w=(), slot=None, q='sp'):
        if slot not in self.slots:
            self.sems[('d', slot)] = self.stack.enter_context(self.nc.semaphore("d%d" % len(self.slots)))
            self.slots[slot] = [('d', slot), 0]
        waits = self._deps(q, r, w)
        ent = self.slots[slot]
        ent[1] += 16
        tok = (ent[0], ent[1])
        self.q[q].append((lambda e, o=out, i=in_: e.dma_start(out=o, in_=i), waits, (ent[0], 16)))
        self._record(tok, r, w)
        return tok

    def barrier(self):
        toks = []
        for e in self.ENGS:
            if self.seq[e] > 0:
                toks.append((('e', e), self.seq[e]))
        for k, ent in self.slots.items():
            if ent[1] > 0:
                toks.append((ent[0], ent[1]))
        for e in self.ENGS:
            kn = self.known[e]
            waits = []
            for s, v in toks:
                if kn.get(s, 0) >= v:
                    continue
                kn[s] = v
                waits.append((s, v))
            if waits:
                self.q[e].append((None, waits, None))
        self.last_w.clear()
        self.readers.clear()

    def finish(self):
        self.barrier()
        nc = self.nc
        sems = self.sems

        def run(eng_obj, lst):
            for fn, waits, inc in lst:
                for s, v in waits:
                    eng_obj.wait_ge(sems[s], v)
                if fn is not None:
                    fn(eng_obj).then_inc(sems[inc[0]], inc[1])
        with nc.Block() as block:
            @block.tensor
            def _(e):
                run(e, self.q['pe'])

            @block.scalar
            def _(e):
                run(e, self.q['act'])

            @block.vector
            def _(e):
                run(e, self.q['dve'])

            @block.gpsimd
            def _(e):
                run(e, self.q['pool'])

            @block.sync
            def _(e):
                run(e, self.q['sp'])
        self.stack.close()
        return nc


def _r3(ap, c):
    return ap.rearrange("p (c n) -> p c n", c=c)


class MK(Builder):
    AW = 50 * 1024

    def __init__(self, dbg=()):
        super().__init__()
        self.dbg = set(dbg)
        self.arena = self.sbuf([128, self.AW], F32, "arena")
        self.aoff = 0
        self.ps = self.psum([128, 8, 512], F32, "psum")
        self.pcur = 0
        self.ktag = 0

    def alloc(self, words):
        a = self.aoff
        self.aoff += words
        assert self.aoff <= self.AW, (self.aoff, self.AW)
        return self.arena[:, a:a + words]

    def allocb(self, n):
        assert n % 2 == 0
        return self.alloc(n // 2).bitcast(BF16)

    def key(self, name):
        self.ktag += 1
        return (name, self.ktag)

    def bank(self):
        i = self.pcur
        self.pcur = (self.pcur + 1) % 8
        return i

    def bank6(self):
        i = getattr(self, 'p6', 0)
        self.p6 = (i + 1) % 6
        return i

    def bank2(self):
        if self.pcur % 2:
            self.pcur = (self.pcur + 1) % 8
        i = self.pcur
        self.pcur = (self.pcur + 2) % 8
        return i

    def scr(self, name, shape, dtype):
        return self.dram(name, shape, dtype, kind="ExternalOutput" if name in self.dbg else "Internal")

    def mm(self, out, lhsT, rhs, start, stop, r, w):
        self.op('pe', lambda e: e.matmul(out, lhsT=lhsT, rhs=rhs, start=start, stop=stop), r=r, w=w)

    def tr(self, out, in_, ident, r, w):
        self.op('pe', lambda e: e.transpose(out=out, in_=in_, identity=ident), r=r, w=w)

    def act(self, out, in_, func, r, w, bias=None, scale=None):
        kw = {}
        if bias is not None:
            kw['bias'] = bias
        if scale is not None:
            kw['scale'] = scale
        self.op('act', lambda e: e.activation(out=out, in_=in_, func=func, **kw), r=r, w=w)

    def tt(self, eng, out, in0, in1, op, r, w):
        self.op(eng, lambda e: e.tensor_tensor(out=out, in0=in0, in1=in1, op=op), r=r, w=w)

    def ts(self, eng, out, in0, s1, op0, r, w, s2=None, op1=None):
        if op1 is None:
            self.op(eng, lambda e: e.tensor_scalar(out=out, in0=in0, scalar1=s1, scalar2=None, op0=op0), r=r, w=w)
        else:
            self.op(eng, lambda e: e.tensor_scalar(out=out, in0=in0, scalar1=s1, scalar2=s2, op0=op0, op1=op1), r=r, w=w)

    def stt(self, out, in0, scalar, in1, op0, op1, r, w):
        self.op('dve', lambda e: e.scalar_tensor_tensor(out=out, in0=in0, scalar=scalar, in1=in1, op0=op0, op1=op1), r=r, w=w)

    def cp(self, eng, out, in_, r, w):
        if eng == 'act':
            self.op('act', lambda e: e.copy(out=out, in_=in_), r=r, w=w)
        else:
            self.op(eng, lambda e: e.tensor_copy(out=out, in_=in_), r=r, w=w)

    def memset(self, eng, ap, val, w):
        self.op(eng, lambda e: e.memset(ap, val), w=w)

    def asel(self, out, in_, pattern, cmp, fill, base, cm, r, w):
        self.op('pool', lambda e: e.affine_select(out=out, in_=in_, pattern=pattern, compare_op=cmp, fill=fill,
                                                  base=base, channel_multiplier=cm), r=r, w=w)

    def init_wbufs(self, nslots, words):
        self.wst = [self.alloc(words) for _ in range(nslots)]
        self.wbf = [self.alloc(words // 2).bitcast(BF16) for _ in range(nslots)]
        self.wi = 0
        self.wn = nslots

    def load_w(self, src, kc, n, cast=True):
        i = self.wi
        self.wi = (self.wi + 1) % self.wn
        st = _r3(self.wst[i][:, 0:kc * n], kc)
        self.dma(st, src.rearrange("(c p) n -> p c n", p=128), w=[('wst', i)], slot=('wst', i))
        if not cast:
            return st, ('wst', i)
        bf = _r3(self.wbf[i][:, 0:kc * n], kc)
        self.cp('pool', bf, st, r=[('wst', i)], w=[('wbf', i)])
        return bf, ('wbf', i)


def build(dbg=(), stop_after=None, layers=(0, 1), MIX='abcd', skip=(), x_first=False):
    m = MK(dbg)
    nc = m.nc

    SHAPES = dict(x=[S, D], c=[128, 8], w_ada=[2, D, 6 * D], b_ada=[2, 1, 6 * D], w_in=[2, D, IN_W],
                  conv_w=[2, 128, 8, 4], conv_b=[2, 128, 8], dt_bias=[2, 8, 1], a_log=[2, 8, 1], d_skip=[2, 1, 8],
                  ssm_norm_g=[2, 1, 512], ret_gn_g=[2, 1, 512], ret_gn_b=[2, 1, 512], w_br=[2, 4, 512, D],
                  w_out=[2, D, D], ln1_g=[2, 1, D], ln1_b=[2, 1, D], ln2_g=[2, 1, D], ln2_b=[2, 1, D],
                  ffn_w_gu=[1, D, 2 * D_FF], ffn_w_down=[1, D_FF, D], router_w=[1, D, 8], router_b=[1, 1, 8],
                  expert_w_gu=[1, 8, D, 2 * D_FFE], expert_w_down=[1, 8, D_FFE, D])
    _ins = {}

    def IN(name):
        if name not in _ins:
            _ins[name] = m.dram(name, SHAPES[name], F32, kind="ExternalInput")
        return _ins[name]
    m.used_inputs = _ins
    y_out = m.dram("y", [S, D], F32, kind="ExternalOutput")

    qT = {n: m.scr(n, [512, S], BF16) for n in ("mqT", "mkT", "rqkT", "bqT", "bkT", "sBCT")}
    mv65 = m.scr("mv65", [S, 520], BF16)
    tokm = {n: m.scr(n, [S, 512], BF16) for n in ("rv", "rg", "sz", "bv", "sx")}
    scumT = m.scr("scumT", [8, S], F32)
    sdtok = m.scr("sdtok", [128, NT * 16], F32)
    oT = m.scr("oT", [4, 512, S], BF16)
    mergedT = m.scr("mergedT", [D, S], BF16)
    x1_d = m.scr("x1", [S, D], F32)
    x2_d = m.scr("x2", [S, D], F32)

    ident = m.alloc(128)
    identb = m.allocb(128)
    ones_row = m.alloc(128)
    one11 = ones_row[0:1, 0:1]
    modT = m.alloc(48)
    scp = m.alloc(16)
    gbc = m.alloc(2 * D)
    lnbc = m.alloc(4 * D)
    small = m.alloc(256)
    persist_end = m.aoff

    m.memset('pool', ident, 1.0, w=['ident'])
    m.asel(ident, ident, [[-1, 128]], ALU.is_equal, 0.0, 0, 1, r=['ident'], w=['ident'])
    m.cp('pool', identb, ident, r=['ident'], w=['identb'])
    m.memset('pool', ones_row, 1.0, w=['ones'])
    m.barrier()

    def ln_epilogue(ps2, xt, xkey, g_ap, lg, lb, out_t, okey, tmp, tkey, st):
        pk, stk = ps2[1], ('st', st)
        m.tt('dve', tmp, ps2[0], g_ap, ALU.mult, r=list(pk) + ['gbc'], w=[tkey])
        m.stt(tmp, xt, ALPHA, tmp, ALU.mult, ALU.add, r=[xkey, tkey], w=[tkey])
        sm = small[:, st * 32:(st + 1) * 32]
        m.op('dve', lambda e: e.bn_stats(out=sm[:, 0:6], in_=tmp[:, 0:512]), r=[tkey], w=[stk])
        m.op('dve', lambda e: e.bn_stats(out=sm[:, 6:12], in_=tmp[:, 512:1024]), r=[tkey], w=[stk])
        m.op('dve', lambda e: e.bn_aggr(out=sm[:, 12:14], in_=sm[:, 0:12]), r=[stk], w=[stk])
        m.ts('dve', sm[:, 14:15], sm[:, 13:14], LN_EPS, ALU.add, r=[stk], w=[stk])
        m.act(sm[:, 15:16], sm[:, 14:15], AF.Sqrt, r=[stk], w=[stk])
        m.op('dve', lambda e: e.reciprocal(out=sm[:, 16:17], in_=sm[:, 15:16]), r=[stk], w=[stk])
        m.ts('dve', tmp, tmp, sm[:, 12:13], ALU.subtract, r=[tkey, stk], w=[tkey], s2=sm[:, 16:17], op1=ALU.mult)
        m.tt('pool', tmp, tmp, lg, ALU.mult, r=[tkey, 'lnbc'], w=[tkey])
        m.tt('pool', out_t, tmp, lb, ALU.add, r=[tkey, 'lnbc'], w=[okey])

    for l in layers:
        x_src = IN("x") if (l == 0 or x_first) else x2_d
        x_dst = x2_d if l == 0 else y_out
        m.aoff = persist_end
        cT = m.alloc(8)
        cact = m.alloc(8)
        modrow = m.alloc(6 * D)
        brow = m.alloc(6 * D)
        m.init_wbufs(2, 8 * 512)
        m.dma(cT, IN("c"), w=['cT'], slot='misc0')
        m.dma(brow[0:1, :], IN("b_ada")[l], w=['brow'], slot='misc1')
        for i, k in enumerate(("ln1_g", "ln1_b", "ln2_g", "ln2_b")):
            m.dma(lnbc[:, i * D:(i + 1) * D], IN(k)[l].partition_broadcast(128), w=['lnbc'], slot=('lnbc', i))
        m.act(cact, cT, AF.Silu, r=['cT'], w=['cact'])
        for j in range(12):
            wb, wk = m.load_w(IN("w_ada")[l][:, j * 512:(j + 1) * 512], 8, 512, cast=False)
            b = m.bank()
            for k in range(8):
                m.mm(m.ps[0:1, b, :], cact[:, k:k + 1], wb[:, k, :], k == 0, k == 7, r=['cact', wk], w=[('ps', b)])
            m.tt('dve', modrow[0:1, j * 512:(j + 1) * 512], m.ps[0:1, b, :], brow[0:1, j * 512:(j + 1) * 512], ALU.add,
                 r=[('ps', b), 'brow'], w=['modrow'])
        b = m.bank()
        for j in range(48):
            m.mm(m.ps[:, b, j:j + 1], modrow[0:1, j * 128:(j + 1) * 128], one11, True, True, r=['modrow', 'ones'], w=[('ps', b)])
        m.cp('dve', modT, m.ps[:, b, 0:48], r=[('ps', b)], w=['modT'])
        m.ts('dve', scp[:, 0:8], modT[:, 8:16], 1.0, ALU.add, r=['modT'], w=['scp'])
        m.ts('dve', scp[:, 8:16], modT[:, 32:40], 1.0, ALU.add, r=['modT'], w=['scp'])
        for gi, c0 in enumerate((2 * D, 5 * D)):
            for hh in range(2):
                b = m.bank()
                m.mm(m.ps[:, b, :], ones_row[0:1, 0:128], modrow[0:1, c0 + hh * 512:c0 + (hh + 1) * 512], True, True,
                     r=['modrow', 'ones'], w=[('ps', b)])
                m.cp('act', gbc[:, gi * D + hh * 512:gi * D + (hh + 1) * 512], m.ps[:, b, :], r=[('ps', b)], w=['gbc'])
        m.barrier()
        if 'modT' in m.dbg:
            dd = m.scr("modT", [128, 48], F32)
            m.dma(dd, modT, r=['modT'], slot='dbg')
        if stop_after == ('mod', l):
            break

        m.aoff = persist_end
        hT = _r3(m.allocb(8 * S), 8)
        p1_end = m.aoff
        xts = [m.alloc(D) for _ in range(3)]

        def make_hT(dst, src_d, sc_ap, sh_ap, tts, dst_off=0):
            for tt_ in tts:
                s_ = tt_ % 3
                m.dma(xts[s_], src_d[tt_ * 128:(tt_ + 1) * 128, :], w=[('xt', s_)], slot=('xt', s_))
                b2 = m.bank2()
                for c in range(8):
                    bb, cc = b2 + c // 4, (c % 4) * 128
                    m.tr(m.ps[:, bb, cc:cc + 128], xts[s_][:, c * 128:(c + 1) * 128], ident, r=[('xt', s_), 'ident'], w=[('ps', bb)])
                for c in range(8):
                    bb, cc = b2 + c // 4, (c % 4) * 128
                    o = (tt_ - dst_off) * 128
                    m.act(dst[:, c, o:o + 128], m.ps[:, bb, cc:cc + 128], AF.Identity, r=[('ps', bb), 'scp', 'modT'],
                          w=[('hT', tt_)], scale=sc_ap[:, c:c + 1], bias=sh_ap[:, c:c + 1])
        make_hT(hT, x_src, scp[:, 0:8], modT[:, 0:8], range(NT))
        m.barrier()
        if 'hT' in m.dbg:
            dd = m.scr("hT", [128, 8 * S], BF16)
            m.dma(dd, hT.rearrange("p c n -> p (c n)"), slot='dbg')
            m.barrier()
        if stop_after == ('hT', l):
            break

        m.aoff = p1_end
        m.init_wbufs(2, 8 * 512)
        rowbuf = [m.allocb(S) for _ in range(2)]
        tokbuf = [m.allocb(8 * 520) for _ in range(2)]
        ri = [0]
        ti = [0]
        for tb_ in tokbuf:
            m.memset('pool', tb_, 1.0, w=[])
        m.barrier()
        HK = [('hT', t) for t in range(NT)]

        def proj_T(col0, dst_rows_list, evac_eng=('act', 'dve')):
            wb, wk = m.load_w(IN("w_in")[l][:, col0:col0 + 512], 8, 512)
            for j in range(4):
                if dst_rows_list[j] is None:
                    continue
                rb = rowbuf[ri[0] % 2]
                rk = ('row', ri[0] % 2)
                ri[0] += 1
                for tb in range(8):
                    b = m.bank()
                    for k in range(8):
                        m.mm(m.ps[:, b, :], wb[:, k, j * 128:(j + 1) * 128], hT[:, k, tb * 512:(tb + 1) * 512], k == 0, k == 7,
                             r=[wk] + HK[tb * 4:tb * 4 + 4], w=[('ps', b)])
                    m.cp(evac_eng[tb % 2], rb[:, tb * 512:(tb + 1) * 512], m.ps[:, b, :], r=[('ps', b)], w=[rk])
                m.dma(dst_rows_list[j], rb, r=[rk], slot=rk)

        def proj_N(col0, dst, width=512):
            wb, wk = m.load_w(IN("w_in")[l][:, col0:col0 + 512], 8, 512)
            for g in range(4):
                tb_ = tokbuf[ti[0] % 2]
                tk = ('tok', ti[0] % 2)
                ti[0] += 1
                t3 = tb_.rearrange("p (t n) -> p t n", t=8)
                for q in range(8):
                    tt_ = g * 8 + q
                    b = m.bank()
                    for k in range(8):
                        m.mm(m.ps[:, b, :], hT[:, k, tt_ * 128:(tt_ + 1) * 128], wb[:, k, :], k == 0, k == 7,
                             r=[wk, HK[tt_]], w=[('ps', b)])
                    if width == 520:
                        o = t3[:, q, :].rearrange("p (h e) -> p h e", e=65)[:, :, 0:64]
                        i_ = m.ps[:, b, :].rearrange("p (h e) -> p h e", e=64)
                    else:
                        o = t3[:, q, 0:512]
                        i_ = m.ps[:, b, :]
                    m.cp(('act', 'dve')[q % 2], o, i_, r=[('ps', b)], w=[tk])
                m.dma(dst[g * 1024:(g + 1) * 1024, :].rearrange("(t p) n -> p t n", p=128), t3[:, :, 0:width], r=[tk], slot=tk)

        def rows(name, j):
            return qT[name][j * 128:(j + 1) * 128, :]
        proj_T(0, [rows("mqT", j) for j in range(4)])
        proj_T(512, [rows("mkT", j) for j in range(4)])
        proj_N(1024, mv65, 520)
        proj_T(1536, [rows("rqkT", j) for j in range(4)])
        proj_N(2048, tokm["rv"])
        proj_N(2560, tokm["rg"])
        proj_N(3072, tokm["sz"])
        proj_T(4616, [rows("bqT", j) for j in range(4)])
        proj_T(5128, [rows("bkT", j) for j in range(4)])
        proj_N(5640, tokm["bv"])
        m.barrier()
        if stop_after == ('proj', l):
            break

        m.aoff = p1_end
        m.init_wbufs(1, 8 * 512)
        xc = m.alloc(S + 4)
        accb = m.alloc(S)
        sc2 = m.alloc(S)
        outb = sc2[:, 0:S // 2].bitcast(BF16)
        xtok = sc2[:, S // 2:S].bitcast(BF16)
        cwt = m.alloc(32)
        cbt = m.alloc(8)
        dtb = m.alloc(1)
        acol = m.alloc(1)
        m.dma(cwt, IN("conv_w")[l].rearrange("p c j -> p (c j)"), w=['cwt'], slot='misc0')
        m.dma(cbt, IN("conv_b")[l], w=['cbt'], slot='misc1')
        m.dma(dtb[0:8, :], IN("dt_bias")[l], w=['dtb'], slot='misc2')
        m.dma(acol[0:8, :], IN("a_log")[l], w=['acol'], slot='misc3')
        m.act(acol[0:8, :], acol[0:8, :], AF.Exp, r=['acol'], w=['acol'])
        m.ts('dve', acol[0:8, :], acol[0:8, :], -1.0, ALU.mult, r=['acol'], w=['acol'])
        m.memset('pool', accb[0:8, :], 1.0, w=['accb'])
        m.memset('pool', xc[:, 0:3], 0.0, w=['xc'])
        wb, wk = m.load_w(IN("w_in")[l][:, 4608:4616], 8, 8)
        dtT = xc[0:8, 4:4 + S]
        cum = sc2[0:8, :]
        for tb in range(8):
            b = m.bank()
            for k in range(8):
                m.mm(m.ps[0:8, b, :], wb[:, k, 0:8], hT[:, k, tb * 512:(tb + 1) * 512], k == 0, k == 7,
                     r=[wk] + HK[tb * 4:tb * 4 + 4], w=[('ps', b)])
            m.act(dtT[:, tb * 512:(tb + 1) * 512], m.ps[0:8, b, :], AF.Exp, r=[('ps', b), 'dtb'], w=['dtT'], bias=dtb[0:8, :])
        m.act(dtT, dtT, AF.Ln, r=['dtT'], w=['dtT'], bias=1.0)
        m.ts('dve', cum, dtT, acol[0:8, :], ALU.mult, r=['dtT', 'acol'], w=['cum'])
        m.op('dve', lambda e: e.tensor_tensor_scan(out=cum, data0=accb[0:8, :], data1=cum, initial=0.0, op0=ALU.mult, op1=ALU.add),
             r=['cum', 'accb'], w=['cum'])
        m.dma(scumT, cum, r=['cum'], slot='misc4')
        b = m.bank()
        for tt_ in range(NT):
            m.tr(m.ps[:, b, tt_ * 16:tt_ * 16 + 8], dtT[:, tt_ * 128:(tt_ + 1) * 128], ident[0:8, 0:8], r=['dtT', 'ident'], w=[('ps', b)])
            m.tr(m.ps[:, b, tt_ * 16 + 8:tt_ * 16 + 16], cum[:, tt_ * 128:(tt_ + 1) * 128], ident[0:8, 0:8], r=['cum', 'ident'], w=[('ps', b)])
        sdt_sb = accb[:, 512:1024]
        m.cp('dve', sdt_sb, m.ps[:, b, :], r=[('ps', b), 'accb'], w=['sdt_sb'])
        ncv = sdt_sb.rearrange("p (t e) -> p t e", e=16)[:, :, 8:16]
        m.ts('dve', ncv, ncv, -1.0, ALU.mult, r=['sdt_sb'], w=['sdt_sb'])
        m.dma(sdtok, sdt_sb, r=['sdt_sb'], slot='misc5')
        m.barrier()
        for blk in range(2):
            wb, wk = m.load_w(IN("w_in")[l][:, 3584 + blk * 512:3584 + (blk + 1) * 512], 8, 512)
            for jj in range(4):
                j = blk * 4 + jj
                for tb in range(8):
                    b = m.bank()
                    for k in range(8):
                        m.mm(m.ps[:, b, :], wb[:, k, jj * 128:(jj + 1) * 128], hT[:, k, tb * 512:(tb + 1) * 512], k == 0, k == 7,
                             r=[wk] + HK[tb * 4:tb * 4 + 4], w=[('ps', b)])
                    m.cp(('act', 'dve')[tb % 2], xc[:, 3 + tb * 512:3 + (tb + 1) * 512], m.ps[:, b, :], r=[('ps', b)], w=['xc'])
                m.act(accb, xc[:, 3:3 + S], AF.Identity, r=['xc', 'cwt', 'cbt'], w=['accb'], scale=cwt[:, j * 4 + 3:j * 4 + 4], bias=cbt[:, j:j + 1])
                for tap in (2, 1, 0):
                    m.stt(accb, xc[:, tap:tap + S], cwt[:, j * 4 + tap:j * 4 + tap + 1], accb, ALU.mult, ALU.add, r=['xc', 'cwt', 'accb'], w=['accb'])
                m.act(outb, accb, AF.Silu, r=['accb'], w=['outb'])
                if j >= 4:
                    m.dma(qT["sBCT"][(j - 4) * 128:(j - 3) * 128, :], outb, r=['outb'], slot='misc6')
                else:
                    for g in range(4):
                        b = m.bank()
                        pb = m.ps[:, b, :].bitcast(BF16)
                        for q in range(8):
                            tt_ = g * 8 + q
                            m.tr(pb[:, q * 128:(q + 1) * 128], outb[:, tt_ * 128:(tt_ + 1) * 128], identb, r=['outb', 'identb'], w=[('ps', b)])
                        m.cp(('act', 'dve')[g % 2], xtok[:, g * 1024:(g + 1) * 1024], pb, r=[('ps', b)], w=['xtok'])
                    m.dma(tokm["sx"][:, j * 128:(j + 1) * 128].rearrange("(t p) n -> p t n", p=128),
                          xtok.rearrange("p (t n) -> p t n", n=128), r=['xtok'], slot='misc7')
        m.barrier()
        if stop_after == ('ssdpre', l):
            break

        m.aoff = persist_end
        O_all = m.allocb(NT * 512).rearrange("p (t n) -> p t n", n=512)
        qkraw = m.alloc(4 * S // 2)
        qk = [[qkraw[:, (2 * s_ + i_) * 2048:(2 * s_ + i_ + 1) * 2048].bitcast(BF16) for i_ in range(2)] for s_ in range(2)]
        rowb = m.allocb(S)
        Ei = m.alloc(512)
        Ef = m.alloc(512)
        mix_base = m.aoff
        m.op('pool', lambda e: e.iota(Ei.bitcast(mybir.dt.int32), pattern=[[1, 512]], base=0, channel_multiplier=-1), w=['Ei'])
        m.cp('dve', Ef, Ei.bitcast(mybir.dt.int32), r=['Ei'], w=['Ef'])
        qki = [0]

        def load_qk(qname, qrow, kname, krow):
            s_ = qki[0] % 2
            qki[0] += 1
            m.dma(qk[s_][0][0:64, :], qT[qname][qrow:qrow + 64, :], w=[('qk', s_)], slot=('qk', s_, 0))
            m.dma(qk[s_][1][0:64, :], qT[kname][krow:krow + 64, :], r=[('qk', s_)], w=[('qk', s_)], slot=('qk', s_, 1))
            return qk[s_][0][0:64, :], qk[s_][1][0:64, :], ('qk', s_)

        def finalize_branch(n):
            for c in range(4):
                for g in range(4):
                    b = m.bank()
                    pb = m.ps[:, b, :].bitcast(BF16)
                    for q in range(8):
                        tt_ = g * 8 + q
                        m.tr(pb[:, q * 128:(q + 1) * 128], O_all[:, tt_, c * 128:(c + 1) * 128], identb, r=[('O', tt_), 'identb'], w=[('ps', b)])
                    m.cp(('act', 'dve')[g % 2], rowb[:, g * 1024:(g + 1) * 1024], pb, r=[('ps', b)], w=['rowb'])
                m.dma(oT[n][c * 128:(c + 1) * 128, :], rowb, r=['rowb'], slot='rowb')
            m.barrier()

        def mk_decay(dst_full, dst_mask, coef, width, strict=False):
            m.act(dst_full[:, 0:width], Ef[:, 0:width], AF.Exp, r=['Ef'], w=['dfull'], scale=coef)
            m.asel(dst_mask[:, 0:width], dst_full[:, 0:width], [[1, width]], ALU.is_ge, 0.0, -1 if strict else 0, -1, r=['dfull'], w=['dmask'])

        def diag_mask(ap, r, w, strict):
            m.asel(ap, ap, [[1, 128]], ALU.is_ge, 0.0, -1 if strict else 0, -1, r=r, w=w)

        if 'a' in MIX:
            m.aoff = mix_base
            V = m.allocb(NT * 520).rearrange("p (t h e) -> p t h e", h=8, e=65)
            m.dma(V.rearrange("p t h e -> p t (h e)"), mv65.rearrange("(t p) n -> p t n", p=128), w=['V'], slot='V')
            Dfull = m.alloc(256)
            Dmask = m.alloc(256)
            ksum = m.alloc(16)
            ktmp = m.alloc(16)
            khl = m.allocb(32)
            gate = m.alloc(512)
            sel = m.alloc(512)
            m8 = m.alloc(8)
            accs = [m.alloc(130) for _ in range(4)]
            rec = m.alloc(4)
            pes = [m.alloc(256) for _ in range(4)]
            pts = [m.allocb(256) for _ in range(6)]
            cnt = 0
            for h in range(8):
                slope = 2.0 ** (-(h + 1))
                qh, kh, qkk = load_qk("mqT", h * 64, "mkT", h * 64)
                mk_decay(Dfull, Dmask, -slope, 256)
                m.op('dve', lambda e, kh=kh: e.tensor_reduce(out=ksum[0:64, :], in_=kh.rearrange("p (n j) -> p n j", j=256), axis=mybir.AxisListType.X, op=ALU.add),
                     r=[qkk], w=['ksum'])
                m.cp('dve', khl[0:64, 0:16], ksum[0:64, :], r=['ksum'], w=['khl'])
                m.tt('dve', ktmp[0:64, :], ksum[0:64, :], khl[0:64, 0:16], ALU.subtract, r=['ksum', 'khl'], w=['ktmp'])
                m.cp('dve', khl[0:64, 16:32], ktmp[0:64, :], r=['ktmp'], w=['khl'])
                b = m.bank()
                for tt_ in range(NT):
                    m.mm(m.ps[:, b, tt_ * 16:(tt_ + 1) * 16], qh[:, tt_ * 128:(tt_ + 1) * 128], khl[0:64, 0:16], True, False, r=[qkk, 'khl'], w=[('ps', b)])
                    m.mm(m.ps[:, b, tt_ * 16:(tt_ + 1) * 16], qh[:, tt_ * 128:(tt_ + 1) * 128], khl[0:64, 16:32], False, True, r=[qkk, 'khl'], w=[('ps', b)])
                m.cp('dve', gate, m.ps[:, b, :], r=[('ps', b)], w=['gate'])
                m.asel(gate.rearrange("p (t n) -> p t n", n=16), gate.rearrange("p (t n) -> p t n", n=16), [[1, 32], [-2, 16]], ALU.is_ge, -1e30, -2, 0,
                       r=['gate'], w=['gate'])
                for tt_ in range(NT):
                    own = tt_ // 2
                    g_ = gate[:, tt_ * 16:(tt_ + 1) * 16]
                    if own >= 4:
                        m.op('dve', lambda e, g_=g_: e.max(out=m8, in_=g_), r=['gate'], w=['m8'])
                        m.ts('dve', sel[:, tt_ * 16:(tt_ + 1) * 16], g_, m8[:, 2:3], ALU.is_ge, r=['gate', 'm8'], w=['sel'])
                    else:
                        m.ts('dve', sel[:, tt_ * 16:(tt_ + 1) * 16], g_, -1e29, ALU.is_gt, r=['gate'], w=['sel'])
                pend = [None]

                def flush():
                    if pend[0] is not None:
                        pend[0]()
                        pend[0] = None
                for qb in range(16):
                    q0 = qb * 256
                    acq = accs[(qb % 2) * 2:(qb % 2) * 2 + 2]
                    for a_ in acq:
                        m.memset('pool', a_, 0.0, w=[('acc', id(a_))])
                    for n in range(qb + 1):
                        diag = (n == qb)
                        ptl = []
                        for kt2 in range(2):
                            kt = 2 * n + kt2
                            c0 = 128 if (diag and kt2 == 1) else 0
                            b = m.bank()
                            m.mm(m.ps[:, b, c0:256], kh[:, kt * 128:(kt + 1) * 128], qh[:, q0 + c0:q0 + 256], True, True, r=[qkk], w=[('ps', b)])
                            pe_ = pes[cnt % 4]
                            pek = ('pe', cnt % 4)
                            pt = pts[cnt % 6]
                            ptk = ('pt', cnt % 6)
                            cnt += 1
                            m.act(pe_[:, c0:256], m.ps[:, b, c0:256], AF.Exp, r=[('ps', b)], w=[pek], scale=0.125)
                            if diag:
                                m.tt('dve', pt[:, c0:256], pe_[:, c0:256], Dmask[:, 0:256 - c0], ALU.mult, r=[pek, 'dmask'], w=[ptk])
                            else:
                                sc = math.exp(-slope * (q0 - kt * 128))
                                m.stt(pt[:, 0:256], pe_[:, 0:256], sc, Dfull[:, 0:256], ALU.mult, ALU.mult, r=[pek, 'dfull'], w=[ptk])
                            ptl.append((pt, ptk, kt, c0))

                        def back(ptl=ptl, qb=qb, n=n, diag=diag, acq=acq):
                            for qt2 in range(2):
                                tt_ = 2 * qb + qt2
                                use = [(pt, ptk, kt) for (pt, ptk, kt, c0) in ptl if c0 <= qt2 * 128]
                                b = m.bank()
                                for i_, (pt, ptk, kt) in enumerate(use):
                                    m.mm(m.ps[:, b, 0:65], pt[:, qt2 * 128:(qt2 + 1) * 128], V[:, kt, h, :], i_ == 0, i_ == len(use) - 1,
                                         r=[ptk, 'V'], w=[('ps', b)])
                                ak = ('acc', id(acq[qt2]))
                                scal = 1.0 if diag else sel[:, tt_ * 16 + n:tt_ * 16 + n + 1]
                                m.stt(acq[qt2][:, 0:65], m.ps[:, b, 0:65], scal, acq[qt2][:, 0:65], ALU.mult, ALU.add, r=[('ps', b), 'sel', ak], w=[ak])
                            if diag:
                                for qt2 in range(2):
                                    tt_ = 2 * qb + qt2
                                    ak = ('acc', id(acq[qt2]))
                                    rc = rec[:, (qb % 2) * 2 + qt2:(qb % 2) * 2 + qt2 + 1]
                                    rk_ = ('rec', (qb % 2) * 2 + qt2)
                                    m.op('dve', lambda e, rc=rc, a_=acq[qt2]: e.reciprocal(out=rc, in_=a_[:, 64:65]), r=[ak], w=[rk_])
                                    m.ts('dve', O_all[:, tt_, h * 64:(h + 1) * 64], acq[qt2][:, 0:64], rc, ALU.mult, r=[ak, rk_], w=[('O', tt_)])
                        prev = pend[0]
                        pend[0] = back
                        if prev is not None:
                            prev()
                flush()
            finalize_branch(0)

        if 'b' in MIX:
            m.aoff = mix_base
            V = m.allocb(NT * 512).rearrange("p (t n) -> p t n", n=512)
            G = m.allocb(NT * 512).rearrange("p (t n) -> p t n", n=512)
            m.dma(V, tokm["rv"].rearrange("(t p) n -> p t n", p=128), w=['V'], slot='V')
            m.dma(G, tokm["rg"].rearrange("(t p) n -> p t n", p=128), w=['G'], slot='G')
            gng = m.alloc(512)
            gnb = m.alloc(512)
            m.dma(gng, IN("ret_gn_g")[l].partition_broadcast(128), w=['gng'], slot='misc0')
            m.dma(gnb, IN("ret_gn_b")[l].partition_broadcast(128), w=['gnb'], slot='misc1')
            Dfull = m.alloc(512)
            Dmask = m.alloc(512)
            pts = [m.allocb(512) for _ in range(4)]
            tmpo = [m.alloc(128) for _ in range(2)]
            tmps = [m.alloc(128) for _ in range(2)]
            cnt = 0
            ecnt = 0
            rqb = 0
            for h in range(4):
                lg = math.log(1.0 - 2.0 ** (-5.0 - h))
                qh, kh, qkk = load_qk("rqkT", h * 64, "rqkT", 256 + h * 64)
                mk_decay(Dfull, Dmask, lg, 512)
                pend = [None]
                for qb in range(8):
                    q0 = qb * 512
                    bo = 6 + (rqb % 2)
                    rqb += 1
                    nk = 4 * qb + 4
                    for kt in range(nk):
                        mdg = kt - 4 * qb
                        c0 = 128 * max(mdg, 0)
                        b = m.bank6()
                        m.mm(m.ps[:, b, c0:512], kh[:, kt * 128:(kt + 1) * 128], qh[:, q0 + c0:q0 + 512], True, True, r=[qkk], w=[('ps', b)])
                        pt = pts[cnt % 4]
                        ptk = ('pt', cnt % 4)
                        cnt += 1
                        if mdg >= 0:
                            m.stt(pt[:, c0:512], m.ps[:, b, c0:512], 0.125, Dmask[:, 0:512 - c0], ALU.mult, ALU.mult, r=[('ps', b), 'dmask'], w=[ptk])
                        else:
                            sc = 0.125 * math.exp(lg * (q0 - kt * 128))
                            m.stt(pt, m.ps[:, b, :], sc, Dfull, ALU.mult, ALU.mult, r=[('ps', b), 'dfull'], w=[ptk])

                        def back(pt=pt, ptk=ptk, kt=kt, mdg=mdg, bo=bo, qb=qb, last=(kt == nk - 1), h=h):
                            nonlocal ecnt
                            for qt in range(max(mdg, 0), 4):
                                m.mm(m.ps[:, bo, qt * 128:(qt + 1) * 128], pt[:, qt * 128:(qt + 1) * 128], V[:, kt, h * 128:(h + 1) * 128], (kt == 0 and qt == 0), False,
                                     r=[ptk, 'V'], w=[('ps', bo)])
                            if not last:
                                return
                            for qt in range(4):
                                tt_ = 4 * qb + qt
                                e_ = ecnt % 2
                                ecnt += 1
                                sm = small[:, 64 + e_ * 32:64 + (e_ + 1) * 32]
                                smk = ('sm', e_)
                                o_ = m.ps[:, bo, qt * 128:(qt + 1) * 128]
                                m.op('dve', lambda e, sm=sm, o_=o_: e.bn_stats(out=sm[:, 0:6], in_=o_), r=[('ps', bo)], w=[smk])
                                m.op('dve', lambda e, sm=sm: e.bn_aggr(out=sm[:, 6:8], in_=sm[:, 0:6]), r=[smk], w=[smk])
                                m.ts('dve', sm[:, 8:9], sm[:, 7:8], NORM_EPS, ALU.add, r=[smk], w=[smk])
                                m.act(sm[:, 9:10], sm[:, 8:9], AF.Sqrt, r=[smk], w=[smk])
                                m.op('dve', lambda e, sm=sm: e.reciprocal(out=sm[:, 10:11], in_=sm[:, 9:10]), r=[smk], w=[smk])
                                to = tmpo[e_]
                                tk = ('tmpo', e_)
                                m.ts('dve', to, o_, sm[:, 6:7], ALU.subtract, r=[('ps', bo), smk], w=[tk], s2=sm[:, 10:11], op1=ALU.mult)
                                m.tt('pool', to, to, gng[:, h * 128:(h + 1) * 128], ALU.mult, r=[tk, 'gng'], w=[tk])
                                m.tt('pool', to, to, gnb[:, h * 128:(h + 1) * 128], ALU.add, r=[tk, 'gnb'], w=[tk])
                                m.act(tmps[e_], G[:, tt_, h * 128:(h + 1) * 128], AF.Silu, r=['G'], w=[('tmps', e_)])
                                m.tt('pool', O_all[:, tt_, h * 128:(h + 1) * 128], to, tmps[e_], ALU.mult, r=[tk, ('tmps', e_)], w=[('O', tt_)])
                        prev = pend[0]
                        pend[0] = back
                        if prev is not None:
                            prev()
                pend[0]()
                pend[0] = None
            finalize_branch(1)

        if 'c' in MIX:
            m.aoff = mix_base
            X = m.allocb(NT * 512).rearrange("p (t n) -> p t n", n=512)
            Z = m.allocb(NT * 512).rearrange("p (t n) -> p t n", n=512)
            m.dma(X, tokm["sx"].rearrange("(t p) n -> p t n", p=128), w=['X'], slot='V')
            m.dma(Z, tokm["sz"].rearrange("(t p) n -> p t n", p=128), w=['Z'], slot='G')
            sdt = m.alloc(512)
            m.dma(sdt, sdtok, w=['sdt'], slot='misc0')
            dsk = m.alloc(8)
            m.dma(dsk, IN("d_skip")[l].partition_broadcast(128), w=['dsk'], slot='misc1')
            ngb = m.alloc(512)
            m.dma(ngb, IN("ssm_norm_g")[l].partition_broadcast(128), w=['ngb'], slot='misc2')
            BT = qk[0][0]
            CT = qk[0][1]
            Gbc = qkraw[:, 4096:8192]
            xdt = m.allocb(NT * 64).rearrange("p (t n) -> p t n", n=64)
            ss = m.alloc(NT * 8)
            rstd = m.alloc(NT * 2)
            Ls = [m.alloc(512) for _ in range(3)]
            wts = [m.allocb(512) for _ in range(4)]
            rqb = 0
            ytmp = [m.alloc(64) for _ in range(2)]
            ztmp = [m.alloc(64) for _ in range(2)]
            junk = m.alloc(64)
            sdt3 = sdt.rearrange("p (t e) -> p t e", e=16)
            cnt = 0
            ecnt = 0
            for g in range(2):
                m.dma(BT, qT["sBCT"][g * 128:(g + 1) * 128, :], w=['BT'], slot='misc3')
                m.dma(CT, qT["sBCT"][256 + g * 128:256 + (g + 1) * 128, :], w=['CT'], slot='misc4')
                for r_ in range(4):
                    h = 4 * g + r_
                    m.dma(Gbc, scumT[h:h + 1, :].partition_broadcast(128), w=['Gbc'], slot='misc5')
                    for tt_ in range(NT):
                        m.ts('pool', xdt[:, tt_, :], X[:, tt_, h * 64:(h + 1) * 64], sdt3[:, tt_, h:h + 1], ALU.mult, r=['X', 'sdt'], w=['xdt'])
                    pend = [None]
                    for tb in range(8):
                        t0 = tb * 512
                        by = 6 + (rqb % 2)
                        rqb += 1
                        ns_ = 4 * tb + 4
                        for st in range(ns_):
                            mdg = st - 4 * tb
                            c0 = 128 * max(mdg, 0)
                            b = m.bank6()
                            m.mm(m.ps[:, b, c0:512], BT[:, st * 128:(st + 1) * 128], CT[:, t0 + c0:t0 + 512], True, True, r=['BT', 'CT'], w=[('ps', b)])
                            L = Ls[cnt % 3]
                            lk = ('L', cnt % 3)
                            wt = wts[cnt % 4]
                            wk_ = ('wt', cnt % 4)
                            cnt += 1
                            m.act(L[:, c0:512], Gbc[:, t0 + c0:t0 + 512], AF.Exp, r=['Gbc', 'sdt'], w=[lk], bias=sdt3[:, st, 8 + h:9 + h])
                            if mdg >= 0:
                                diag_mask(L[:, c0:c0 + 128], r=[lk], w=[lk], strict=False)
                            m.tt('dve', wt[:, c0:512], L[:, c0:512], m.ps[:, b, c0:512], ALU.mult, r=[lk, ('ps', b)], w=[wk_])

                            def back(wt=wt, wk_=wk_, st=st, mdg=mdg, by=by, tb=tb, last=(st == ns_ - 1), h=h):
                                nonlocal ecnt
                                for qt in range(max(mdg, 0), 4):
                                    m.mm(m.ps[:, by, qt * 64:(qt + 1) * 64], wt[:, qt * 128:(qt + 1) * 128], xdt[:, st, :], (st == 0 and qt == 0), False,
                                         r=[wk_, 'xdt'], w=[('ps', by)])
                                if not last:
                                    return
                                for qt in range(4):
                                    tt_ = 4 * tb + qt
                                    e_ = ecnt % 2
                                    ecnt += 1
                                    yk = ('ytmp', e_)
                                    m.stt(ytmp[e_], X[:, tt_, h * 64:(h + 1) * 64], dsk[:, h:h + 1], m.ps[:, by, qt * 64:(qt + 1) * 64], ALU.mult, ALU.add,
                                          r=['X', 'dsk', ('ps', by)], w=[yk])
                                    m.act(ztmp[e_], Z[:, tt_, h * 64:(h + 1) * 64], AF.Silu, r=['Z'], w=[('ztmp', e_)])
                                    m.tt('dve', ytmp[e_], ytmp[e_], ztmp[e_], ALU.mult, r=[yk, ('ztmp', e_)], w=[yk])
                                    m.op('act', lambda e, e_=e_, tt_=tt_, h=h: e.activation(out=junk, in_=ytmp[e_], func=AF.Square, accum_out=ss[:, tt_ * 8 + h:tt_ * 8 + h + 1]),
                                         r=[yk], w=['junk', ('ss', tt_)])
                                    m.cp('pool', O_all[:, tt_, h * 64:(h + 1) * 64], ytmp[e_], r=[yk], w=[('O', tt_)])
                            prev = pend[0]
                            pend[0] = back
                            if prev is not None:
                                prev()
                    pend[0]()
                    pend[0] = None
                ssv = ss.rearrange("p (t e) -> p t e", e=8)[:, :, 4 * g:4 * g + 4]
                rg_ = rstd[:, g * NT:(g + 1) * NT]
                m.op('dve', lambda e, ssv=ssv, rg_=rg_: e.tensor_reduce(out=rg_, in_=ssv, axis=mybir.AxisListType.X, op=ALU.add),
                     r=[('ss', t) for t in range(NT)], w=[('rstd', g)])
                m.ts('dve', rg_, rg_, 1.0 / 256.0, ALU.mult, r=[('rstd', g)], w=[('rstd', g)], s2=LN_EPS, op1=ALU.add)
                m.act(rg_, rg_, AF.Sqrt, r=[('rstd', g)], w=[('rstd', g)])
                m.op('dve', lambda e, rg_=rg_: e.reciprocal(out=rg_, in_=rg_), r=[('rstd', g)], w=[('rstd', g)])
                for tt_ in range(NT):
                    o_ = O_all[:, tt_, g * 256:(g + 1) * 256]
                    m.stt(o_, o_, rg_[:, tt_:tt_ + 1], ngb[:, g * 256:(g + 1) * 256], ALU.mult, ALU.mult, r=[('O', tt_), ('rstd', g), 'ngb'], w=[('O', tt_)])
            finalize_branch(2)

        if 'd' in MIX:
            m.aoff = mix_base
            V = m.allocb(NT * 512).rearrange("p (t n) -> p t n", n=512)
            m.dma(V, tokm["bv"].rearrange("(t p) n -> p t n", p=128), w=['V'], slot='V')
            triU = m.allocb(128)
            triL = m.allocb(128)
            onesb = m.allocb(128)
            m.memset('pool', onesb, 1.0, w=['onesb'])
            m.asel(triU, onesb, [[-1, 128]], ALU.is_ge, 0.0, -1, 1, r=['onesb'], w=['triU'])
            m.asel(triL, onesb, [[1, 128]], ALU.is_ge, 0.0, 0, -1, r=['onesb'], w=['triL'])
            NS = 3
            es = [m.alloc(512) for _ in range(NS)]
            sps = [m.alloc(512) for _ in range(NS)]
            spbs = [m.allocb(512) for _ in range(NS)]
            ts_ = [m.alloc(512) for _ in range(NS)]
            wts = [m.allocb(512) for _ in range(NS)]
            BACC, BO = 6, 7
            zb = [0]

            def zbank():
                i = zb[0]
                zb[0] = (zb[0] + 1) % 6
                return i
            cnt = 0
            import os
            for h in range(int(os.environ.get('SB_H', 8))):
                qh, kh, qkk = load_qk("bqT", h * 64, "bkT", h * 64)
                for qb in range(int(os.environ.get('SB_QB', 8))):
                    q0 = qb * 512
                    kts = list(range(4 * qb + 3, -1, -1))
                    st_ = {}

                    def stageA(kt):
                        nonlocal cnt
                        i = cnt % NS
                        cnt += 1
                        mdg = kt - 4 * qb
                        c0 = 128 * max(mdg, 0)
                        b = zbank()
                        m.mm(m.ps[:, b, c0:512], kh[:, kt * 128:(kt + 1) * 128], qh[:, q0 + c0:q0 + 512], True, True, r=[qkk], w=[('ps', b)])
                        m.act(es[i][:, c0:512], m.ps[:, b, c0:512], AF.Exp, r=[('ps', b)], w=[('es', i)], scale=0.125)
                        m.act(sps[i][:, c0:512], es[i][:, c0:512], AF.Ln, r=[('es', i)], w=[('sps', i)], bias=1.0)
                        m.cp('dve', spbs[i][:, c0:512], sps[i][:, c0:512], r=[('sps', i)], w=[('spb', i)])
                        if mdg >= 0:
                            diag_mask(spbs[i][:, c0:c0 + 128], r=[('spb', i)], w=[('spb', i)], strict=True)
                        m.stt(ts_[i][:, c0:512], m.ps[:, b, c0:512], 0.125, sps[i][:, c0:512], ALU.mult, ALU.subtract, r=[('ps', b), ('sps', i)], w=[('ts', i)])
                        st_[kt] = (i, c0, mdg)

                    def stageB(kt, first):
                        i, c0, mdg = st_[kt]
                        m.mm(m.ps[:, BACC, c0:512], triU, spbs[i][:, c0:512], first, False, r=['triU', ('spb', i)], w=[('ps', BACC)])
                        m.tt('dve', ts_[i][:, c0:512], ts_[i][:, c0:512], m.ps[:, BACC, c0:512], ALU.subtract, r=[('ts', i), ('ps', BACC)], w=[('ts', i)])
                        m.mm(m.ps[:, BACC, c0:512], triL, spbs[i][:, c0:512], False, False, r=['triL', ('spb', i)], w=[('ps', BACC)])
                        m.act(wts[i][:, c0:512], ts_[i][:, c0:512], AF.Exp, r=[('ts', i)], w=[('wt', i)])
                        if mdg >= 0:
                            diag_mask(wts[i][:, c0:c0 + 128], r=[('wt', i)], w=[('wt', i)], strict=True)

                    def stageC(kt, first):
                        i, c0, mdg = st_[kt]
                        for qt in range(max(mdg, 0), 4):
                            m.mm(m.ps[:, BO, qt * 64:(qt + 1) * 64], wts[i][:, qt * 128:(qt + 1) * 128], V[:, kt, h * 64:(h + 1) * 64], first and qt == max(mdg, 0), False,
                                 r=[('wt', i), 'V'], w=[('ps', BO)])
                    n_ = len(kts)
                    stageA(kts[0])
                    for i_ in range(n_):
                        if i_ + 1 < n_:
                            stageA(kts[i_ + 1])
                        stageB(kts[i_], i_ == 0)
                        if i_ >= 1:
                            stageC(kts[i_ - 1], i_ - 1 == 0)
                    stageC(kts[n_ - 1], n_ - 1 == 0)
                    for qt in range(4):
                        tt_ = 4 * qb + qt
                        m.cp('dve', O_all[:, tt_, h * 64:(h + 1) * 64], m.ps[:, BO, qt * 64:(qt + 1) * 64], r=[('ps', BO)], w=[('O', tt_)])
            finalize_branch(3)
        m.barrier()
        if stop_after == ('mix', l):
            break

        m.aoff = persist_end
        hT = _r3(m.allocb(8 * S), 8)
        p1_end = m.aoff
        xts = [m.alloc(D) for _ in range(3)]
        make_hT(hT, x_src, scp[:, 0:8], modT[:, 0:8], range(NT))
        m.barrier()
        m.aoff = p1_end
        wst4 = [m.alloc(8 * 512)] * 2
        wg4 = [m.allocb(8 * 512) for _ in range(2)]
        wb4 = [m.allocb(4 * 512) for _ in range(2)]
        obs = [m.allocb(16 * 512) for _ in range(2)]
        rowbs = [m.allocb(S) for _ in range(2)]
        sigs = [m.alloc(512) for _ in range(2)]
        accs4 = [m.alloc(512) for _ in range(2)]
        tmps4 = [m.alloc(512) for _ in range(2)]
        oc = 0
        sc_ = 0
        for dc in range(8):
            i = dc % 2
            st_g = wst4[0].rearrange("p (c n j) -> p c n j", c=8, n=4)
            for n in range(4):
                c0 = 6152 + n * 1024 + dc * 128
                m.dma(st_g[:, :, n, :], IN("w_in")[l][:, c0:c0 + 128].rearrange("(c p) j -> p c j", p=128), w=[('wst4', 0)], r=[('wst4', 0)], slot=('wst4', 0, n))
            wg = wg4[i].rearrange("p (c n j) -> p c n j", c=8, n=4)
            m.cp('pool', wg4[i], wst4[i], r=[('wst4', 0)], w=[('wg4', i)])
            st_b = wst4[i][:, 0:2048].rearrange("p (n c j) -> p n c j", n=4, c=4)
            for n in range(4):
                m.dma(st_b[:, n, :, :], IN("w_br")[l][n][:, dc * 128:(dc + 1) * 128].rearrange("(c p) j -> p c j", p=128), w=[('wst4', 0)], r=[('wst4', 0)], slot=('wst4', 0, n))
            wbr = wb4[i].rearrange("p (n c j) -> p n c j", n=4, c=4)
            m.cp('pool', wb4[i], wst4[i][:, 0:2048], r=[('wst4', 0)], w=[('wb4', i)])
            rb = rowbs[dc % 2]
            rk = ('rowb4', dc % 2)
            for tb in range(8):
                o_i = oc % 2
                oc += 1
                ob = obs[o_i].rearrange("p (n c t) -> p n c t", n=4, c=4)
                for n in range(4):
                    m.dma(ob[:, n, :, :], oT[n][:, tb * 512:(tb + 1) * 512].rearrange("(c p) t -> p c t", p=128), w=[('ob', o_i, n)], slot=('ob', o_i, n))
                a_i = tb % 2
                for n in range(4):
                    bg = m.bank()
                    for k in range(8):
                        m.mm(m.ps[:, bg, :], wg[:, k, n, :], hT[:, k, tb * 512:(tb + 1) * 512], k == 0, k == 7, r=[('wg4', i)] + HK[tb * 4:tb * 4 + 4], w=[('ps', bg)])
                    by = m.bank()
                    for c in range(4):
                        m.mm(m.ps[:, by, :], wbr[:, n, c, :], ob[:, n, c, :], c == 0, c == 3, r=[('wb4', i), ('ob', o_i, n)], w=[('ps', by)])
                    s_i = sc_ % 2
                    sc_ += 1
                    m.act(sigs[s_i], m.ps[:, bg, :], AF.Sigmoid, r=[('ps', bg)], w=[('sig', s_i)])
                    if n == 0:
                        m.tt('dve', accs4[a_i], sigs[s_i], m.ps[:, by, :], ALU.mult, r=[('sig', s_i), ('ps', by)], w=[('acc4', a_i)])
                    else:
                        m.tt('dve', tmps4[s_i], sigs[s_i], m.ps[:, by, :], ALU.mult, r=[('sig', s_i), ('ps', by)], w=[('tmp4', s_i)])
                        if n < 3:
                            m.tt('pool', accs4[a_i], accs4[a_i], tmps4[s_i], ALU.add, r=[('acc4', a_i), ('tmp4', s_i)], w=[('acc4', a_i)])
                        else:
                            m.tt('pool', rb[:, tb * 512:(tb + 1) * 512], accs4[a_i], tmps4[s_i], ALU.add, r=[('acc4', a_i), ('tmp4', s_i)], w=[rk])
            m.dma(mergedT[dc * 128:(dc + 1) * 128, :], rb, r=[rk], slot=rk)
        m.barrier()
        if stop_after == ('merge', l):
            break

        m.aoff = persist_end
        m.init_wbufs(2, 8 * 512)
        wo = [m.load_w(IN("w_out")[l][:, hh * 512:(hh + 1) * 512], 8, 512) for hh in range(2)]
        mts = [m.allocb(8 * 128) for _ in range(2)]
        xts = [m.alloc(D) for _ in range(3)]
        tmp5 = [m.alloc(D) for _ in range(2)]
        out5 = [m.alloc(D) for _ in range(2)]
        for tt_ in range(NT):
            i = tt_ % 2
            mt = _r3(mts[i], 8)
            m.dma(mt, mergedT[:, tt_ * 128:(tt_ + 1) * 128].rearrange("(c p) t -> p c t", p=128), w=[('mt', i)], slot=('mt', i))
            xs = tt_ % 3
            m.dma(xts[xs], x_src[tt_ * 128:(tt_ + 1) * 128, :], w=[('xt', xs)], slot=('xt', xs))
            b2 = m.bank2()
            for hh in range(2):
                for k in range(8):
                    m.mm(m.ps[:, b2 + hh, :], mt[:, k, :], wo[hh][0][:, k, :], k == 0, k == 7, r=[('mt', i), wo[hh][1]], w=[('ps', b2 + hh)])
            ps2 = (m.ps[:, b2:b2 + 2, :].rearrange("p a n -> p (a n)"), [('ps', b2), ('ps', b2 + 1)])
            ln_epilogue(ps2, xts[xs], ('xt', xs), gbc[:, 0:D], lnbc[:, 0:D], lnbc[:, D:2 * D], out5[i], ('out5', i), tmp5[i], ('tmp5', i), i)
            m.dma(x1_d[tt_ * 128:(tt_ + 1) * 128, :], out5[i], r=[('out5', i)], slot=('out5', i))
        m.barrier()
        if stop_after == ('ln1', l):
            break

        m.aoff = persist_end
        moe = (l % 2 == 1)
        TBK = 1024
        h2T = _r3(m.allocb(8 * TBK), 8)
        yacc = _r3(m.alloc(8 * TBK), 8)
        aTs = [m.allocb(4 * TBK).rearrange("p (c t) -> p c t", c=4) for _ in range(2)]
        wst6 = [m.alloc(8 * 512) for _ in range(2)]
        wbf6 = [m.allocb(8 * 512) for _ in range(4)]
        xts = [m.alloc(D) for _ in range(2)]
        tmp6 = [m.alloc(D) for _ in range(1)]
        out6 = [m.alloc(D) for _ in range(2)]
        sil = [m.alloc(512) for _ in range(2)]
        tmy = [m.alloc(512) for _ in range(2)]
        wsi = [0]
        wbi = [0]

        def load6(src_r3, kc, n):
            si = wsi[0] % 2
            wsi[0] += 1
            bi = wbi[0] % 4
            wbi[0] += 1
            st = _r3(wst6[si][:, 0:kc * n], kc)
            m.dma(st, src_r3, w=[('wst6', si)], slot=('wst6', si))
            bf = _r3(wbf6[bi][:, 0:kc * n], kc)
            m.cp('act', bf, st, r=[('wst6', si)], w=[('wbf6', bi)])
            return bf, ('wbf6', bi)
        if moe:
            rw = m.alloc(64)
            rbb = m.alloc(8)
            selE = m.alloc(8 * 128)
            combT = m.alloc(TBK)
            cbt_ = m.alloc(TBK)
            hf = [m.alloc(8 * 128)] * 2
            rs = m.alloc(64)
            cpad = m.alloc(128)
            m.memset('pool', cpad, 0.0, w=['cpad'])
            m.dma(_r3(rw, 8), IN("router_w")[0].rearrange("(c p) e -> p c e", p=128), w=['rw'], slot='misc0')
            m.dma(rbb, IN("router_b")[0].partition_broadcast(128), w=['rbb'], slot='misc1')
            m.memset('pool', selE[0:8, :], 1.0, w=['selE'])
            for e_ in range(8):
                m.ts('dve', selE[0:8, e_ * 128:(e_ + 1) * 128], selE[0:8, e_ * 128:(e_ + 1) * 128], ident[0:8, e_:e_ + 1], ALU.mult, r=['selE', 'ident'], w=['selE'])
            experts = [(IN("expert_w_gu")[0][e], IN("expert_w_down")[0][e], D_FFE, e) for e in range(8)]
        else:
            experts = [(IN("ffn_w_gu")[l // 2], IN("ffn_w_down")[l // 2], D_FF, None)]
        for tbk in range(S // TBK):
            tts = list(range(tbk * 8, tbk * 8 + 8))
            for tt_ in tts:
                s_ = tt_ % 2
                m.dma(xts[s_], x1_d[tt_ * 128:(tt_ + 1) * 128, :], w=[('xt', s_)], slot=('xt', s_))
                b2 = m.bank2()
                for c in range(8):
                    bb, cc = b2 + c // 4, (c % 4) * 128
                    m.tr(m.ps[:, bb, cc:cc + 128], xts[s_][:, c * 128:(c + 1) * 128], ident, r=[('xt', s_), 'ident'], w=[('ps', bb)])
                o = (tt_ - tbk * 8) * 128
                import os
                MA = int(os.environ.get('MOE_A', 9))
                if not moe:
                    for c in range(8):
                        bb, cc = b2 + c // 4, (c % 4) * 128
                        m.act(h2T[:, c, o:o + 128], m.ps[:, bb, cc:cc + 128], AF.Identity, r=[('ps', bb), 'scp', 'modT'],
                              w=[('h2T', tt_ % 8)], scale=scp[:, 8 + c:9 + c], bias=modT[:, 24 + c:25 + c])
                else:
                    hfi = hf[0]
                    hk = ('hf', 0)
                    for c in range(8):
                        bb, cc = b2 + c // 4, (c % 4) * 128
                        m.act(hfi[:, c * 128:(c + 1) * 128], m.ps[:, bb, cc:cc + 128], AF.Identity, r=[('ps', bb), 'scp', 'modT'],
                              w=[hk], scale=scp[:, 8 + c:9 + c], bias=modT[:, 24 + c:25 + c])
                    m.cp('pool', h2T[:, :, o:o + 128], _r3(hfi, 8), r=[hk], w=[('h2T', tt_ % 8)])
                if moe and MA >= 1:
                    bl = m.bank()
                    for c in range(8):
                        m.mm(m.ps[:, bl, 0:8], hfi[:, c * 128:(c + 1) * 128], rw[:, c * 8:(c + 1) * 8], c == 0, c == 7, r=[hk, 'rw'], w=[('ps', bl)])
                    m.tt('dve', rs[:, 0:8], m.ps[:, bl, 0:8], rbb, ALU.add, r=[('ps', bl), 'rbb'], w=['rs'])
                if moe and MA >= 2:
                    m.op('dve', lambda e: e.max(out=rs[:, 8:16], in_=rs[:, 0:8]), r=['rs'], w=['rs'])
                    m.ts('dve', rs[:, 16:24], rs[:, 0:8], rs[:, 9:10], ALU.is_ge, r=['rs'], w=['rs'])
                    m.ts('dve', rs[:, 24:25], rs[:, 8:9], -1.0, ALU.mult, r=['rs'], w=['rs'])
                    m.act(rs[:, 32:40], rs[:, 0:8], AF.Exp, r=['rs'], w=['rs'], bias=rs[:, 24:25])
                    m.act(rs[:, 25:26], rs[:, 9:10], AF.Exp, r=['rs'], w=['rs'], bias=rs[:, 24:25])
                    m.ts('dve', rs[:, 25:26], rs[:, 25:26], 1.0, ALU.add, r=['rs'], w=['rs'])
                    m.op('dve', lambda e: e.reciprocal(out=rs[:, 26:27], in_=rs[:, 25:26]), r=['rs'], w=['rs'])
                    m.tt('dve', rs[:, 40:48], rs[:, 32:40], rs[:, 16:24], ALU.mult, r=['rs'], w=['rs'])
                    m.ts('dve', rs[:, 40:48], rs[:, 40:48], rs[:, 26:27], ALU.mult, r=['rs'], w=['rs'])
                if moe and MA >= 3:
                    bt = m.bank()
                    m.cp('dve', cpad[:, 0:8], rs[:, 40:48], r=['rs'], w=['cpad'])
                    m.tr(m.ps[:, bt, 0:128], cpad, ident, r=['cpad', 'ident'], w=[('ps', bt)])
                    m.cp('dve', combT[0:8, o:o + 128], m.ps[0:8, bt, 0:128], r=[('ps', bt)], w=['combT'])
            import os
            MD = os.environ.get('MOE_DBG', 'ABC')
            m.memset('pool', yacc, 0.0, w=['yacc'])
            H2K = [('h2T', i_) for i_ in range(8)]
            gi = 0
            for (wgu, wdn, F, e) in (experts if 'B' in MD else []):
                if e is not None:
                    for half in range(2):
                        bc_ = m.bank()
                        m.mm(m.ps[:, bc_, :], selE[0:8, e * 128:(e + 1) * 128], combT[0:8, half * 512:(half + 1) * 512], True, True, r=['selE', 'combT'], w=[('ps', bc_)])
                        m.cp('act', cbt_[:, half * 512:(half + 1) * 512], m.ps[:, bc_, :], r=[('ps', bc_)], w=['cbt_'])
                nfc = F // 128
                for f0 in range(0, nfc, 4):
                    ncq = min(4, nfc - f0)
                    ncol = ncq * 128
                    gw, gk = load6(wgu[:, f0 * 128:f0 * 128 + ncol].rearrange("(c p) n -> p c n", p=128), 8, ncol)
                    uw, uk = load6(wgu[:, F + f0 * 128:F + f0 * 128 + ncol].rearrange("(c p) n -> p c n", p=128), 8, ncol)
                    dw, dk = load6(wdn[f0 * 128:f0 * 128 + ncol, :].rearrange("(c p) n -> p c n", p=128), ncq, D)
                    aT = aTs[gi % 2]
                    ak = ('aT', gi % 2)
                    gi += 1
                    for fc in range(ncq):
                        for half in range(2):
                            pg = m.bank()
                            for k in range(8):
                                m.mm(m.ps[:, pg, :], gw[:, k, fc * 128:(fc + 1) * 128], h2T[:, k, half * 512:(half + 1) * 512], k == 0, k == 7,
                                     r=[gk] + H2K[half * 4:half * 4 + 4], w=[('ps', pg)])
                            pu = m.bank()
                            for k in range(8):
                                m.mm(m.ps[:, pu, :], uw[:, k, fc * 128:(fc + 1) * 128], h2T[:, k, half * 512:(half + 1) * 512], k == 0, k == 7,
                                     r=[uk] + H2K[half * 4:half * 4 + 4], w=[('ps', pu)])
                            s_i = (fc * 2 + half) % 2
                            m.act(sil[s_i], m.ps[:, pg, :], AF.Silu, r=[('ps', pg)], w=[('sil', s_i)])
                            m.tt('dve', aT[:, fc, half * 512:(half + 1) * 512], sil[s_i], m.ps[:, pu, :], ALU.mult, r=[('sil', s_i), ('ps', pu)], w=[ak])
                    for dc in range(8):
                        for half in range(2):
                            py = m.bank()
                            for fc in range(ncq):
                                m.mm(m.ps[:, py, :], dw[:, fc, dc * 128:(dc + 1) * 128], aT[:, fc, half * 512:(half + 1) * 512], fc == 0, fc == ncq - 1,
                                     r=[dk, ak], w=[('ps', py)])
                            ya = yacc[:, dc, half * 512:(half + 1) * 512]
                            yk = ('yacc', dc, half)
                            if e is not None:
                                t_i = (dc * 2 + half) % 2
                                m.tt('dve', tmy[t_i], m.ps[:, py, :], cbt_[:, half * 512:(half + 1) * 512], ALU.mult, r=[('ps', py), 'cbt_'], w=[('tmy', t_i)])
                                m.tt('dve', ya, ya, tmy[t_i], ALU.add, r=['yacc', yk, ('tmy', t_i)], w=[yk])
                            else:
                                m.tt('dve', ya, ya, m.ps[:, py, :], ALU.add, r=['yacc', yk, ('ps', py)], w=[yk])
            YK = [('yacc', dc, half) for dc in range(8) for half in range(2)] + ['yacc']
            for tt_ in (tts if 'C' in MD else []):
                s_ = tt_ % 2
                o = (tt_ - tbk * 8) * 128
                m.dma(xts[s_], x1_d[tt_ * 128:(tt_ + 1) * 128, :], w=[('xt', s_)], slot=('xt', s_))
                b2 = m.bank2()
                for c in range(8):
                    bb, cc = b2 + c // 4, (c % 4) * 128
                    m.tr(m.ps[:, bb, cc:cc + 128], yacc[:, c, o:o + 128], ident, r=YK + ['ident'], w=[('ps', bb)])
                ps2 = (m.ps[:, b2:b2 + 2, :].rearrange("p a n -> p (a n)"), [('ps', b2), ('ps', b2 + 1)])
                ln_epilogue(ps2, xts[s_], ('xt', s_), gbc[:, D:2 * D], lnbc[:, 2 * D:3 * D], lnbc[:, 3 * D:4 * D], out6[s_], ('out6', s_), tmp6[0], ('tmp6', 0), s_)
                m.dma(x_dst[tt_ * 128:(tt_ + 1) * 128, :], out6[s_], r=[('out6', s_)], slot=('out6', s_))
        m.barrier()
        if stop_after == ('ffn', l):
            break
    m.barrier()
    nc_ = m.finish()
    nc_.used_inputs = list(_ins.keys())
    return nc_


_CACHE = {}


def _prep_inputs(inputs, b):
    f = lambda a: np.ascontiguousarray(a, dtype=np.float32)
    R = {
        "x": lambda a: a[b],
        "c": lambda a: np.asarray(a[b]).reshape(8, 128).T,
        "b_ada": lambda a: np.asarray(a).reshape(2, 1, 6 * D),
        "conv_w": lambda a: np.asarray(a).reshape(2, 4, 8, 128).transpose(0, 3, 2, 1),
        "conv_b": lambda a: np.asarray(a).reshape(2, 8, 128).transpose(0, 2, 1),
        "dt_bias": lambda a: np.asarray(a).reshape(2, 8, 1),
        "a_log": lambda a: np.asarray(a).reshape(2, 8, 1),
        "d_skip": lambda a: np.asarray(a).reshape(2, 1, 8),
        "ssm_norm_g": lambda a: np.asarray(a).reshape(2, 1, 512),
        "ret_gn_g": lambda a: np.asarray(a).reshape(2, 1, 512),
        "ret_gn_b": lambda a: np.asarray(a).reshape(2, 1, 512),
        "ln1_g": lambda a: np.asarray(a).reshape(2, 1, D),
        "ln1_b": lambda a: np.asarray(a).reshape(2, 1, D),
        "ln2_g": lambda a: np.asarray(a).reshape(2, 1, D),
        "ln2_b": lambda a: np.asarray(a).reshape(2, 1, D),
        "router_b": lambda a: np.asarray(a).reshape(1, 1, 8),
    }
    return {k: f(R[k](v) if k in R else v) for k, v in inputs.items()}


def kernel(**inputs):
    nc = build()
    shared = _prep_inputs(inputs, 0)
    in_maps = []
    for b in range(8):
        d = dict(shared)
        d["x"] = np.ascontiguousarray(inputs["x"][b], dtype=np.float32)
        d["c"] = np.ascontiguousarray(np.asarray(inputs["c"][b]).reshape(8, 128).T, dtype=np.float32)
        in_maps.append(d)
    res = run_bass_kernel_spmd(nc, in_maps, core_ids=list(range(8)))
    return np.stack([np.asarray(r["y"], dtype=np.float32) for r in res.results], axis=0)
```

```python
import contextlib
import math
import numpy as np
import concourse.bass as bass
import concourse.mybir as mybir
from concourse.bass_utils import run_bass_kernel_spmd

F32 = mybir.dt.float32
BF16 = mybir.dt.bfloat16
AF = mybir.ActivationFunctionType
ALU = mybir.AluOpType

S = 4096
D = 1024
NT = 32
IN_W = 10248
ALPHA = 4.0 ** 0.25
LN_EPS = 1e-5
NORM_EPS = 1e-6
D_FF = 2816
D_FFE = 3584


class Builder:
    ENGS = ('pe', 'act', 'dve', 'pool', 'sp')

    def __init__(self):
        self.nc = bass.Bass("TRN2", target_bir_lowering=False)
        self.stack = contextlib.ExitStack()
        self.q = {e: [] for e in self.ENGS}
        self.seq = {e: 0 for e in self.ENGS}
        self.known = {e: {} for e in self.ENGS}
        self.last_w = {}
        self.readers = {}
        self.slots = {}
        self.sems = {}
        for e in self.ENGS:
            self.sems[('e', e)] = self.stack.enter_context(self.nc.semaphore("c_" + e))
        self._uid = 0

    def sbuf(self, shape, dtype, name=None):
        self._uid += 1
        return self.stack.enter_context(self.nc.sbuf_tensor(name or f"sb{self._uid}", list(shape), dtype))

    def psum(self, shape, dtype, name=None):
        self._uid += 1
        return self.stack.enter_context(self.nc.psum_tensor(name or f"ps{self._uid}", list(shape), dtype))

    def dram(self, name, shape, dtype, kind="Internal"):
        return self.nc.dram_tensor(name, list(shape), dtype, kind=kind).ap()

    def _deps(self, eng, r, w, skip_same_w=False):
        deps = {}

        def add(tok):
            s, v = tok
            if deps.get(s, 0) < v:
                deps[s] = v
        for k in r:
            t = self.last_w.get(k)
            if t is not None:
                add(t)
        for k in w:
            t = self.last_w.get(k)
            if t is not None and not (skip_same_w and t[0] == ('e', eng)):
                add(t)
            for t in self.readers.get(k, ()):
                add(t)
        waits = []
        kn = self.known[eng]
        for s, v in deps.items():
            if kn.get(s, 0) >= v:
                continue
            kn[s] = v
            waits.append((s, v))
        return waits

    def _record(self, tok, r, w):
        for k in w:
            self.last_w[k] = tok
            self.readers[k] = []
        for k in r:
            self.readers.setdefault(k, []).append(tok)

    def op(self, eng, fn, r=(), w=()):
        waits = self._deps(eng, r, w, skip_same_w=(eng == 'pe'))
        self.seq[eng] += 1
        tok = (('e', eng), self.seq[eng])
        self.q[eng].append((fn, waits, (('e', eng), 1)))
        self._record(tok, r, w)
        return tok

    def dma(self, out, in_, r=(), w=(), slot=None, q='sp'):
        if slot not in self.slots:
            self.sems[('d', slot)] = self.stack.enter_context(self.nc.semaphore("d%d" % len(self.slots)))
            self.slots[slot] = [('d', slot), 0]
        waits = self._deps(q, r, w)
        ent = self.slots[slot]
        ent[1] += 16
        tok = (ent[0], ent[1])
        self.q[q].append((lambda e, o=out, i=in_: e.dma_start(out=o, in_=i), waits, (ent[0], 16)))
        self._record(tok, r, w)
        return tok

    def barrier(self):
        toks = []
        for e in self.ENGS:
            if self.seq[e] > 0:
                toks.append((('e', e), self.seq[e]))
        for k, ent in self.slots.items():
            if ent[1] > 0:
                toks.append((ent[0], ent[1]))
        for e in self.ENGS:
            kn = self.known[e]
            waits = []
            for s, v in toks:
                if kn.get(s, 0) >= v:
                    continue
                kn[s] = v
                waits.append((s, v))
            if waits:
                self.q[e].append((None, waits, None))
        self.last_w.clear()
        self.readers.clear()

    def finish(self):
        self.barrier()
        nc = self.nc
        sems = self.sems

        def run(eng_obj, lst):
            for fn, waits, inc in lst:
                for s, v in waits:
                    eng_obj.wait_ge(sems[s], v)
                if fn is not None:
                    fn(eng_obj).then_inc(sems[inc[0]], inc[1])
        with nc.Block() as block:
            @block.tensor
            def _(e):
                run(e, self.q['pe'])

            @block.scalar
            def _(e):
                run(e, self.q['act'])

            @block.vector
            def _(e):
                run(e, self.q['dve'])

            @block.gpsimd
            def _(e):
                run(e, self.q['pool'])

            @block.sync
            def _(e):
                run(e, self.q['sp'])
        self.stack.close()
        return nc


def _r3(ap, c):
    return ap.rearrange("p (c n) -> p c n", c=c)


class MK(Builder):
    AW = 50 * 1024

    def __init__(self, dbg=()):
        super().__init__()
        self.dbg = set(dbg)
        self.arena = self.sbuf([128, self.AW], F32, "arena")
        self.aoff = 0
        self.ps = self.psum([128, 8, 512], F32, "psum")
        self.pcur = 0
        self.ktag = 0

    def alloc(self, words):
        a = self.aoff
        self.aoff += words
        assert self.aoff <= self.AW, (self.aoff, self.AW)
        return self.arena[:, a:a + words]

    def allocb(self, n):
        assert n % 2 == 0
        return self.alloc(n // 2).bitcast(BF16)

    def key(self, name):
        self.ktag += 1
        return (name, self.ktag)

    def bank(self):
        i = self.pcur
        self.pcur = (self.pcur + 1) % 8
        return i

    def bank6(self):
        i = getattr(self, 'p6', 0)
        self.p6 = (i + 1) % 6
        return i

    def bank2(self):
        if self.pcur % 2:
            self.pcur = (self.pcur + 1) % 8
        i = self.pcur
        self.pcur = (self.pcur + 2) % 8
        return i

    def scr(self, name, shape, dtype):
        return self.dram(name, shape, dtype, kind="ExternalOutput" if name in self.dbg else "Internal")

    def mm(self, out, lhsT, rhs, start, stop, r, w):
        self.op('pe', lambda e: e.matmul(out, lhsT=lhsT, rhs=rhs, start=start, stop=stop), r=r, w=w)

    def tr(self, out, in_, ident, r, w):
        self.op('pe', lambda e: e.transpose(out=out, in_=in_, identity=ident), r=r, w=w)

    def act(self, out, in_, func, r, w, bias=None, scale=None):
        kw = {}
        if bias is not None:
            kw['bias'] = bias
        if scale is not None:
            kw['scale'] = scale
        self.op('act', lambda e: e.activation(out=out, in_=in_, func=func, **kw), r=r, w=w)

    def tt(self, eng, out, in0, in1, op, r, w):
        self.op(eng, lambda e: e.tensor_tensor(out=out, in0=in0, in1=in1, op=op), r=r, w=w)

    def ts(self, eng, out, in0, s1, op0, r, w, s2=None, op1=None):
        if op1 is None:
            self.op(eng, lambda e: e.tensor_scalar(out=out, in0=in0, scalar1=s1, scalar2=None, op0=op0), r=r, w=w)
        else:
            self.op(eng, lambda e: e.tensor_scalar(out=out, in0=in0, scalar1=s1, scalar2=s2, op0=op0, op1=op1), r=r, w=w)

    def stt(self, out, in0, scalar, in1, op0, op1, r, w):
        self.op('dve', lambda e: e.scalar_tensor_tensor(out=out, in0=in0, scalar=scalar, in1=in1, op0=op0, op1=op1), r=r, w=w)

    def cp(self, eng, out, in_, r, w):
        if eng == 'act':
            self.op('act', lambda e: e.copy(out=out, in_=in_), r=r, w=w)
        else:
            self.op(eng, lambda e: e.tensor_copy(out=out, in_=in_), r=r, w=w)

    def memset(self, eng, ap, val, w):
        self.op(eng, lambda e: e.memset(ap, val), w=w)

    def asel(self, out, in_, pattern, cmp, fill, base, cm, r, w):
        self.op('pool', lambda e: e.affine_select(out=out, in_=in_, pattern=pattern, compare_op=cmp, fill=fill,
                                                  base=base, channel_multiplier=cm), r=r, w=w)

    def init_wbufs(self, nslots, words):
        self.wst = [self.alloc(words) for _ in range(nslots)]
        self.wbf = [self.alloc(words // 2).bitcast(BF16) for _ in range(nslots)]
        self.wi = 0
        self.wn = nslots

    def load_w(self, src, kc, n, cast=True):
        i = self.wi
        self.wi = (self.wi + 1) % self.wn
        st = _r3(self.wst[i][:, 0:kc * n], kc)
        self.dma(st, src.rearrange("(c p) n -> p c n", p=128), w=[('wst', i)], slot=('wst', i))
        if not cast:
            return st, ('wst', i)
        bf = _r3(self.wbf[i][:, 0:kc * n], kc)
        self.cp('pool', bf, st, r=[('wst', i)], w=[('wbf', i)])
        return bf, ('wbf', i)


def build(dbg=(), stop_after=None, layers=(0, 1), MIX='abcd', skip=(), x_first=False):
    m = MK(dbg)
    nc = m.nc

    SHAPES = dict(x=[S, D], c=[128, 8], w_ada=[2, D, 6 * D], b_ada=[2, 1, 6 * D], w_in=[2, D, IN_W],
                  conv_w=[2, 128, 8, 4], conv_b=[2, 128, 8], dt_bias=[2, 8, 1], a_log=[2, 8, 1], d_skip=[2, 1, 8],
                  ssm_norm_g=[2, 1, 512], ret_gn_g=[2, 1, 512], ret_gn_b=[2, 1, 512], w_br=[2, 4, 512, D],
                  w_out=[2, D, D], ln1_g=[2, 1, D], ln1_b=[2, 1, D], ln2_g=[2, 1, D], ln2_b=[2, 1, D],
                  ffn_w_gu=[1, D, 2 * D_FF], ffn_w_down=[1, D_FF, D], router_w=[1, D, 8], router_b=[1, 1, 8],
                  expert_w_gu=[1, 8, D, 2 * D_FFE], expert_w_down=[1, 8, D_FFE, D])
    _ins = {}

    def IN(name):
        if name not in _ins:
            _ins[name] = m.dram(name, SHAPES[name], F32, kind="ExternalInput")
        return _ins[name]
    m.used_inputs = _ins
    y_out = m.dram("y", [S, D], F32, kind="ExternalOutput")

    qT = {n: m.scr(n, [512, S], BF16) for n in ("mqT", "mkT", "rqkT", "bqT", "bkT", "sBCT")}
    mv65 = m.scr("mv65", [S, 520], BF16)
    tokm = {n: m.scr(n, [S, 512], BF16) for n in ("rv", "rg", "sz", "bv", "sx")}
    scumT = m.scr("scumT", [8, S], F32)
    sdtok = m.scr("sdtok", [128, NT * 16], F32)
    oT = m.scr("oT", [4, 512, S], BF16)
    mergedT = m.scr("mergedT", [D, S], BF16)
    x1_d = m.scr("x1", [S, D], F32)
    x2_d = m.scr("x2", [S, D], F32)

    ident = m.alloc(128)
    identb = m.allocb(128)
    ones_row = m.alloc(128)
    one11 = ones_row[0:1, 0:1]
    modT = m.alloc(48)
    scp = m.alloc(16)
    gbc = m.alloc(2 * D)
    lnbc = m.alloc(4 * D)
    small = m.alloc(256)
    persist_end = m.aoff

    m.memset('pool', ident, 1.0, w=['ident'])
    m.asel(ident, ident, [[-1, 128]], ALU.is_equal, 0.0, 0, 1, r=['ident'], w=['ident'])
    m.cp('pool', identb, ident, r=['ident'], w=['identb'])
    m.memset('pool', ones_row, 1.0, w=['ones'])
    m.barrier()

    def ln_epilogue(ps2, xt, xkey, g_ap, lg, lb, out_t, okey, tmp, tkey, st):
        pk, stk = ps2[1], ('st', st)
        m.tt('dve', tmp, ps2[0], g_ap, ALU.mult, r=list(pk) + ['gbc'], w=[tkey])
        m.stt(tmp, xt, ALPHA, tmp, ALU.mult, ALU.add, r=[xkey, tkey], w=[tkey])
        sm = small[:, st * 32:(st + 1) * 32]
        m.op('dve', lambda e: e.bn_stats(out=sm[:, 0:6], in_=tmp[:, 0:512]), r=[tkey], w=[stk])
        m.op('dve', lambda e: e.bn_stats(out=sm[:, 6:12], in_=tmp[:, 512:1024]), r=[tkey], w=[stk])
        m.op('dve', lambda e: e.bn_aggr(out=sm[:, 12:14], in_=sm[:, 0:12]), r=[stk], w=[stk])
        m.ts('dve', sm[:, 14:15], sm[:, 13:14], LN_EPS, ALU.add, r=[stk], w=[stk])
        m.act(sm[:, 15:16], sm[:, 14:15], AF.Sqrt, r=[stk], w=[stk])
        m.op('dve', lambda e: e.reciprocal(out=sm[:, 16:17], in_=sm[:, 15:16]), r=[stk], w=[stk])
        m.ts('dve', tmp, tmp, sm[:, 12:13], ALU.subtract, r=[tkey, stk], w=[tkey], s2=sm[:, 16:17], op1=ALU.mult)
        m.tt('pool', tmp, tmp, lg, ALU.mult, r=[tkey, 'lnbc'], w=[tkey])
        m.tt('pool', out_t, tmp, lb, ALU.add, r=[tkey, 'lnbc'], w=[okey])

    for l in layers:
        x_src = IN("x") if (l == 0 or x_first) else x2_d
        x_dst = x2_d if l == 0 else y_out
        m.aoff = persist_end
        cT = m.alloc(8)
        cact = m.alloc(8)
        modrow = m.alloc(6 * D)
        brow = m.alloc(6 * D)
        m.init_wbufs(2, 8 * 512)
        m.dma(cT, IN("c"), w=['cT'], slot='misc0')
        m.dma(brow[0:1, :], IN("b_ada")[l], w=['brow'], slot='misc1')
        for i, k in enumerate(("ln1_g", "ln1_b", "ln2_g", "ln2_b")):
            m.dma(lnbc[:, i * D:(i + 1) * D], IN(k)[l].partition_broadcast(128), w=['lnbc'], slot=('lnbc', i))
        m.act(cact, cT, AF.Silu, r=['cT'], w=['cact'])
        for j in range(12):
            wb, wk = m.load_w(IN("w_ada")[l][:, j * 512:(j + 1) * 512], 8, 512, cast=False)
            b = m.bank()
            for k in range(8):
                m.mm(m.ps[0:1, b, :], cact[:, k:k + 1], wb[:, k, :], k == 0, k == 7, r=['cact', wk], w=[('ps', b)])
            m.tt('dve', modrow[0:1, j * 512:(j + 1) * 512], m.ps[0:1, b, :], brow[0:1, j * 512:(j + 1) * 512], ALU.add,
                 r=[('ps', b), 'brow'], w=['modrow'])
        b = m.bank()
        for j in range(48):
            m.mm(m.ps[:, b, j:j + 1], modrow[0:1, j * 128:(j + 1) * 128], one11, True, True, r=['modrow', 'ones'], w=[('ps', b)])
        m.cp('dve', modT, m.ps[:, b, 0:48], r=[('ps', b)], w=['modT'])
        m.ts('dve', scp[:, 0:8], modT[:, 8:16], 1.0, ALU.add, r=['modT'], w=['scp'])
        m.ts('dve', scp[:, 8:16], modT[:, 32:40], 1.0, ALU.add, r=['modT'], w=['scp'])
        for gi, c0 in enumerate((2 * D, 5 * D)):
            for hh in range(2):
                b = m.bank()
                m.mm(m.ps[:, b, :], ones_row[0:1, 0:128], modrow[0:1, c0 + hh * 512:c0 + (hh + 1) * 512], True, True,
                     r=['modrow', 'ones'], w=[('ps', b)])
                m.cp('act', gbc[:, gi * D + hh * 512:gi * D + (hh + 1) * 512], m.ps[:, b, :], r=[('ps', b)], w=['gbc'])
        m.barrier()
        if 'modT' in m.dbg:
            dd = m.scr("modT", [128, 48], F32)
            m.dma(dd, modT, r=['modT'], slot='dbg')
        if stop_after == ('mod', l):
            break

        m.aoff = persist_end
        hT = _r3(m.allocb(8 * S), 8)
        p1_end = m.aoff
        xts = [m.alloc(D) for _ in range(3)]

        def make_hT(dst, src_d, sc_ap, sh_ap, tts, dst_off=0):
            for tt_ in tts:
                s_ = tt_ % 3
                m.dma(xts[s_], src_d[tt_ * 128:(tt_ + 1) * 128, :], w=[('xt', s_)], slot=('xt', s_))
                b2 = m.bank2()
                for c in range(8):
                    bb, cc = b2 + c // 4, (c % 4) * 128
                    m.tr(m.ps[:, bb, cc:cc + 128], xts[s_][:, c * 128:(c + 1) * 128], ident, r=[('xt', s_), 'ident'], w=[('ps', bb)])
                for c in range(8):
                    bb, cc = b2 + c // 4, (c % 4) * 128
                    o = (tt_ - dst_off) * 128
                    m.act(dst[:, c, o:o + 128], m.ps[:, bb, cc:cc + 128], AF.Identity, r=[('ps', bb), 'scp', 'modT'],
                          w=[('hT', tt_)], scale=sc_ap[:, c:c + 1], bias=sh_ap[:, c:c + 1])
        make_hT(hT, x_src, scp[:, 0:8], modT[:, 0:8], range(NT))
        m.barrier()
        if 'hT' in m.dbg:
            dd = m.scr("hT", [128, 8 * S], BF16)
            m.dma(dd, hT.rearrange("p c n -> p (c n)"), slot='dbg')
            m.barrier()
        if stop_after == ('hT', l):
            break

        m.aoff = p1_end
        m.init_wbufs(2, 8 * 512)
        rowbuf = [m.allocb(S) for _ in range(2)]
        tokbuf = [m.allocb(8 * 520) for _ in range(2)]
        ri = [0]
        ti = [0]
        for tb_ in tokbuf:
            m.memset('pool', tb_, 1.0, w=[])
        m.barrier()
        HK = [('hT', t) for t in range(NT)]

        def proj_T(col0, dst_rows_list, evac_eng=('act', 'dve')):
            wb, wk = m.load_w(IN("w_in")[l][:, col0:col0 + 512], 8, 512)
            for j in range(4):
                if dst_rows_list[j] is None:
                    continue
                rb = rowbuf[ri[0] % 2]
                rk = ('row', ri[0] % 2)
                ri[0] += 1
                for tb in range(8):
                    b = m.bank()
                    for k in range(8):
                        m.mm(m.ps[:, b, :], wb[:, k, j * 128:(j + 1) * 128], hT[:, k, tb * 512:(tb + 1) * 512], k == 0, k == 7,
                             r=[wk] + HK[tb * 4:tb * 4 + 4], w=[('ps', b)])
                    m.cp(evac_eng[tb % 2], rb[:, tb * 512:(tb + 1) * 512], m.ps[:, b, :], r=[('ps', b)], w=[rk])
                m.dma(dst_rows_list[j], rb, r=[rk], slot=rk)

        def proj_N(col0, dst, width=512):
            wb, wk = m.load_w(IN("w_in")[l][:, col0:col0 + 512], 8, 512)
            for g in range(4):
                tb_ = tokbuf[ti[0] % 2]
                tk = ('tok', ti[0] % 2)
                ti[0] += 1
                t3 = tb_.rearrange("p (t n) -> p t n", t=8)
                for q in range(8):
                    tt_ = g * 8 + q
                    b = m.bank()
                    for k in range(8):
                        m.mm(m.ps[:, b, :], hT[:, k, tt_ * 128:(tt_ + 1) * 128], wb[:, k, :], k == 0, k == 7,
                             r=[wk, HK[tt_]], w=[('ps', b)])
                    if width == 520:
                        o = t3[:, q, :].rearrange("p (h e) -> p h e", e=65)[:, :, 0:64]
                        i_ = m.ps[:, b, :].rearrange("p (h e) -> p h e", e=64)
                    else:
                        o = t3[:, q, 0:512]
                        i_ = m.ps[:, b, :]
                    m.cp(('act', 'dve')[q % 2], o, i_, r=[('ps', b)], w=[tk])
                m.dma(dst[g * 1024:(g + 1) * 1024, :].rearrange("(t p) n -> p t n", p=128), t3[:, :, 0:width], r=[tk], slot=tk)

        def rows(name, j):
            return qT[name][j * 128:(j + 1) * 128, :]
        proj_T(0, [rows("mqT", j) for j in range(4)])
        proj_T(512, [rows("mkT", j) for j in range(4)])
        proj_N(1024, mv65, 520)
        proj_T(1536, [rows("rqkT", j) for j in range(4)])
        proj_N(2048, tokm["rv"])
        proj_N(2560, tokm["rg"])
        proj_N(3072, tokm["sz"])
        proj_T(4616, [rows("bqT", j) for j in range(4)])
        proj_T(5128, [rows("bkT", j) for j in range(4)])
        proj_N(5640, tokm["bv"])
        m.barrier()
        if stop_after == ('proj', l):
            break

        m.aoff = p1_end
        m.init_wbufs(1, 8 * 512)
        xc = m.alloc(S + 4)
        accb = m.alloc(S)
        sc2 = m.alloc(S)
        outb = sc2[:, 0:S // 2].bitcast(BF16)
        xtok = sc2[:, S // 2:S].bitcast(BF16)
        cwt = m.alloc(32)
        cbt = m.alloc(8)
        dtb = m.alloc(1)
        acol = m.alloc(1)
        m.dma(cwt, IN("conv_w")[l].rearrange("p c j -> p (c j)"), w=['cwt'], slot='misc0')
        m.dma(cbt, IN("conv_b")[l], w=['cbt'], slot='misc1')
        m.dma(dtb[0:8, :], IN("dt_bias")[l], w=['dtb'], slot='misc2')
        m.dma(acol[0:8, :], IN("a_log")[l], w=['acol'], slot='misc3')
        m.act(acol[0:8, :], acol[0:8, :], AF.Exp, r=['acol'], w=['acol'])
        m.ts('dve', acol[0:8, :], acol[0:8, :], -1.0, ALU.mult, r=['acol'], w=['acol'])
        m.memset('pool', accb[0:8, :], 1.0, w=['accb'])
        m.memset('pool', xc[:, 0:3], 0.0, w=['xc'])
        wb, wk = m.load_w(IN("w_in")[l][:, 4608:4616], 8, 8)
        dtT = xc[0:8, 4:4 + S]
        cum = sc2[0:8, :]
        for tb in range(8):
            b = m.bank()
            for k in range(8):
                m.mm(m.ps[0:8, b, :], wb[:, k, 0:8], hT[:, k, tb * 512:(tb + 1) * 512], k == 0, k == 7,
                     r=[wk] + HK[tb * 4:tb * 4 + 4], w=[('ps', b)])
            m.act(dtT[:, tb * 512:(tb + 1) * 512], m.ps[0:8, b, :], AF.Exp, r=[('ps', b), 'dtb'], w=['dtT'], bias=dtb[0:8, :])
        m.act(dtT, dtT, AF.Ln, r=['dtT'], w=['dtT'], bias=1.0)
        m.ts('dve', cum, dtT, acol[0:8, :], ALU.mult, r=['dtT', 'acol'], w=['cum'])
        m.op('dve', lambda e: e.tensor_tensor_scan(out=cum, data0=accb[0:8, :], data1=cum, initial=0.0, op0=ALU.mult, op1=ALU.add),
             r=['cum', 'accb'], w=['cum'])
        m.dma(scumT, cum, r=['cum'], slot='misc4')
        b = m.bank()
        for tt_ in range(NT):
            m.tr(m.ps[:, b, tt_ * 16:tt_ * 16 + 8], dtT[:, tt_ * 128:(tt_ + 1) * 128], ident[0:8, 0:8], r=['dtT', 'ident'], w=[('ps', b)])
            m.tr(m.ps[:, b, tt_ * 16 + 8:tt_ * 16 + 16], cum[:, tt_ * 128:(tt_ + 1) * 128], ident[0:8, 0:8], r=['cum', 'ident'], w=[('ps', b)])
        sdt_sb = accb[:, 512:1024]
        m.cp('dve', sdt_sb, m.ps[:, b, :], r=[('ps', b), 'accb'], w=['sdt_sb'])
        ncv = sdt_sb.rearrange("p (t e) -> p t e", e=16)[:, :, 8:16]
        m.ts('dve', ncv, ncv, -1.0, ALU.mult, r=['sdt_sb'], w=['sdt_sb'])
        m.dma(sdtok, sdt_sb, r=['sdt_sb'], slot='misc5')
        m.barrier()
        for blk in range(2):
            wb, wk = m.load_w(IN("w_in")[l][:, 3584 + blk * 512:3584 + (blk + 1) * 512], 8, 512)
            for jj in range(4):
                j = blk * 4 + jj
                for tb in range(8):
                    b = m.bank()
                    for k in range(8):
                        m.mm(m.ps[:, b, :], wb[:, k, jj * 128:(jj + 1) * 128], hT[:, k, tb * 512:(tb + 1) * 512], k == 0, k == 7,
                             r=[wk] + HK[tb * 4:tb * 4 + 4], w=[('ps', b)])
                    m.cp(('act', 'dve')[tb % 2], xc[:, 3 + tb * 512:3 + (tb + 1) * 512], m.ps[:, b, :], r=[('ps', b)], w=['xc'])
                m.act(accb, xc[:, 3:3 + S], AF.Identity, r=['xc', 'cwt', 'cbt'], w=['accb'], scale=cwt[:, j * 4 + 3:j * 4 + 4], bias=cbt[:, j:j + 1])
                for tap in (2, 1, 0):
                    m.stt(accb, xc[:, tap:tap + S], cwt[:, j * 4 + tap:j * 4 + tap + 1], accb, ALU.mult, ALU.add, r=['xc', 'cwt', 'accb'], w=['accb'])
                m.act(outb, accb, AF.Silu, r=['accb'], w=['outb'])
                if j >= 4:
                    m.dma(qT["sBCT"][(j - 4) * 128:(j - 3) * 128, :], outb, r=['outb'], slot='misc6')
                else:
                    for g in range(4):
                        b = m.bank()
                        pb = m.ps[:, b, :].bitcast(BF16)
                        for q in range(8):
                            tt_ = g * 8 + q
                            m.tr(pb[:, q * 128:(q + 1) * 128], outb[:, tt_ * 128:(tt_ + 1) * 128], identb, r=['outb', 'identb'], w=[('ps', b)])
                        m.cp(('act', 'dve')[g % 2], xtok[:, g * 1024:(g + 1) * 1024], pb, r=[('ps', b)], w=['xtok'])
                    m.dma(tokm["sx"][:, j * 128:(j + 1) * 128].rearrange("(t p) n -> p t n", p=128),
                          xtok.rearrange("p (t n) -> p t n", n=128), r=['xtok'], slot='misc7')
        m.barrier()
        if stop_after == ('ssdpre', l):
            break

        m.aoff = persist_end
        O_all = m.allocb(NT * 512).rearrange("p (t n) -> p t n", n=512)
        qkraw = m.alloc(4 * S // 2)
        qk = [[qkraw[:, (2 * s_ + i_) * 2048:(2 * s_ + i_ + 1) * 2048].bitcast(BF16) for i_ in range(2)] for s_ in range(2)]
        rowb = m.allocb(S)
        Ei = m.alloc(512)
        Ef = m.alloc(512)
        mix_base = m.aoff
        m.op('pool', lambda e: e.iota(Ei.bitcast(mybir.dt.int32), pattern=[[1, 512]], base=0, channel_multiplier=-1), w=['Ei'])
        m.cp('dve', Ef, Ei.bitcast(mybir.dt.int32), r=['Ei'], w=['Ef'])
        qki = [0]

        def load_qk(qname, qrow, kname, krow):
            s_ = qki[0] % 2
            qki[0] += 1
            m.dma(qk[s_][0][0:64, :], qT[qname][qrow:qrow + 64, :], w=[('qk', s_)], slot=('qk', s_, 0))
            m.dma(qk[s_][1][0:64, :], qT[kname][krow:krow + 64, :], r=[('qk', s_)], w=[('qk', s_)], slot=('qk', s_, 1))
            return qk[s_][0][0:64, :], qk[s_][1][0:64, :], ('qk', s_)

        def finalize_branch(n):
            for c in range(4):
                for g in range(4):
                    b = m.bank()
                    pb = m.ps[:, b, :].bitcast(BF16)
                    for q in range(8):
                        tt_ = g * 8 + q
                        m.tr(pb[:, q * 128:(q + 1) * 128], O_all[:, tt_, c * 128:(c + 1) * 128], identb, r=[('O', tt_), 'identb'], w=[('ps', b)])
                    m.cp(('act', 'dve')[g % 2], rowb[:, g * 1024:(g + 1) * 1024], pb, r=[('ps', b)], w=['rowb'])
                m.dma(oT[n][c * 128:(c + 1) * 128, :], rowb, r=['rowb'], slot='rowb')
            m.barrier()

        def mk_decay(dst_full, dst_mask, coef, width, strict=False):
            m.act(dst_full[:, 0:width], Ef[:, 0:width], AF.Exp, r=['Ef'], w=['dfull'], scale=coef)
            m.asel(dst_mask[:, 0:width], dst_full[:, 0:width], [[1, width]], ALU.is_ge, 0.0, -1 if strict else 0, -1, r=['dfull'], w=['dmask'])

        def diag_mask(ap, r, w, strict):
            m.asel(ap, ap, [[1, 128]], ALU.is_ge, 0.0, -1 if strict else 0, -1, r=r, w=w)

        if 'a' in MIX:
            m.aoff = mix_base
            V = m.allocb(NT * 520).rearrange("p (t h e) -> p t h e", h=8, e=65)
            m.dma(V.rearrange("p t h e -> p t (h e)"), mv65.rearrange("(t p) n -> p t n", p=128), w=['V'], slot='V')
            Eiw = m.alloc(4352)
            Efw = m.alloc(4352)
            m.op('pool', lambda e: e.iota(Eiw.bitcast(mybir.dt.int32), pattern=[[1, 4352]], base=0, channel_multiplier=-1), w=['Eiw'])
            m.cp('dve', Efw, Eiw.bitcast(mybir.dt.int32), r=['Eiw'], w=['Efw'])
            ksum = m.alloc(16)
            ktmp = m.alloc(16)
            khl = m.allocb(32)
            gate = m.alloc(512)
            sel = m.alloc(512)
            m8 = m.alloc(8)
            accs = [m.alloc(130) for _ in range(4)]
            rec = m.alloc(4)
            pes = [m.alloc(256) for _ in range(4)]
            pts = [m.allocb(256) for _ in range(6)]
            cnt = 0
            for h in range(8):
                slope = 2.0 ** (-(h + 1))
                qh, kh, qkk = load_qk("mqT", h * 64, "mkT", h * 64)
                m.op('dve', lambda e, kh=kh: e.tensor_reduce(out=ksum[0:64, :], in_=kh.rearrange("p (n j) -> p n j", j=256), axis=mybir.AxisListType.X, op=ALU.add),
                     r=[qkk], w=['ksum'])
                m.cp('dve', khl[0:64, 0:16], ksum[0:64, :], r=['ksum'], w=['khl'])
                m.tt('dve', ktmp[0:64, :], ksum[0:64, :], khl[0:64, 0:16], ALU.subtract, r=['ksum', 'khl'], w=['ktmp'])
                m.cp('dve', khl[0:64, 16:32], ktmp[0:64, :], r=['ktmp'], w=['khl'])
                b = m.bank()
                for tt_ in range(NT):
                    m.mm(m.ps[:, b, tt_ * 16:(tt_ + 1) * 16], qh[:, tt_ * 128:(tt_ + 1) * 128], khl[0:64, 0:16], True, False, r=[qkk, 'khl'], w=[('ps', b)])
                    m.mm(m.ps[:, b, tt_ * 16:(tt_ + 1) * 16], qh[:, tt_ * 128:(tt_ + 1) * 128], khl[0:64, 16:32], False, True, r=[qkk, 'khl'], w=[('ps', b)])
                m.cp('dve', gate, m.ps[:, b, :], r=[('ps', b)], w=['gate'])
                m.asel(gate.rearrange("p (t n) -> p t n", n=16), gate.rearrange("p (t n) -> p t n", n=16), [[1, 32], [-2, 16]], ALU.is_ge, -1e30, -2, 0,
                       r=['gate'], w=['gate'])
                for tt_ in range(NT):
                    own = tt_ // 2
                    g_ = gate[:, tt_ * 16:(tt_ + 1) * 16]
                    if own >= 4:
                        m.op('dve', lambda e, g_=g_: e.max(out=m8, in_=g_), r=['gate'], w=['m8'])
                        m.ts('dve', sel[:, tt_ * 16:(tt_ + 1) * 16], g_, m8[:, 2:3], ALU.is_ge, r=['gate', 'm8'], w=['sel'])
                    else:
                        m.ts('dve', sel[:, tt_ * 16:(tt_ + 1) * 16], g_, -1e29, ALU.is_gt, r=['gate'], w=['sel'])
                pend = [None]

                def flush():
                    if pend[0] is not None:
                        pend[0]()
                        pend[0] = None
                for qb in range(16):
                    q0 = qb * 256
                    acq = accs[(qb % 2) * 2:(qb % 2) * 2 + 2]
                    for a_ in acq:
                        m.memset('pool', a_, 0.0, w=[('acc', id(a_))])
                    for n in range(qb + 1):
                        diag = (n == qb)
                        ptl = []
                        for kt2 in range(2):
                            kt = 2 * n + kt2
                            c0 = 128 if (diag and kt2 == 1) else 0
                            b = m.bank()
                            m.mm(m.ps[:, b, c0:256], kh[:, kt * 128:(kt + 1) * 128], qh[:, q0 + c0:q0 + 256], True, True, r=[qkk], w=[('ps', b)])
                            pe_ = pes[cnt % 4]
                            pek = ('pe', cnt % 4)
                            pt = pts[cnt % 6]
                            ptk = ('pt', cnt % 6)
                            cnt += 1
                            off = q0 - kt * 128
                            m.stt(pe_[:, c0:256], Efw[:, off + c0:off + 256], -8.0 * slope, m.ps[:, b, c0:256], ALU.mult, ALU.add, r=[('ps', b), 'Efw'], w=[pek])
                            m.act(pt[:, c0:256], pe_[:, c0:256], AF.Exp, r=[pek], w=[ptk], scale=0.125)
                            if diag:
                                diag_mask(pt[:, c0:c0 + 128], r=[ptk], w=[ptk], strict=False)
                            ptl.append((pt, ptk, kt, c0))

                        def back(ptl=ptl, qb=qb, n=n, diag=diag, acq=acq):
                            for qt2 in range(2):
                                tt_ = 2 * qb + qt2
                                use = [(pt, ptk, kt) for (pt, ptk, kt, c0) in ptl if c0 <= qt2 * 128]
                                b = m.bank()
                                for i_, (pt, ptk, kt) in enumerate(use):
                                    m.mm(m.ps[:, b, 0:65], pt[:, qt2 * 128:(qt2 + 1) * 128], V[:, kt, h, :], i_ == 0, i_ == len(use) - 1,
                                         r=[ptk, 'V'], w=[('ps', b)])
                                ak = ('acc', id(acq[qt2]))
                                scal = 1.0 if diag else sel[:, tt_ * 16 + n:tt_ * 16 + n + 1]
                                m.stt(acq[qt2][:, 0:65], m.ps[:, b, 0:65], scal, acq[qt2][:, 0:65], ALU.mult, ALU.add, r=[('ps', b), 'sel', ak], w=[ak])
                            if diag:
                                for qt2 in range(2):
                                    tt_ = 2 * qb + qt2
                                    ak = ('acc', id(acq[qt2]))
                                    rc = rec[:, (qb % 2) * 2 + qt2:(qb % 2) * 2 + qt2 + 1]
                                    rk_ = ('rec', (qb % 2) * 2 + qt2)
                                    m.op('dve', lambda e, rc=rc, a_=acq[qt2]: e.reciprocal(out=rc, in_=a_[:, 64:65]), r=[ak], w=[rk_])
                                    m.ts('dve', O_all[:, tt_, h * 64:(h + 1) * 64], acq[qt2][:, 0:64], rc, ALU.mult, r=[ak, rk_], w=[('O', tt_)])
                        prev = pend[0]
                        pend[0] = back
                        if prev is not None:
                            prev()
                flush()
            finalize_branch(0)

        if 'b' in MIX:
            m.aoff = mix_base
            V = m.allocb(NT * 512).rearrange("p (t n) -> p t n", n=512)
            G = m.allocb(NT * 512).rearrange("p (t n) -> p t n", n=512)
            m.dma(V, tokm["rv"].rearrange("(t p) n -> p t n", p=128), w=['V'], slot='V')
            m.dma(G, tokm["rg"].rearrange("(t p) n -> p t n", p=128), w=['G'], slot='G')
            gng = m.alloc(512)
            gnb = m.alloc(512)
            m.dma(gng, IN("ret_gn_g")[l].partition_broadcast(128), w=['gng'], slot='misc0')
            m.dma(gnb, IN("ret_gn_b")[l].partition_broadcast(128), w=['gnb'], slot='misc1')
            Dfull = m.alloc(512)
            Dmask = m.alloc(512)
            pts = [m.allocb(512) for _ in range(4)]
            tmpo = [m.alloc(128) for _ in range(2)]
            tmps = [m.alloc(128) for _ in range(2)]
            cnt = 0
            ecnt = 0
            rqb = 0
            for h in range(4):
                lg = math.log(1.0 - 2.0 ** (-5.0 - h))
                qh, kh, qkk = load_qk("rqkT", h * 64, "rqkT", 256 + h * 64)
                mk_decay(Dfull, Dmask, lg, 512)
                pend = [None]
                for qb in range(8):
                    q0 = qb * 512
                    bo = 6 + (rqb % 2)
                    rqb += 1
                    nk = 4 * qb + 4
                    for kt in range(nk):
                        mdg = kt - 4 * qb
                        c0 = 128 * max(mdg, 0)
                        b = m.bank6()
                        m.mm(m.ps[:, b, c0:512], kh[:, kt * 128:(kt + 1) * 128], qh[:, q0 + c0:q0 + 512], True, True, r=[qkk], w=[('ps', b)])
                        pt = pts[cnt % 4]
                        ptk = ('pt', cnt % 4)
                        cnt += 1
                        if mdg >= 0:
                            m.stt(pt[:, c0:512], m.ps[:, b, c0:512], 0.125, Dmask[:, 0:512 - c0], ALU.mult, ALU.mult, r=[('ps', b), 'dmask'], w=[ptk])
                        else:
                            sc = 0.125 * math.exp(lg * (q0 - kt * 128))
                            m.stt(pt, m.ps[:, b, :], sc, Dfull, ALU.mult, ALU.mult, r=[('ps', b), 'dfull'], w=[ptk])

                        def back(pt=pt, ptk=ptk, kt=kt, mdg=mdg, bo=bo, qb=qb, last=(kt == nk - 1), h=h):
                            nonlocal ecnt
                            for qt in range(max(mdg, 0), 4):
                                m.mm(m.ps[:, bo, qt * 128:(qt + 1) * 128], pt[:, qt * 128:(qt + 1) * 128], V[:, kt, h * 128:(h + 1) * 128], (kt == 0 and qt == 0), False,
                                     r=[ptk, 'V'], w=[('ps', bo)])
                            if not last:
                                return
                            for qt in range(4):
                                tt_ = 4 * qb + qt
                                e_ = ecnt % 2
                                ecnt += 1
                                sm = small[:, 64 + e_ * 32:64 + (e_ + 1) * 32]
                                smk = ('sm', e_)
                                o_ = m.ps[:, bo, qt * 128:(qt + 1) * 128]
                                m.op('dve', lambda e, sm=sm, o_=o_: e.bn_stats(out=sm[:, 0:6], in_=o_), r=[('ps', bo)], w=[smk])
                                m.op('dve', lambda e, sm=sm: e.bn_aggr(out=sm[:, 6:8], in_=sm[:, 0:6]), r=[smk], w=[smk])
                                m.ts('dve', sm[:, 8:9], sm[:, 7:8], NORM_EPS, ALU.add, r=[smk], w=[smk])
                                m.act(sm[:, 9:10], sm[:, 8:9], AF.Sqrt, r=[smk], w=[smk])
                                m.op('dve', lambda e, sm=sm: e.reciprocal(out=sm[:, 10:11], in_=sm[:, 9:10]), r=[smk], w=[smk])
                                to = tmpo[e_]
                                tk = ('tmpo', e_)
                                m.ts('dve', to, o_, sm[:, 6:7], ALU.subtract, r=[('ps', bo), smk], w=[tk], s2=sm[:, 10:11], op1=ALU.mult)
                                m.tt('pool', to, to, gng[:, h * 128:(h + 1) * 128], ALU.mult, r=[tk, 'gng'], w=[tk])
                                m.tt('pool', to, to, gnb[:, h * 128:(h + 1) * 128], ALU.add, r=[tk, 'gnb'], w=[tk])
                                m.act(tmps[e_], G[:, tt_, h * 128:(h + 1) * 128], AF.Silu, r=['G'], w=[('tmps', e_)])
                                m.tt('pool', O_all[:, tt_, h * 128:(h + 1) * 128], to, tmps[e_], ALU.mult, r=[tk, ('tmps', e_)], w=[('O', tt_)])
                        prev = pend[0]
                        pend[0] = back
                        if prev is not None:
                            prev()
                pend[0]()
                pend[0] = None
            finalize_branch(1)

        if 'c' in MIX:
            m.aoff = mix_base
            X = m.allocb(NT * 512).rearrange("p (t n) -> p t n", n=512)
            Z = m.allocb(NT * 512).rearrange("p (t n) -> p t n", n=512)
            m.dma(X, tokm["sx"].rearrange("(t p) n -> p t n", p=128), w=['X'], slot='V')
            m.dma(Z, tokm["sz"].rearrange("(t p) n -> p t n", p=128), w=['Z'], slot='G')
            sdt = m.alloc(512)
            m.dma(sdt, sdtok, w=['sdt'], slot='misc0')
            dsk = m.alloc(8)
            m.dma(dsk, IN("d_skip")[l].partition_broadcast(128), w=['dsk'], slot='misc1')
            ngb = m.alloc(512)
            m.dma(ngb, IN("ssm_norm_g")[l].partition_broadcast(128), w=['ngb'], slot='misc2')
            BT = qk[0][0]
            CT = qk[0][1]
            Gbc = qkraw[:, 4096:8192]
            xdt = m.allocb(NT * 64).rearrange("p (t n) -> p t n", n=64)
            ss = m.alloc(NT * 8)
            rstd = m.alloc(NT * 2)
            Ls = [m.alloc(512) for _ in range(3)]
            wts = [m.allocb(512) for _ in range(4)]
            rqb = 0
            ytmp = [m.alloc(64) for _ in range(2)]
            ztmp = [m.alloc(64) for _ in range(2)]
            junk = m.alloc(64)
            sdt3 = sdt.rearrange("p (t e) -> p t e", e=16)
            cnt = 0
            ecnt = 0
            for g in range(2):
                m.dma(BT, qT["sBCT"][g * 128:(g + 1) * 128, :], w=['BT'], slot='misc3')
                m.dma(CT, qT["sBCT"][256 + g * 128:256 + (g + 1) * 128, :], w=['CT'], slot='misc4')
                for r_ in range(4):
                    h = 4 * g + r_
                    m.dma(Gbc, scumT[h:h + 1, :].partition_broadcast(128), w=['Gbc'], slot='misc5')
                    for tt_ in range(NT):
                        m.ts('pool', xdt[:, tt_, :], X[:, tt_, h * 64:(h + 1) * 64], sdt3[:, tt_, h:h + 1], ALU.mult, r=['X', 'sdt'], w=['xdt'])
                    pend = [None]
                    for tb in range(8):
                        t0 = tb * 512
                        by = 6 + (rqb % 2)
                        rqb += 1
                        ns_ = 4 * tb + 4
                        for st in range(ns_):
                            mdg = st - 4 * tb
                            c0 = 128 * max(mdg, 0)
                            b = m.bank6()
                            m.mm(m.ps[:, b, c0:512], BT[:, st * 128:(st + 1) * 128], CT[:, t0 + c0:t0 + 512], True, True, r=['BT', 'CT'], w=[('ps', b)])
                            L = Ls[cnt % 3]
                            lk = ('L', cnt % 3)
                            wt = wts[cnt % 4]
                            wk_ = ('wt', cnt % 4)
                            cnt += 1
                            m.act(L[:, c0:512], Gbc[:, t0 + c0:t0 + 512], AF.Exp, r=['Gbc', 'sdt'], w=[lk], bias=sdt3[:, st, 8 + h:9 + h])
                            if mdg >= 0:
                                diag_mask(L[:, c0:c0 + 128], r=[lk], w=[lk], strict=False)
                            m.tt('dve', wt[:, c0:512], L[:, c0:512], m.ps[:, b, c0:512], ALU.mult, r=[lk, ('ps', b)], w=[wk_])

                            def back(wt=wt, wk_=wk_, st=st, mdg=mdg, by=by, tb=tb, last=(st == ns_ - 1), h=h):
                                nonlocal ecnt
                                for qt in range(max(mdg, 0), 4):
                                    m.mm(m.ps[:, by, qt * 64:(qt + 1) * 64], wt[:, qt * 128:(qt + 1) * 128], xdt[:, st, :], (st == 0 and qt == 0), False,
                                         r=[wk_, 'xdt'], w=[('ps', by)])
                                if not last:
                                    return
                                for qt in range(4):
                                    tt_ = 4 * tb + qt
                                    e_ = ecnt % 2
                                    ecnt += 1
                                    yk = ('ytmp', e_)
                                    m.stt(ytmp[e_], X[:, tt_, h * 64:(h + 1) * 64], dsk[:, h:h + 1], m.ps[:, by, qt * 64:(qt + 1) * 64], ALU.mult, ALU.add,
                                          r=['X', 'dsk', ('ps', by)], w=[yk])
                                    m.act(ztmp[e_], Z[:, tt_, h * 64:(h + 1) * 64], AF.Silu, r=['Z'], w=[('ztmp', e_)])
                                    m.tt('dve', ytmp[e_], ytmp[e_], ztmp[e_], ALU.mult, r=[yk, ('ztmp', e_)], w=[yk])
                                    m.op('act', lambda e, e_=e_, tt_=tt_, h=h: e.activation(out=junk, in_=ytmp[e_], func=AF.Square, accum_out=ss[:, tt_ * 8 + h:tt_ * 8 + h + 1]),
                                         r=[yk], w=['junk', ('ss', tt_)])
                                    m.cp('pool', O_all[:, tt_, h * 64:(h + 1) * 64], ytmp[e_], r=[yk], w=[('O', tt_)])
                            prev = pend[0]
                            pend[0] = back
                            if prev is not None:
                                prev()
                    pend[0]()
                    pend[0] = None
                ssv = ss.rearrange("p (t e) -> p t e", e=8)[:, :, 4 * g:4 * g + 4]
                rg_ = rstd[:, g * NT:(g + 1) * NT]
                m.op('dve', lambda e, ssv=ssv, rg_=rg_: e.tensor_reduce(out=rg_, in_=ssv, axis=mybir.AxisListType.X, op=ALU.add),
                     r=[('ss', t) for t in range(NT)], w=[('rstd', g)])
                m.ts('dve', rg_, rg_, 1.0 / 256.0, ALU.mult, r=[('rstd', g)], w=[('rstd', g)], s2=LN_EPS, op1=ALU.add)
                m.act(rg_, rg_, AF.Sqrt, r=[('rstd', g)], w=[('rstd', g)])
                m.op('dve', lambda e, rg_=rg_: e.reciprocal(out=rg_, in_=rg_), r=[('rstd', g)], w=[('rstd', g)])
                for tt_ in range(NT):
                    o_ = O_all[:, tt_, g * 256:(g + 1) * 256]
                    m.stt(o_, o_, rg_[:, tt_:tt_ + 1], ngb[:, g * 256:(g + 1) * 256], ALU.mult, ALU.mult, r=[('O', tt_), ('rstd', g), 'ngb'], w=[('O', tt_)])
            finalize_branch(2)

        if 'd' in MIX:
            m.aoff = mix_base
            V = m.allocb(NT * 512).rearrange("p (t n) -> p t n", n=512)
            m.dma(V, tokm["bv"].rearrange("(t p) n -> p t n", p=128), w=['V'], slot='V')
            triU = m.allocb(128)
            triL = m.allocb(128)
            onesb = m.allocb(128)
            m.memset('pool', onesb, 1.0, w=['onesb'])
            m.asel(triU, onesb, [[-1, 128]], ALU.is_ge, 0.0, -1, 1, r=['onesb'], w=['triU'])
            m.asel(triL, onesb, [[1, 128]], ALU.is_ge, 0.0, 0, -1, r=['onesb'], w=['triL'])
            NS = 3
            bufs = []
            for hd in range(2):
                bufs.append(([m.alloc(512) for _ in range(NS)], [m.allocb(512) for _ in range(NS)],
                             [m.alloc(512) for _ in range(NS)], [m.allocb(512) for _ in range(NS)]))
            cntA = [0, 0]
            zc = [0, 0]

            def sb_head_qb(hd, h, qb, qh, kh, qkk):
                q0 = qb * 512
                kts = list(range(4 * qb + 3, -1, -1))
                n_ = len(kts)
                BACC, BO = 4 + hd, 6 + hd
                es_, spb_, ts__, wt_ = bufs[hd]
                info = {}

                def A(j):
                    kt = kts[j]
                    i = cntA[hd] % NS
                    cntA[hd] += 1
                    mdg = kt - 4 * qb
                    c0 = 128 * max(mdg, 0)
                    b = 2 * hd + (zc[hd] % 2)
                    zc[hd] += 1
                    ek, sk, tk, = ('es', hd, i), ('spb', hd, i), ('ts', hd, i)
                    m.mm(m.ps[:, b, c0:512], kh[:, kt * 128:(kt + 1) * 128], qh[:, q0 + c0:q0 + 512], True, True, r=[qkk], w=[('ps', b)])
                    m.act(es_[i][:, c0:512], m.ps[:, b, c0:512], AF.Exp, r=[('ps', b)], w=[ek], scale=0.125)
                    m.act(spb_[i][:, c0:512], es_[i][:, c0:512], AF.Ln, r=[ek], w=[sk], bias=1.0)
                    m.stt(ts__[i][:, c0:512], m.ps[:, b, c0:512], 0.125, spb_[i][:, c0:512], ALU.mult, ALU.subtract, r=[('ps', b), sk], w=[tk])
                    if mdg >= 0:
                        diag_mask(spb_[i][:, c0:c0 + 128], r=[sk], w=[sk], strict=True)
                    info[j] = (i, c0, mdg, kt)

                def B(j):
                    i, c0, mdg, kt = info[j]
                    sk, tk, wk_ = ('spb', hd, i), ('ts', hd, i), ('wt', hd, i)
                    m.mm(m.ps[:, BACC, c0:512], triU, spb_[i][:, c0:512], j == 0, False, r=['triU', sk], w=[('ps', BACC)])
                    m.tt('dve', ts__[i][:, c0:512], ts__[i][:, c0:512], m.ps[:, BACC, c0:512], ALU.subtract, r=[tk, ('ps', BACC)], w=[tk])
                    m.mm(m.ps[:, BACC, c0:512], triL, spb_[i][:, c0:512], False, False, r=['triL', sk], w=[('ps', BACC)])
                    m.act(wt_[i][:, c0:512], ts__[i][:, c0:512], AF.Exp, r=[tk], w=[wk_])
                    if mdg >= 0:
                        diag_mask(wt_[i][:, c0:c0 + 128], r=[wk_], w=[wk_], strict=True)

                def C(j):
                    i, c0, mdg, kt = info[j]
                    wk_ = ('wt', hd, i)
                    for qt in range(max(mdg, 0), 4):
                        m.mm(m.ps[:, BO, qt * 64:(qt + 1) * 64], wt_[i][:, qt * 128:(qt + 1) * 128], V[:, kt, h * 64:(h + 1) * 64], j == 0 and qt == max(mdg, 0), False,
                             r=[wk_, 'V'], w=[('ps', BO)])

                def E():
                    for qt in range(4):
                        tt_ = 4 * qb + qt
                        m.cp('act', O_all[:, tt_, h * 64:(h + 1) * 64], m.ps[:, BO, qt * 64:(qt + 1) * 64], r=[('ps', BO)], w=[('O', tt_)])
                return n_, A, B, C, E
            import os
            for hp in range(int(os.environ.get('SB_H', 8)) // 2):
                hs = (2 * hp, 2 * hp + 1)
                lq = [load_qk("bqT", h_ * 64, "bkT", h_ * 64) for h_ in hs]
                for qb in range(int(os.environ.get('SB_QB', 8))):
                    S_ = [sb_head_qb(hd, hs[hd], qb, lq[hd][0], lq[hd][1], lq[hd][2]) for hd in range(2)]
                    n_ = S_[0][0]
                    for hd in range(2):
                        S_[hd][1](0)
                    for j in range(n_):
                        if j + 1 < n_:
                            for hd in range(2):
                                S_[hd][1](j + 1)
                        for hd in range(2):
                            S_[hd][2](j)
                        if j >= 1:
                            for hd in range(2):
                                S_[hd][3](j - 1)
                    for hd in range(2):
                        S_[hd][3](n_ - 1)
                        S_[hd][4]()
            finalize_branch(3)
        m.barrier()
        if stop_after == ('mix', l):
            break

        m.aoff = persist_end
        hT = _r3(m.allocb(8 * S), 8)
        p1_end = m.aoff
        xts = [m.alloc(D) for _ in range(3)]
        make_hT(hT, x_src, scp[:, 0:8], modT[:, 0:8], range(NT))
        m.barrier()
        m.aoff = p1_end
        wst4 = [m.alloc(8 * 512)] * 2
        wg4 = [m.allocb(8 * 512) for _ in range(2)]
        wb4 = [m.allocb(4 * 512) for _ in range(2)]
        obs = [m.allocb(16 * 512) for _ in range(2)]
        rowbs = [m.allocb(S) for _ in range(2)]
        sigs = [m.alloc(512) for _ in range(2)]
        accs4 = [m.alloc(512) for _ in range(2)]
        tmps4 = [m.alloc(512) for _ in range(2)]
        oc = 0
        sc_ = 0
        for dc in range(8):
            i = dc % 2
            st_g = wst4[0].rearrange("p (c n j) -> p c n j", c=8, n=4)
            for n in range(4):
                c0 = 6152 + n * 1024 + dc * 128
                m.dma(st_g[:, :, n, :], IN("w_in")[l][:, c0:c0 + 128].rearrange("(c p) j -> p c j", p=128), w=[('wst4', 0)], r=[('wst4', 0)], slot=('wst4', 0, n))
            wg = wg4[i].rearrange("p (c n j) -> p c n j", c=8, n=4)
            m.cp('pool', wg4[i], wst4[i], r=[('wst4', 0)], w=[('wg4', i)])
            st_b = wst4[i][:, 0:2048].rearrange("p (n c j) -> p n c j", n=4, c=4)
            for n in range(4):
                m.dma(st_b[:, n, :, :], IN("w_br")[l][n][:, dc * 128:(dc + 1) * 128].rearrange("(c p) j -> p c j", p=128), w=[('wst4', 0)], r=[('wst4', 0)], slot=('wst4', 0, n))
            wbr = wb4[i].rearrange("p (n c j) -> p n c j", n=4, c=4)
            m.cp('pool', wb4[i], wst4[i][:, 0:2048], r=[('wst4', 0)], w=[('wb4', i)])
            rb = rowbs[dc % 2]
            rk = ('rowb4', dc % 2)
            for tb in range(8):
                o_i = oc % 2
                oc += 1
                ob = obs[o_i].rearrange("p (n c t) -> p n c t", n=4, c=4)
                for n in range(4):
                    m.dma(ob[:, n, :, :], oT[n][:, tb * 512:(tb + 1) * 512].rearrange("(c p) t -> p c t", p=128), w=[('ob', o_i, n)], slot=('ob', o_i, n))
                a_i = tb % 2
                for n in range(4):
                    bg = m.bank()
                    for k in range(8):
                        m.mm(m.ps[:, bg, :], wg[:, k, n, :], hT[:, k, tb * 512:(tb + 1) * 512], k == 0, k == 7, r=[('wg4', i)] + HK[tb * 4:tb * 4 + 4], w=[('ps', bg)])
                    by = m.bank()
                    for c in range(4):
                        m.mm(m.ps[:, by, :], wbr[:, n, c, :], ob[:, n, c, :], c == 0, c == 3, r=[('wb4', i), ('ob', o_i, n)], w=[('ps', by)])
                    s_i = sc_ % 2
                    sc_ += 1
                    m.act(sigs[s_i], m.ps[:, bg, :], AF.Sigmoid, r=[('ps', bg)], w=[('sig', s_i)])
                    if n == 0:
                        m.tt('dve', accs4[a_i], sigs[s_i], m.ps[:, by, :], ALU.mult, r=[('sig', s_i), ('ps', by)], w=[('acc4', a_i)])
                    else:
                        m.tt('dve', tmps4[s_i], sigs[s_i], m.ps[:, by, :], ALU.mult, r=[('sig', s_i), ('ps', by)], w=[('tmp4', s_i)])
                        if n < 3:
                            m.tt('pool', accs4[a_i], accs4[a_i], tmps4[s_i], ALU.add, r=[('acc4', a_i), ('tmp4', s_i)], w=[('acc4', a_i)])
                        else:
                            m.tt('pool', rb[:, tb * 512:(tb + 1) * 512], accs4[a_i], tmps4[s_i], ALU.add, r=[('acc4', a_i), ('tmp4', s_i)], w=[rk])
            m.dma(mergedT[dc * 128:(dc + 1) * 128, :], rb, r=[rk], slot=rk)
        m.barrier()
        if stop_after == ('merge', l):
            break

        m.aoff = persist_end
        m.init_wbufs(2, 8 * 512)
        wo = [m.load_w(IN("w_out")[l][:, hh * 512:(hh + 1) * 512], 8, 512) for hh in range(2)]
        mts = [m.allocb(8 * 128) for _ in range(2)]
        xts = [m.alloc(D) for _ in range(3)]
        tmp5 = [m.alloc(D) for _ in range(2)]
        out5 = [m.alloc(D) for _ in range(2)]
        for tt_ in range(NT):
            i = tt_ % 2
            mt = _r3(mts[i], 8)
            m.dma(mt, mergedT[:, tt_ * 128:(tt_ + 1) * 128].rearrange("(c p) t -> p c t", p=128), w=[('mt', i)], slot=('mt', i))
            xs = tt_ % 3
            m.dma(xts[xs], x_src[tt_ * 128:(tt_ + 1) * 128, :], w=[('xt', xs)], slot=('xt', xs))
            b2 = m.bank2()
            for hh in range(2):
                for k in range(8):
                    m.mm(m.ps[:, b2 + hh, :], mt[:, k, :], wo[hh][0][:, k, :], k == 0, k == 7, r=[('mt', i), wo[hh][1]], w=[('ps', b2 + hh)])
            ps2 = (m.ps[:, b2:b2 + 2, :].rearrange("p a n -> p (a n)"), [('ps', b2), ('ps', b2 + 1)])
            ln_epilogue(ps2, xts[xs], ('xt', xs), gbc[:, 0:D], lnbc[:, 0:D], lnbc[:, D:2 * D], out5[i], ('out5', i), tmp5[i], ('tmp5', i), i)
            m.dma(x1_d[tt_ * 128:(tt_ + 1) * 128, :], out5[i], r=[('out5', i)], slot=('out5', i))
        m.barrier()
        if stop_after == ('ln1', l):
            break

        m.aoff = persist_end
        moe = (l % 2 == 1)
        TBK = 1024
        h2T = _r3(m.allocb(8 * TBK), 8)
        yacc = _r3(m.alloc(8 * TBK), 8)
        aTs = [m.allocb(4 * TBK).rearrange("p (c t) -> p c t", c=4) for _ in range(2)]
        wst6 = [m.alloc(8 * 512) for _ in range(2)]
        wbf6 = [m.allocb(8 * 512) for _ in range(4)]
        xts = [m.alloc(D) for _ in range(2)]
        tmp6 = [m.alloc(D) for _ in range(1)]
        out6 = [m.alloc(D) for _ in range(2)]
        sil = [m.alloc(512) for _ in range(2)]
        tmy = [m.alloc(512) for _ in range(2)]
        wsi = [0]
        wbi = [0]

        def load6(src_r3, kc, n):
            si = wsi[0] % 2
            wsi[0] += 1
            bi = wbi[0] % 4
            wbi[0] += 1
            st = _r3(wst6[si][:, 0:kc * n], kc)
            m.dma(st, src_r3, w=[('wst6', si)], slot=('wst6', si))
            bf = _r3(wbf6[bi][:, 0:kc * n], kc)
            m.cp('act', bf, st, r=[('wst6', si)], w=[('wbf6', bi)])
            return bf, ('wbf6', bi)
        if moe:
            rw = m.alloc(64)
            rbb = m.alloc(8)
            selE = m.alloc(8 * 128)
            combT = m.alloc(TBK)
            cbt_ = m.alloc(TBK)
            hf = [m.alloc(8 * 128)] * 2
            rs = m.alloc(64)
            cpad = m.alloc(128)
            m.memset('pool', cpad, 0.0, w=['cpad'])
            m.dma(_r3(rw, 8), IN("router_w")[0].rearrange("(c p) e -> p c e", p=128), w=['rw'], slot='misc0')
            m.dma(rbb, IN("router_b")[0].partition_broadcast(128), w=['rbb'], slot='misc1')
            m.memset('pool', selE[0:8, :], 1.0, w=['selE'])
            for e_ in range(8):
                m.ts('dve', selE[0:8, e_ * 128:(e_ + 1) * 128], selE[0:8, e_ * 128:(e_ + 1) * 128], ident[0:8, e_:e_ + 1], ALU.mult, r=['selE', 'ident'], w=['selE'])
            experts = [(IN("expert_w_gu")[0][e], IN("expert_w_down")[0][e], D_FFE, e) for e in range(8)]
        else:
            experts = [(IN("ffn_w_gu")[l // 2], IN("ffn_w_down")[l // 2], D_FF, None)]
        for tbk in range(S // TBK):
            tts = list(range(tbk * 8, tbk * 8 + 8))
            for tt_ in tts:
                s_ = tt_ % 2
                m.dma(xts[s_], x1_d[tt_ * 128:(tt_ + 1) * 128, :], w=[('xt', s_)], slot=('xt', s_))
                b2 = m.bank2()
                for c in range(8):
                    bb, cc = b2 + c // 4, (c % 4) * 128
                    m.tr(m.ps[:, bb, cc:cc + 128], xts[s_][:, c * 128:(c + 1) * 128], ident, r=[('xt', s_), 'ident'], w=[('ps', bb)])
                o = (tt_ - tbk * 8) * 128
                import os
                MA = int(os.environ.get('MOE_A', 9))
                if not moe:
                    for c in range(8):
                        bb, cc = b2 + c // 4, (c % 4) * 128
                        m.act(h2T[:, c, o:o + 128], m.ps[:, bb, cc:cc + 128], AF.Identity, r=[('ps', bb), 'scp', 'modT'],
                              w=[('h2T', tt_ % 8)], scale=scp[:, 8 + c:9 + c], bias=modT[:, 24 + c:25 + c])
                else:
                    hfi = hf[0]
                    hk = ('hf', 0)
                    for c in range(8):
                        bb, cc = b2 + c // 4, (c % 4) * 128
                        m.act(hfi[:, c * 128:(c + 1) * 128], m.ps[:, bb, cc:cc + 128], AF.Identity, r=[('ps', bb), 'scp', 'modT'],
                              w=[hk], scale=scp[:, 8 + c:9 + c], bias=modT[:, 24 + c:25 + c])
                    m.cp('pool', h2T[:, :, o:o + 128], _r3(hfi, 8), r=[hk], w=[('h2T', tt_ % 8)])
                if moe and MA >= 1:
                    bl = m.bank()
                    for c in range(8):
                        m.mm(m.ps[:, bl, 0:8], hfi[:, c * 128:(c + 1) * 128], rw[:, c * 8:(c + 1) * 8], c == 0, c == 7, r=[hk, 'rw'], w=[('ps', bl)])
                    m.tt('dve', rs[:, 0:8], m.ps[:, bl, 0:8], rbb, ALU.add, r=[('ps', bl), 'rbb'], w=['rs'])
                if moe and MA >= 2:
                    m.op('dve', lambda e: e.max(out=rs[:, 8:16], in_=rs[:, 0:8]), r=['rs'], w=['rs'])
                    m.ts('dve', rs[:, 16:24], rs[:, 0:8], rs[:, 9:10], ALU.is_ge, r=['rs'], w=['rs'])
                    m.ts('dve', rs[:, 24:25], rs[:, 8:9], -1.0, ALU.mult, r=['rs'], w=['rs'])
                    m.act(rs[:, 32:40], rs[:, 0:8], AF.Exp, r=['rs'], w=['rs'], bias=rs[:, 24:25])
                    m.act(rs[:, 25:26], rs[:, 9:10], AF.Exp, r=['rs'], w=['rs'], bias=rs[:, 24:25])
                    m.ts('dve', rs[:, 25:26], rs[:, 25:26], 1.0, ALU.add, r=['rs'], w=['rs'])
                    m.op('dve', lambda e: e.reciprocal(out=rs[:, 26:27], in_=rs[:, 25:26]), r=['rs'], w=['rs'])
                    m.tt('dve', rs[:, 40:48], rs[:, 32:40], rs[:, 16:24], ALU.mult, r=['rs'], w=['rs'])
                    m.ts('dve', rs[:, 40:48], rs[:, 40:48], rs[:, 26:27], ALU.mult, r=['rs'], w=['rs'])
                if moe and MA >= 3:
                    bt = m.bank()
                    m.cp('dve', cpad[:, 0:8], rs[:, 40:48], r=['rs'], w=['cpad'])
                    m.tr(m.ps[:, bt, 0:128], cpad, ident, r=['cpad', 'ident'], w=[('ps', bt)])
                    m.cp('dve', combT[0:8, o:o + 128], m.ps[0:8, bt, 0:128], r=[('ps', bt)], w=['combT'])
            import os
            MD = os.environ.get('MOE_DBG', 'ABC')
            m.memset('pool', yacc, 0.0, w=['yacc'])
            H2K = [('h2T', i_) for i_ in range(8)]
            gi = 0
            for (wgu, wdn, F, e) in (experts if 'B' in MD else []):
                if e is not None:
                    for half in range(2):
                        bc_ = m.bank()
                        m.mm(m.ps[:, bc_, :], selE[0:8, e * 128:(e + 1) * 128], combT[0:8, half * 512:(half + 1) * 512], True, True, r=['selE', 'combT'], w=[('ps', bc_)])
                        m.cp('act', cbt_[:, half * 512:(half + 1) * 512], m.ps[:, bc_, :], r=[('ps', bc_)], w=['cbt_'])
                nfc = F // 128
                for f0 in range(0, nfc, 4):
                    ncq = min(4, nfc - f0)
                    ncol = ncq * 128
                    gw, gk = load6(wgu[:, f0 * 128:f0 * 128 + ncol].rearrange("(c p) n -> p c n", p=128), 8, ncol)
                    uw, uk = load6(wgu[:, F + f0 * 128:F + f0 * 128 + ncol].rearrange("(c p) n -> p c n", p=128), 8, ncol)
                    dw, dk = load6(wdn[f0 * 128:f0 * 128 + ncol, :].rearrange("(c p) n -> p c n", p=128), ncq, D)
                    aT = aTs[gi % 2]
                    ak = ('aT', gi % 2)
                    gi += 1
                    for fc in range(ncq):
                        for half in range(2):
                            pg = m.bank()
                            for k in range(8):
                                m.mm(m.ps[:, pg, :], gw[:, k, fc * 128:(fc + 1) * 128], h2T[:, k, half * 512:(half + 1) * 512], k == 0, k == 7,
                                     r=[gk] + H2K[half * 4:half * 4 + 4], w=[('ps', pg)])
                            pu = m.bank()
                            for k in range(8):
                                m.mm(m.ps[:, pu, :], uw[:, k, fc * 128:(fc + 1) * 128], h2T[:, k, half * 512:(half + 1) * 512], k == 0, k == 7,
                                     r=[uk] + H2K[half * 4:half * 4 + 4], w=[('ps', pu)])
                            s_i = (fc * 2 + half) % 2
                            m.act(sil[s_i], m.ps[:, pg, :], AF.Silu, r=[('ps', pg)], w=[('sil', s_i)])
                            m.tt('dve', aT[:, fc, half * 512:(half + 1) * 512], sil[s_i], m.ps[:, pu, :], ALU.mult, r=[('sil', s_i), ('ps', pu)], w=[ak])
                    for dc in range(8):
                        for half in range(2):
                            py = m.bank()
                            for fc in range(ncq):
                                m.mm(m.ps[:, py, :], dw[:, fc, dc * 128:(dc + 1) * 128], aT[:, fc, half * 512:(half + 1) * 512], fc == 0, fc == ncq - 1,
                                     r=[dk, ak], w=[('ps', py)])
                            ya = yacc[:, dc, half * 512:(half + 1) * 512]
                            yk = ('yacc', dc, half)
                            if e is not None:
                                t_i = (dc * 2 + half) % 2
                                m.tt('dve', tmy[t_i], m.ps[:, py, :], cbt_[:, half * 512:(half + 1) * 512], ALU.mult, r=[('ps', py), 'cbt_'], w=[('tmy', t_i)])
                                m.tt('dve', ya, ya, tmy[t_i], ALU.add, r=['yacc', yk, ('tmy', t_i)], w=[yk])
                            else:
                                m.tt('dve', ya, ya, m.ps[:, py, :], ALU.add, r=['yacc', yk, ('ps', py)], w=[yk])
            YK = [('yacc', dc, half) for dc in range(8) for half in range(2)] + ['yacc']
            for tt_ in (tts if 'C' in MD else []):
                s_ = tt_ % 2
                o = (tt_ - tbk * 8) * 128
                m.dma(xts[s_], x1_d[tt_ * 128:(tt_ + 1) * 128, :], w=[('xt', s_)], slot=('xt', s_))
                b2 = m.bank2()
                for c in range(8):
                    bb, cc = b2 + c // 4, (c % 4) * 128
                    m.tr(m.ps[:, bb, cc:cc + 128], yacc[:, c, o:o + 128], ident, r=YK + ['ident'], w=[('ps', bb)])
                ps2 = (m.ps[:, b2:b2 + 2, :].rearrange("p a n -> p (a n)"), [('ps', b2), ('ps', b2 + 1)])
                ln_epilogue(ps2, xts[s_], ('xt', s_), gbc[:, D:2 * D], lnbc[:, 2 * D:3 * D], lnbc[:, 3 * D:4 * D], out6[s_], ('out6', s_), tmp6[0], ('tmp6', 0), s_)
                m.dma(x_dst[tt_ * 128:(tt_ + 1) * 128, :], out6[s_], r=[('out6', s_)], slot=('out6', s_))
        m.barrier()
        if stop_after == ('ffn', l):
            break
    m.barrier()
    nc_ = m.finish()
    nc_.used_inputs = list(_ins.keys())
    return nc_


_CACHE = {}


def _prep_inputs(inputs, b):
    f = lambda a: np.ascontiguousarray(a, dtype=np.float32)
    R = {
        "x": lambda a: a[b],
        "c": lambda a: np.asarray(a[b]).reshape(8, 128).T,
        "b_ada": lambda a: np.asarray(a).reshape(2, 1, 6 * D),
        "conv_w": lambda a: np.asarray(a).reshape(2, 4, 8, 128).transpose(0, 3, 2, 1),
        "conv_b": lambda a: np.asarray(a).reshape(2, 8, 128).transpose(0, 2, 1),
        "dt_bias": lambda a: np.asarray(a).reshape(2, 8, 1),
        "a_log": lambda a: np.asarray(a).reshape(2, 8, 1),
        "d_skip": lambda a: np.asarray(a).reshape(2, 1, 8),
        "ssm_norm_g": lambda a: np.asarray(a).reshape(2, 1, 512),
        "ret_gn_g": lambda a: np.asarray(a).reshape(2, 1, 512),
        "ret_gn_b": lambda a: np.asarray(a).reshape(2, 1, 512),
        "ln1_g": lambda a: np.asarray(a).reshape(2, 1, D),
        "ln1_b": lambda a: np.asarray(a).reshape(2, 1, D),
        "ln2_g": lambda a: np.asarray(a).reshape(2, 1, D),
        "ln2_b": lambda a: np.asarray(a).reshape(2, 1, D),
        "router_b": lambda a: np.asarray(a).reshape(1, 1, 8),
    }
    return {k: f(R[k](v) if k in R else v) for k, v in inputs.items()}


def kernel(**inputs):
    nc = build()
    shared = _prep_inputs(inputs, 0)
    in_maps = []
    for b in range(8):
        d = dict(shared)
        d["x"] = np.ascontiguousarray(inputs["x"][b], dtype=np.float32)
        d["c"] = np.ascontiguousarray(np.asarray(inputs["c"][b]).reshape(8, 128).T, dtype=np.float32)
        in_maps.append(d)
    res = run_bass_kernel_spmd(nc, in_maps, core_ids=list(range(8)))
    return np.stack([np.asarray(r["y"], dtype=np.float32) for r in res.results], axis=0)
```

```python
import contextlib
import math
import numpy as np
import concourse.bass as bass
import concourse.mybir as mybir
from concourse.bass_utils import run_bass_kernel_spmd

F32 = mybir.dt.float32
BF16 = mybir.dt.bfloat16
AF = mybir.ActivationFunctionType
ALU = mybir.AluOpType

S = 4096
D = 1024
NT = 32
IN_W = 10248
ALPHA = 4.0 ** 0.25
LN_EPS = 1e-5
NORM_EPS = 1e-6
D_FF = 2816
D_FFE = 3584


class Builder:
    ENGS = ('pe', 'act', 'dve', 'pool', 'sp')

    def __init__(self):
        self.nc = bass.Bass("TRN2", target_bir_lowering=False)
        self.stack = contextlib.ExitStack()
        self.q = {e: [] for e in self.ENGS}
        self.seq = {e: 0 for e in self.ENGS}
        self.known = {e: {} for e in self.ENGS}
        self.last_w = {}
        self.readers = {}
        self.slots = {}
        self.sems = {}
        for e in self.ENGS:
            self.sems[('e', e)] = self.stack.enter_context(self.nc.semaphore("c_" + e))
        self._uid = 0

    def sbuf(self, shape, dtype, name=None):
        self._uid += 1
        return self.stack.enter_context(self.nc.sbuf_tensor(name or f"sb{self._uid}", list(shape), dtype))

    def psum(self, shape, dtype, name=None):
        self._uid += 1
        return self.stack.enter_context(self.nc.psum_tensor(name or f"ps{self._uid}", list(shape), dtype))

    def dram(self, name, shape, dtype, kind="Internal"):
        return self.nc.dram_tensor(name, list(shape), dtype, kind=kind).ap()

    def _deps(self, eng, r, w, skip_same_w=False):
        deps = {}

        def add(tok):
            s, v = tok
            if deps.get(s, 0) < v:
                deps[s] = v
        for k in r:
            t = self.last_w.get(k)
            if t is not None:
                add(t)
        for k in w:
            t = self.last_w.get(k)
            if t is not None and not (skip_same_w and t[0] == ('e', eng)):
                add(t)
            for t in self.readers.get(k, ()):
                add(t)
        waits = []
        kn = self.known[eng]
        for s, v in deps.items():
            if kn.get(s, 0) >= v:
                continue
            kn[s] = v
            waits.append((s, v))
        return waits

    def _record(self, tok, r, w):
        for k in w:
            self.last_w[k] = tok
            self.readers[k] = []
        for k in r:
            self.readers.setdefault(k, []).append(tok)

    def op(self, eng, fn, r=(), w=()):
        waits = self._deps(eng, r, w, skip_same_w=(eng == 'pe'))
        self.seq[eng] += 1
        tok = (('e', eng), self.seq[eng])
        self.q[eng].append((fn, waits, (('e', eng), 1)))
        self._record(tok, r, w)
        return tok

    def dma(self, out, in_, r=(), w=(), slot=None, q='sp'):
        if slot not in self.slots:
            self.sems[('d', slot)] = self.stack.enter_context(self.nc.semaphore("d%d" % len(self.slots)))
            self.slots[slot] = [('d', slot), 0]
        waits = self._deps(q, r, w)
        ent = self.slots[slot]
        ent[1] += 16
        tok = (ent[0], ent[1])
        self.q[q].append((lambda e, o=out, i=in_: e.dma_start(out=o, in_=i), waits, (ent[0], 16)))
        self._record(tok, r, w)
        return tok

    def barrier(self):
        toks = []
        for e in self.ENGS:
            if self.seq[e] > 0:
                toks.append((('e', e), self.seq[e]))
        for k, ent in self.slots.items():
            if ent[1] > 0:
                toks.append((ent[0], ent[1]))
        for e in self.ENGS:
            kn = self.known[e]
            waits = []
            for s, v in toks:
                if kn.get(s, 0) >= v:
                    continue
                kn[s] = v
                waits.append((s, v))
            if waits:
                self.q[e].append((None, waits, None))
        self.last_w.clear()
        self.readers.clear()

    def finish(self):
        self.barrier()
        nc = self.nc
        sems = self.sems

        def run(eng_obj, lst):
            for fn, waits, inc in lst:
                for s, v in waits:
                    eng_obj.wait_ge(sems[s], v)
                if fn is not None:
                    fn(eng_obj).then_inc(sems[inc[0]], inc[1])
        with nc.Block() as block:
            @block.tensor
            def _(e):
                run(e, self.q['pe'])

            @block.scalar
            def _(e):
                run(e, self.q['act'])

            @block.vector
            def _(e):
                run(e, self.q['dve'])

            @block.gpsimd
            def _(e):
                run(e, self.q['pool'])

            @block.sync
            def _(e):
                run(e, self.q['sp'])
        self.stack.close()
        return nc


def _r3(ap, c):
    return ap.rearrange("p (c n) -> p c n", c=c)


class MK(Builder):
    AW = 50 * 1024

    def __init__(self, dbg=()):
        super().__init__()
        self.dbg = set(dbg)
        self.arena = self.sbuf([128, self.AW], F32, "arena")
        self.aoff = 0
        self.ps = self.psum([128, 8, 512], F32, "psum")
        self.pcur = 0
        self.ktag = 0

    def alloc(self, words):
        a = self.aoff
        self.aoff += words
        assert self.aoff <= self.AW, (self.aoff, self.AW)
        return self.arena[:, a:a + words]

    def allocb(self, n):
        assert n % 2 == 0
        return self.alloc(n // 2).bitcast(BF16)

    def key(self, name):
        self.ktag += 1
        return (name, self.ktag)

    def bank(self):
        i = self.pcur
        self.pcur = (self.pcur + 1) % 8
        return i

    def bank6(self):
        i = getattr(self, 'p6', 0)
        self.p6 = (i + 1) % 6
        return i

    def bank2(self):
        if self.pcur % 2:
            self.pcur = (self.pcur + 1) % 8
        i = self.pcur
        self.pcur = (self.pcur + 2) % 8
        return i

    def scr(self, name, shape, dtype):
        return self.dram(name, shape, dtype, kind="ExternalOutput" if name in self.dbg else "Internal")

    def mm(self, out, lhsT, rhs, start, stop, r, w):
        self.op('pe', lambda e: e.matmul(out, lhsT=lhsT, rhs=rhs, start=start, stop=stop), r=r, w=w)

    def tr(self, out, in_, ident, r, w):
        self.op('pe', lambda e: e.transpose(out=out, in_=in_, identity=ident), r=r, w=w)

    def act(self, out, in_, func, r, w, bias=None, scale=None):
        kw = {}
        if bias is not None:
            kw['bias'] = bias
        if scale is not None:
            kw['scale'] = scale
        self.op('act', lambda e: e.activation(out=out, in_=in_, func=func, **kw), r=r, w=w)

    def tt(self, eng, out, in0, in1, op, r, w):
        self.op(eng, lambda e: e.tensor_tensor(out=out, in0=in0, in1=in1, op=op), r=r, w=w)

    def ts(self, eng, out, in0, s1, op0, r, w, s2=None, op1=None):
        if op1 is None:
            self.op(eng, lambda e: e.tensor_scalar(out=out, in0=in0, scalar1=s1, scalar2=None, op0=op0), r=r, w=w)
        else:
            self.op(eng, lambda e: e.tensor_scalar(out=out, in0=in0, scalar1=s1, scalar2=s2, op0=op0, op1=op1), r=r, w=w)

    def stt(self, out, in0, scalar, in1, op0, op1, r, w):
        self.op('dve', lambda e: e.scalar_tensor_tensor(out=out, in0=in0, scalar=scalar, in1=in1, op0=op0, op1=op1), r=r, w=w)

    def cp(self, eng, out, in_, r, w):
        if eng == 'act':
            self.op('act', lambda e: e.copy(out=out, in_=in_), r=r, w=w)
        else:
            self.op(eng, lambda e: e.tensor_copy(out=out, in_=in_), r=r, w=w)

    def memset(self, eng, ap, val, w):
        self.op(eng, lambda e: e.memset(ap, val), w=w)

    def asel(self, out, in_, pattern, cmp, fill, base, cm, r, w):
        self.op('pool', lambda e: e.affine_select(out=out, in_=in_, pattern=pattern, compare_op=cmp, fill=fill,
                                                  base=base, channel_multiplier=cm), r=r, w=w)

    def init_wbufs(self, nslots, words):
        self.wst = [self.alloc(words) for _ in range(nslots)]
        self.wbf = [self.alloc(words // 2).bitcast(BF16) for _ in range(nslots)]
        self.wi = 0
        self.wn = nslots

    def load_w(self, src, kc, n, cast=True):
        i = self.wi
        self.wi = (self.wi + 1) % self.wn
        st = _r3(self.wst[i][:, 0:kc * n], kc)
        self.dma(st, src.rearrange("(c p) n -> p c n", p=128), w=[('wst', i)], slot=('wst', i))
        if not cast:
            return st, ('wst', i)
        bf = _r3(self.wbf[i][:, 0:kc * n], kc)
        self.cp('pool', bf, st, r=[('wst', i)], w=[('wbf', i)])
        return bf, ('wbf', i)


def build(dbg=(), stop_after=None, layers=(0, 1), MIX='abcd', skip=(), x_first=False):
    m = MK(dbg)
    nc = m.nc

    SHAPES = dict(x=[S, D], c=[128, 8], w_ada=[2, D, 6 * D], b_ada=[2, 1, 6 * D], w_in=[2, D, IN_W],
                  conv_w=[2, 128, 8, 4], conv_b=[2, 128, 8], dt_bias=[2, 8, 1], a_log=[2, 8, 1], d_skip=[2, 1, 8],
                  ssm_norm_g=[2, 1, 512], ret_gn_g=[2, 1, 512], ret_gn_b=[2, 1, 512], w_br=[2, 4, 512, D],
                  w_out=[2, D, D], ln1_g=[2, 1, D], ln1_b=[2, 1, D], ln2_g=[2, 1, D], ln2_b=[2, 1, D],
                  ffn_w_gu=[1, D, 2 * D_FF], ffn_w_down=[1, D_FF, D], router_w=[1, D, 8], router_b=[1, 1, 8],
                  expert_w_gu=[1, 8, D, 2 * D_FFE], expert_w_down=[1, 8, D_FFE, D])
    _ins = {}

    def IN(name):
        if name not in _ins:
            _ins[name] = m.dram(name, SHAPES[name], F32, kind="ExternalInput")
        return _ins[name]
    m.used_inputs = _ins
    y_out = m.dram("y", [S, D], F32, kind="ExternalOutput")

    qT = {n: m.scr(n, [512, S], BF16) for n in ("mqT", "mkT", "rqkT", "bqT", "bkT", "sBCT")}
    mv65 = m.scr("mv65", [S, 520], BF16)
    tokm = {n: m.scr(n, [S, 512], BF16) for n in ("rv", "rg", "sz", "bv", "sx")}
    scumT = m.scr("scumT", [8, S], F32)
    sdtok = m.scr("sdtok", [128, NT * 16], F32)
    oT = m.scr("oT", [4, 512, S], BF16)
    mergedT = m.scr("mergedT", [D, S], BF16)
    x1_d = m.scr("x1", [S, D], F32)
    x2_d = m.scr("x2", [S, D], F32)

    ident = m.alloc(128)
    identb = m.allocb(128)
    ones_row = m.alloc(128)
    one11 = ones_row[0:1, 0:1]
    modT = m.alloc(48)
    scp = m.alloc(16)
    gbc = m.alloc(2 * D)
    lnbc = m.alloc(4 * D)
    small = m.alloc(256)
    persist_end = m.aoff

    m.memset('pool', ident, 1.0, w=['ident'])
    m.asel(ident, ident, [[-1, 128]], ALU.is_equal, 0.0, 0, 1, r=['ident'], w=['ident'])
    m.cp('pool', identb, ident, r=['ident'], w=['identb'])
    m.memset('pool', ones_row, 1.0, w=['ones'])
    m.barrier()

    def ln_epilogue(ps2, xt, xkey, g_ap, lg, lb, out_t, okey, tmp, tkey, st):
        pk, stk = ps2[1], ('st', st)
        m.tt('dve', tmp, ps2[0], g_ap, ALU.mult, r=list(pk) + ['gbc'], w=[tkey])
        m.stt(tmp, xt, ALPHA, tmp, ALU.mult, ALU.add, r=[xkey, tkey], w=[tkey])
        sm = small[:, st * 32:(st + 1) * 32]
        m.op('dve', lambda e: e.bn_stats(out=sm[:, 0:6], in_=tmp[:, 0:512]), r=[tkey], w=[stk])
        m.op('dve', lambda e: e.bn_stats(out=sm[:, 6:12], in_=tmp[:, 512:1024]), r=[tkey], w=[stk])
        m.op('dve', lambda e: e.bn_aggr(out=sm[:, 12:14], in_=sm[:, 0:12]), r=[stk], w=[stk])
        m.ts('dve', sm[:, 14:15], sm[:, 13:14], LN_EPS, ALU.add, r=[stk], w=[stk])
        m.act(sm[:, 15:16], sm[:, 14:15], AF.Sqrt, r=[stk], w=[stk])
        m.op('dve', lambda e: e.reciprocal(out=sm[:, 16:17], in_=sm[:, 15:16]), r=[stk], w=[stk])
        m.ts('dve', tmp, tmp, sm[:, 12:13], ALU.subtract, r=[tkey, stk], w=[tkey], s2=sm[:, 16:17], op1=ALU.mult)
        m.tt('pool', tmp, tmp, lg, ALU.mult, r=[tkey, 'lnbc'], w=[tkey])
        m.tt('pool', out_t, tmp, lb, ALU.add, r=[tkey, 'lnbc'], w=[okey])

    for l in layers:
        x_src = IN("x") if (l == 0 or x_first) else x2_d
        x_dst = x2_d if l == 0 else y_out
        m.aoff = persist_end
        cT = m.alloc(8)
        cact = m.alloc(8)
        modrow = m.alloc(6 * D)
        brow = m.alloc(6 * D)
        m.init_wbufs(2, 8 * 512)
        m.dma(cT, IN("c"), w=['cT'], slot='misc0')
        m.dma(brow[0:1, :], IN("b_ada")[l], w=['brow'], slot='misc1')
        for i, k in enumerate(("ln1_g", "ln1_b", "ln2_g", "ln2_b")):
            m.dma(lnbc[:, i * D:(i + 1) * D], IN(k)[l].partition_broadcast(128), w=['lnbc'], slot=('lnbc', i))
        m.act(cact, cT, AF.Silu, r=['cT'], w=['cact'])
        for j in range(12):
            wb, wk = m.load_w(IN("w_ada")[l][:, j * 512:(j + 1) * 512], 8, 512, cast=False)
            b = m.bank()
            for k in range(8):
                m.mm(m.ps[0:1, b, :], cact[:, k:k + 1], wb[:, k, :], k == 0, k == 7, r=['cact', wk], w=[('ps', b)])
            m.tt('dve', modrow[0:1, j * 512:(j + 1) * 512], m.ps[0:1, b, :], brow[0:1, j * 512:(j + 1) * 512], ALU.add,
                 r=[('ps', b), 'brow'], w=['modrow'])
        b = m.bank()
        for j in range(48):
            m.mm(m.ps[:, b, j:j + 1], modrow[0:1, j * 128:(j + 1) * 128], one11, True, True, r=['modrow', 'ones'], w=[('ps', b)])
        m.cp('dve', modT, m.ps[:, b, 0:48], r=[('ps', b)], w=['modT'])
        m.ts('dve', scp[:, 0:8], modT[:, 8:16], 1.0, ALU.add, r=['modT'], w=['scp'])
        m.ts('dve', scp[:, 8:16], modT[:, 32:40], 1.0, ALU.add, r=['modT'], w=['scp'])
        for gi, c0 in enumerate((2 * D, 5 * D)):
            for hh in range(2):
                b = m.bank()
                m.mm(m.ps[:, b, :], ones_row[0:1, 0:128], modrow[0:1, c0 + hh * 512:c0 + (hh + 1) * 512], True, True,
                     r=['modrow', 'ones'], w=[('ps', b)])
                m.cp('act', gbc[:, gi * D + hh * 512:gi * D + (hh + 1) * 512], m.ps[:, b, :], r=[('ps', b)], w=['gbc'])
        m.barrier()
        if 'modT' in m.dbg:
            dd = m.scr("modT", [128, 48], F32)
            m.dma(dd, modT, r=['modT'], slot='dbg')
        if stop_after == ('mod', l):
            break

        m.aoff = persist_end
        hT = _r3(m.allocb(8 * S), 8)
        p1_end = m.aoff
        xts = [m.alloc(D) for _ in range(3)]

        def make_hT(dst, src_d, sc_ap, sh_ap, tts, dst_off=0):
            for tt_ in tts:
                s_ = tt_ % 3
                m.dma(xts[s_], src_d[tt_ * 128:(tt_ + 1) * 128, :], w=[('xt', s_)], slot=('xt', s_))
                b2 = m.bank2()
                for c in range(8):
                    bb, cc = b2 + c // 4, (c % 4) * 128
                    m.tr(m.ps[:, bb, cc:cc + 128], xts[s_][:, c * 128:(c + 1) * 128], ident, r=[('xt', s_), 'ident'], w=[('ps', bb)])
                for c in range(8):
                    bb, cc = b2 + c // 4, (c % 4) * 128
                    o = (tt_ - dst_off) * 128
                    m.act(dst[:, c, o:o + 128], m.ps[:, bb, cc:cc + 128], AF.Identity, r=[('ps', bb), 'scp', 'modT'],
                          w=[('hT', tt_)], scale=sc_ap[:, c:c + 1], bias=sh_ap[:, c:c + 1])
        make_hT(hT, x_src, scp[:, 0:8], modT[:, 0:8], range(NT))
        m.barrier()
        if 'hT' in m.dbg:
            dd = m.scr("hT", [128, 8 * S], BF16)
            m.dma(dd, hT.rearrange("p c n -> p (c n)"), slot='dbg')
            m.barrier()
        if stop_after == ('hT', l):
            break

        m.aoff = p1_end
        m.init_wbufs(2, 8 * 512)
        rowbuf = [m.allocb(S) for _ in range(2)]
        tokbuf = [m.allocb(8 * 520) for _ in range(2)]
        ri = [0]
        ti = [0]
        for tb_ in tokbuf:
            m.memset('pool', tb_, 1.0, w=[])
        m.barrier()
        HK = [('hT', t) for t in range(NT)]

        def proj_T(col0, dst_rows_list, evac_eng=('act', 'dve')):
            wb, wk = m.load_w(IN("w_in")[l][:, col0:col0 + 512], 8, 512)
            for j in range(4):
                if dst_rows_list[j] is None:
                    continue
                rb = rowbuf[ri[0] % 2]
                rk = ('row', ri[0] % 2)
                ri[0] += 1
                for tb in range(8):
                    b = m.bank()
                    for k in range(8):
                        m.mm(m.ps[:, b, :], wb[:, k, j * 128:(j + 1) * 128], hT[:, k, tb * 512:(tb + 1) * 512], k == 0, k == 7,
                             r=[wk] + HK[tb * 4:tb * 4 + 4], w=[('ps', b)])
                    m.cp(evac_eng[tb % 2], rb[:, tb * 512:(tb + 1) * 512], m.ps[:, b, :], r=[('ps', b)], w=[rk])
                m.dma(dst_rows_list[j], rb, r=[rk], slot=rk)

        def proj_N(col0, dst, width=512):
            wb, wk = m.load_w(IN("w_in")[l][:, col0:col0 + 512], 8, 512)
            for g in range(4):
                tb_ = tokbuf[ti[0] % 2]
                tk = ('tok', ti[0] % 2)
                ti[0] += 1
                t3 = tb_.rearrange("p (t n) -> p t n", t=8)
                for q in range(8):
                    tt_ = g * 8 + q
                    b = m.bank()
                    for k in range(8):
                        m.mm(m.ps[:, b, :], hT[:, k, tt_ * 128:(tt_ + 1) * 128], wb[:, k, :], k == 0, k == 7,
                             r=[wk, HK[tt_]], w=[('ps', b)])
                    if width == 520:
                        o = t3[:, q, :].rearrange("p (h e) -> p h e", e=65)[:, :, 0:64]
                        i_ = m.ps[:, b, :].rearrange("p (h e) -> p h e", e=64)
                    else:
                        o = t3[:, q, 0:512]
                        i_ = m.ps[:, b, :]
                    m.cp(('act', 'dve')[q % 2], o, i_, r=[('ps', b)], w=[tk])
                m.dma(dst[g * 1024:(g + 1) * 1024, :].rearrange("(t p) n -> p t n", p=128), t3[:, :, 0:width], r=[tk], slot=tk)

        def rows(name, j):
            return qT[name][j * 128:(j + 1) * 128, :]
        proj_T(0, [rows("mqT", j) for j in range(4)])
        proj_T(512, [rows("mkT", j) for j in range(4)])
        proj_N(1024, mv65, 520)
        proj_T(1536, [rows("rqkT", j) for j in range(4)])
        proj_N(2048, tokm["rv"])
        proj_N(2560, tokm["rg"])
        proj_N(3072, tokm["sz"])
        proj_T(4616, [rows("bqT", j) for j in range(4)])
        proj_T(5128, [rows("bkT", j) for j in range(4)])
        proj_N(5640, tokm["bv"])
        m.barrier()
        if stop_after == ('proj', l):
            break

        m.aoff = p1_end
        m.init_wbufs(1, 8 * 512)
        xc = m.alloc(S + 4)
        accb = m.alloc(S)
        sc2 = m.alloc(S)
        outb = sc2[:, 0:S // 2].bitcast(BF16)
        xtok = sc2[:, S // 2:S].bitcast(BF16)
        cwt = m.alloc(32)
        cbt = m.alloc(8)
        dtb = m.alloc(1)
        acol = m.alloc(1)
        m.dma(cwt, IN("conv_w")[l].rearrange("p c j -> p (c j)"), w=['cwt'], slot='misc0')
        m.dma(cbt, IN("conv_b")[l], w=['cbt'], slot='misc1')
        m.dma(dtb[0:8, :], IN("dt_bias")[l], w=['dtb'], slot='misc2')
        m.dma(acol[0:8, :], IN("a_log")[l], w=['acol'], slot='misc3')
        m.act(acol[0:8, :], acol[0:8, :], AF.Exp, r=['acol'], w=['acol'])
        m.ts('dve', acol[0:8, :], acol[0:8, :], -1.0, ALU.mult, r=['acol'], w=['acol'])
        m.memset('pool', accb[0:8, :], 1.0, w=['accb'])
        m.memset('pool', xc[:, 0:3], 0.0, w=['xc'])
        wb, wk = m.load_w(IN("w_in")[l][:, 4608:4616], 8, 8)
        dtT = xc[0:8, 4:4 + S]
        cum = sc2[0:8, :]
        for tb in range(8):
            b = m.bank()
            for k in range(8):
                m.mm(m.ps[0:8, b, :], wb[:, k, 0:8], hT[:, k, tb * 512:(tb + 1) * 512], k == 0, k == 7,
                     r=[wk] + HK[tb * 4:tb * 4 + 4], w=[('ps', b)])
            m.act(dtT[:, tb * 512:(tb + 1) * 512], m.ps[0:8, b, :], AF.Exp, r=[('ps', b), 'dtb'], w=['dtT'], bias=dtb[0:8, :])
        m.act(dtT, dtT, AF.Ln, r=['dtT'], w=['dtT'], bias=1.0)
        m.ts('dve', cum, dtT, acol[0:8, :], ALU.mult, r=['dtT', 'acol'], w=['cum'])
        m.op('dve', lambda e: e.tensor_tensor_scan(out=cum, data0=accb[0:8, :], data1=cum, initial=0.0, op0=ALU.mult, op1=ALU.add),
             r=['cum', 'accb'], w=['cum'])
        m.dma(scumT, cum, r=['cum'], slot='misc4')
        b = m.bank()
        for tt_ in range(NT):
            m.tr(m.ps[:, b, tt_ * 16:tt_ * 16 + 8], dtT[:, tt_ * 128:(tt_ + 1) * 128], ident[0:8, 0:8], r=['dtT', 'ident'], w=[('ps', b)])
            m.tr(m.ps[:, b, tt_ * 16 + 8:tt_ * 16 + 16], cum[:, tt_ * 128:(tt_ + 1) * 128], ident[0:8, 0:8], r=['cum', 'ident'], w=[('ps', b)])
        sdt_sb = accb[:, 512:1024]
        m.cp('dve', sdt_sb, m.ps[:, b, :], r=[('ps', b), 'accb'], w=['sdt_sb'])
        ncv = sdt_sb.rearrange("p (t e) -> p t e", e=16)[:, :, 8:16]
        m.ts('dve', ncv, ncv, -1.0, ALU.mult, r=['sdt_sb'], w=['sdt_sb'])
        m.dma(sdtok, sdt_sb, r=['sdt_sb'], slot='misc5')
        m.barrier()
        for blk in range(2):
            wb, wk = m.load_w(IN("w_in")[l][:, 3584 + blk * 512:3584 + (blk + 1) * 512], 8, 512)
            for jj in range(4):
                j = blk * 4 + jj
                for tb in range(8):
                    b = m.bank()
                    for k in range(8):
                        m.mm(m.ps[:, b, :], wb[:, k, jj * 128:(jj + 1) * 128], hT[:, k, tb * 512:(tb + 1) * 512], k == 0, k == 7,
                             r=[wk] + HK[tb * 4:tb * 4 + 4], w=[('ps', b)])
                    m.cp(('act', 'dve')[tb % 2], xc[:, 3 + tb * 512:3 + (tb + 1) * 512], m.ps[:, b, :], r=[('ps', b)], w=['xc'])
                m.act(accb, xc[:, 3:3 + S], AF.Identity, r=['xc', 'cwt', 'cbt'], w=['accb'], scale=cwt[:, j * 4 + 3:j * 4 + 4], bias=cbt[:, j:j + 1])
                for tap in (2, 1, 0):
                    m.stt(accb, xc[:, tap:tap + S], cwt[:, j * 4 + tap:j * 4 + tap + 1], accb, ALU.mult, ALU.add, r=['xc', 'cwt', 'accb'], w=['accb'])
                m.act(outb, accb, AF.Silu, r=['accb'], w=['outb'])
                if j >= 4:
                    m.dma(qT["sBCT"][(j - 4) * 128:(j - 3) * 128, :], outb, r=['outb'], slot='misc6')
                else:
                    for g in range(4):
                        b = m.bank()
                        pb = m.ps[:, b, :].bitcast(BF16)
                        for q in range(8):
                            tt_ = g * 8 + q
                            m.tr(pb[:, q * 128:(q + 1) * 128], outb[:, tt_ * 128:(tt_ + 1) * 128], identb, r=['outb', 'identb'], w=[('ps', b)])
                        m.cp(('act', 'dve')[g % 2], xtok[:, g * 1024:(g + 1) * 1024], pb, r=[('ps', b)], w=['xtok'])
                    m.dma(tokm["sx"][:, j * 128:(j + 1) * 128].rearrange("(t p) n -> p t n", p=128),
                          xtok.rearrange("p (t n) -> p t n", n=128), r=['xtok'], slot='misc7')
        m.barrier()
        if stop_after == ('ssdpre', l):
            break

        m.aoff = persist_end
        O_all = m.allocb(NT * 512).rearrange("p (t n) -> p t n", n=512)
        qkraw = m.alloc(4 * S // 2)
        qk = [[qkraw[:, (2 * s_ + i_) * 2048:(2 * s_ + i_ + 1) * 2048].bitcast(BF16) for i_ in range(2)] for s_ in range(2)]
        rowb = m.allocb(S)
        Ei = m.alloc(512)
        Ef = m.alloc(512)
        mix_base = m.aoff
        m.op('pool', lambda e: e.iota(Ei.bitcast(mybir.dt.int32), pattern=[[1, 512]], base=0, channel_multiplier=-1), w=['Ei'])
        m.cp('dve', Ef, Ei.bitcast(mybir.dt.int32), r=['Ei'], w=['Ef'])
        qki = [0]

        def load_qk(qname, qrow, kname, krow):
            s_ = qki[0] % 2
            qki[0] += 1
            m.dma(qk[s_][0][0:64, :], qT[qname][qrow:qrow + 64, :], w=[('qk', s_)], slot=('qk', s_, 0))
            m.dma(qk[s_][1][0:64, :], qT[kname][krow:krow + 64, :], r=[('qk', s_)], w=[('qk', s_)], slot=('qk', s_, 1))
            return qk[s_][0][0:64, :], qk[s_][1][0:64, :], ('qk', s_)

        def finalize_branch(n):
            for c in range(4):
                for g in range(4):
                    b = m.bank()
                    pb = m.ps[:, b, :].bitcast(BF16)
                    for q in range(8):
                        tt_ = g * 8 + q
                        m.tr(pb[:, q * 128:(q + 1) * 128], O_all[:, tt_, c * 128:(c + 1) * 128], identb, r=[('O', tt_), 'identb'], w=[('ps', b)])
                    m.cp(('act', 'dve')[g % 2], rowb[:, g * 1024:(g + 1) * 1024], pb, r=[('ps', b)], w=['rowb'])
                m.dma(oT[n][c * 128:(c + 1) * 128, :], rowb, r=['rowb'], slot='rowb')
            m.barrier()

        def mk_decay(dst_full, dst_mask, coef, width, strict=False):
            m.act(dst_full[:, 0:width], Ef[:, 0:width], AF.Exp, r=['Ef'], w=['dfull'], scale=coef)
            m.asel(dst_mask[:, 0:width], dst_full[:, 0:width], [[1, width]], ALU.is_ge, 0.0, -1 if strict else 0, -1, r=['dfull'], w=['dmask'])

        def diag_mask(ap, r, w, strict):
            m.asel(ap, ap, [[1, 128]], ALU.is_ge, 0.0, -1 if strict else 0, -1, r=r, w=w)

        if 'a' in MIX:
            m.aoff = mix_base
            V = m.allocb(NT * 520).rearrange("p (t h e) -> p t h e", h=8, e=65)
            m.dma(V.rearrange("p t h e -> p t (h e)"), mv65.rearrange("(t p) n -> p t n", p=128), w=['V'], slot='V')
            Eiw = m.alloc(4352)
            Efw = m.alloc(4352)
            m.op('pool', lambda e: e.iota(Eiw.bitcast(mybir.dt.int32), pattern=[[1, 4352]], base=0, channel_multiplier=-1), w=['Eiw'])
            m.cp('dve', Efw, Eiw.bitcast(mybir.dt.int32), r=['Eiw'], w=['Efw'])
            ksum = m.alloc(16)
            ktmp = m.alloc(16)
            khl = m.allocb(32)
            gate = m.alloc(512)
            sel = m.alloc(512)
            m8 = m.alloc(8)
            accs = [m.alloc(130) for _ in range(4)]
            rec = m.alloc(4)
            pes = [m.alloc(256) for _ in range(4)]
            pts = [m.allocb(256) for _ in range(6)]
            cnt = 0
            for h in range(8):
                slope = 2.0 ** (-(h + 1))
                qh, kh, qkk = load_qk("mqT", h * 64, "mkT", h * 64)
                m.op('dve', lambda e, kh=kh: e.tensor_reduce(out=ksum[0:64, :], in_=kh.rearrange("p (n j) -> p n j", j=256), axis=mybir.AxisListType.X, op=ALU.add),
                     r=[qkk], w=['ksum'])
                m.cp('dve', khl[0:64, 0:16], ksum[0:64, :], r=['ksum'], w=['khl'])
                m.tt('dve', ktmp[0:64, :], ksum[0:64, :], khl[0:64, 0:16], ALU.subtract, r=['ksum', 'khl'], w=['ktmp'])
                m.cp('dve', khl[0:64, 16:32], ktmp[0:64, :], r=['ktmp'], w=['khl'])
                b = m.bank()
                for tt_ in range(NT):
                    m.mm(m.ps[:, b, tt_ * 16:(tt_ + 1) * 16], qh[:, tt_ * 128:(tt_ + 1) * 128], khl[0:64, 0:16], True, False, r=[qkk, 'khl'], w=[('ps', b)])
                    m.mm(m.ps[:, b, tt_ * 16:(tt_ + 1) * 16], qh[:, tt_ * 128:(tt_ + 1) * 128], khl[0:64, 16:32], False, True, r=[qkk, 'khl'], w=[('ps', b)])
                m.cp('dve', gate, m.ps[:, b, :], r=[('ps', b)], w=['gate'])
                m.asel(gate.rearrange("p (t n) -> p t n", n=16), gate.rearrange("p (t n) -> p t n", n=16), [[1, 32], [-2, 16]], ALU.is_ge, -1e30, -2, 0,
                       r=['gate'], w=['gate'])
                for tt_ in range(NT):
                    own = tt_ // 2
                    g_ = gate[:, tt_ * 16:(tt_ + 1) * 16]
                    if own >= 4:
                        m.op('dve', lambda e, g_=g_: e.max(out=m8, in_=g_), r=['gate'], w=['m8'])
                        m.ts('dve', sel[:, tt_ * 16:(tt_ + 1) * 16], g_, m8[:, 2:3], ALU.is_ge, r=['gate', 'm8'], w=['sel'])
                    else:
                        m.ts('dve', sel[:, tt_ * 16:(tt_ + 1) * 16], g_, -1e29, ALU.is_gt, r=['gate'], w=['sel'])
                pend = [None]

                def flush():
                    if pend[0] is not None:
                        pend[0]()
                        pend[0] = None
                for qb in range(16):
                    q0 = qb * 256
                    acq = accs[(qb % 2) * 2:(qb % 2) * 2 + 2]
                    for a_ in acq:
                        m.memset('pool', a_, 0.0, w=[('acc', id(a_))])
                    for n in range(qb + 1):
                        diag = (n == qb)
                        ptl = []
                        for kt2 in range(2):
                            kt = 2 * n + kt2
                            c0 = 128 if (diag and kt2 == 1) else 0
                            b = m.bank()
                            m.mm(m.ps[:, b, c0:256], kh[:, kt * 128:(kt + 1) * 128], qh[:, q0 + c0:q0 + 256], True, True, r=[qkk], w=[('ps', b)])
                            pe_ = pes[cnt % 4]
                            pek = ('pe', cnt % 4)
                            pt = pts[cnt % 6]
                            ptk = ('pt', cnt % 6)
                            cnt += 1
                            off = q0 - kt * 128
                            m.stt(pe_[:, c0:256], Efw[:, off + c0:off + 256], -8.0 * slope, m.ps[:, b, c0:256], ALU.mult, ALU.add, r=[('ps', b), 'Efw'], w=[pek])
                            m.act(pt[:, c0:256], pe_[:, c0:256], AF.Exp, r=[pek], w=[ptk], scale=0.125)
                            if diag:
                                diag_mask(pt[:, c0:c0 + 128], r=[ptk], w=[ptk], strict=False)
                            ptl.append((pt, ptk, kt, c0))

                        def back(ptl=ptl, qb=qb, n=n, diag=diag, acq=acq):
                            for qt2 in range(2):
                                tt_ = 2 * qb + qt2
                                use = [(pt, ptk, kt) for (pt, ptk, kt, c0) in ptl if c0 <= qt2 * 128]
                                b = m.bank()
                                for i_, (pt, ptk, kt) in enumerate(use):
                                    m.mm(m.ps[:, b, 0:65], pt[:, qt2 * 128:(qt2 + 1) * 128], V[:, kt, h, :], i_ == 0, i_ == len(use) - 1,
                                         r=[ptk, 'V'], w=[('ps', b)])
                                ak = ('acc', id(acq[qt2]))
                                scal = 1.0 if diag else sel[:, tt_ * 16 + n:tt_ * 16 + n + 1]
                                m.stt(acq[qt2][:, 0:65], m.ps[:, b, 0:65], scal, acq[qt2][:, 0:65], ALU.mult, ALU.add, r=[('ps', b), 'sel', ak], w=[ak])
                            if diag:
                                for qt2 in range(2):
                                    tt_ = 2 * qb + qt2
                                    ak = ('acc', id(acq[qt2]))
                                    rc = rec[:, (qb % 2) * 2 + qt2:(qb % 2) * 2 + qt2 + 1]
                                    rk_ = ('rec', (qb % 2) * 2 + qt2)
                                    m.op('dve', lambda e, rc=rc, a_=acq[qt2]: e.reciprocal(out=rc, in_=a_[:, 64:65]), r=[ak], w=[rk_])
                                    m.ts('dve', O_all[:, tt_, h * 64:(h + 1) * 64], acq[qt2][:, 0:64], rc, ALU.mult, r=[ak, rk_], w=[('O', tt_)])
                        prev = pend[0]
                        pend[0] = back
                        if prev is not None:
                            prev()
                flush()
            finalize_branch(0)

        if 'b' in MIX:
            m.aoff = mix_base
            V = m.allocb(NT * 512).rearrange("p (t n) -> p t n", n=512)
            G = m.allocb(NT * 512).rearrange("p (t n) -> p t n", n=512)
            m.dma(V, tokm["rv"].rearrange("(t p) n -> p t n", p=128), w=['V'], slot='V')
            m.dma(G, tokm["rg"].rearrange("(t p) n -> p t n", p=128), w=['G'], slot='G')
            gng = m.alloc(512)
            gnb = m.alloc(512)
            m.dma(gng, IN("ret_gn_g")[l].partition_broadcast(128), w=['gng'], slot='misc0')
            m.dma(gnb, IN("ret_gn_b")[l].partition_broadcast(128), w=['gnb'], slot='misc1')
            Dfull = m.alloc(512)
            Dmask = m.alloc(512)
            pts = [m.allocb(512) for _ in range(4)]
            tmpo = [m.alloc(128) for _ in range(2)]
            tmps = [m.alloc(128) for _ in range(2)]
            cnt = 0
            ecnt = 0
            rqb = 0
            for h in range(4):
                lg = math.log(1.0 - 2.0 ** (-5.0 - h))
                qh, kh, qkk = load_qk("rqkT", h * 64, "rqkT", 256 + h * 64)
                mk_decay(Dfull, Dmask, lg, 512)
                pend = [None]
                for qb in range(8):
                    q0 = qb * 512
                    bo = 6 + (rqb % 2)
                    rqb += 1
                    nk = 4 * qb + 4
                    for kt in range(nk):
                        mdg = kt - 4 * qb
                        c0 = 128 * max(mdg, 0)
                        b = m.bank6()
                        m.mm(m.ps[:, b, c0:512], kh[:, kt * 128:(kt + 1) * 128], qh[:, q0 + c0:q0 + 512], True, True, r=[qkk], w=[('ps', b)])
                        pt = pts[cnt % 4]
                        ptk = ('pt', cnt % 4)
                        cnt += 1
                        if mdg >= 0:
                            m.stt(pt[:, c0:512], m.ps[:, b, c0:512], 0.125, Dmask[:, 0:512 - c0], ALU.mult, ALU.mult, r=[('ps', b), 'dmask'], w=[ptk])
                        else:
                            sc = 0.125 * math.exp(lg * (q0 - kt * 128))
                            m.stt(pt, m.ps[:, b, :], sc, Dfull, ALU.mult, ALU.mult, r=[('ps', b), 'dfull'], w=[ptk])

                        def back(pt=pt, ptk=ptk, kt=kt, mdg=mdg, bo=bo, qb=qb, last=(kt == nk - 1), h=h):
                            nonlocal ecnt
                            for qt in range(max(mdg, 0), 4):
                                m.mm(m.ps[:, bo, qt * 128:(qt + 1) * 128], pt[:, qt * 128:(qt + 1) * 128], V[:, kt, h * 128:(h + 1) * 128], (kt == 0 and qt == 0), False,
                                     r=[ptk, 'V'], w=[('ps', bo)])
                            if not last:
                                return
                            for qt in range(4):
                                tt_ = 4 * qb + qt
                                e_ = ecnt % 2
                                ecnt += 1
                                sm = small[:, 64 + e_ * 32:64 + (e_ + 1) * 32]
                                smk = ('sm', e_)
                                o_ = m.ps[:, bo, qt * 128:(qt + 1) * 128]
                                m.op('dve', lambda e, sm=sm, o_=o_: e.bn_stats(out=sm[:, 0:6], in_=o_), r=[('ps', bo)], w=[smk])
                                m.op('dve', lambda e, sm=sm: e.bn_aggr(out=sm[:, 6:8], in_=sm[:, 0:6]), r=[smk], w=[smk])
                                m.ts('dve', sm[:, 8:9], sm[:, 7:8], NORM_EPS, ALU.add, r=[smk], w=[smk])
                                m.act(sm[:, 9:10], sm[:, 8:9], AF.Sqrt, r=[smk], w=[smk])
                                m.op('dve', lambda e, sm=sm: e.reciprocal(out=sm[:, 10:11], in_=sm[:, 9:10]), r=[smk], w=[smk])
                                to = tmpo[e_]
                                tk = ('tmpo', e_)
                                m.ts('dve', to, o_, sm[:, 6:7], ALU.subtract, r=[('ps', bo), smk], w=[tk], s2=sm[:, 10:11], op1=ALU.mult)
                                m.tt('pool', to, to, gng[:, h * 128:(h + 1) * 128], ALU.mult, r=[tk, 'gng'], w=[tk])
                                m.tt('pool', to, to, gnb[:, h * 128:(h + 1) * 128], ALU.add, r=[tk, 'gnb'], w=[tk])
                                m.act(tmps[e_], G[:, tt_, h * 128:(h + 1) * 128], AF.Silu, r=['G'], w=[('tmps', e_)])
                                m.tt('pool', O_all[:, tt_, h * 128:(h + 1) * 128], to, tmps[e_], ALU.mult, r=[tk, ('tmps', e_)], w=[('O', tt_)])
                        prev = pend[0]
                        pend[0] = back
                        if prev is not None:
                            prev()
                pend[0]()
                pend[0] = None
            finalize_branch(1)

        if 'c' in MIX:
            m.aoff = mix_base
            X = m.allocb(NT * 512).rearrange("p (t n) -> p t n", n=512)
            Z = m.allocb(NT * 512).rearrange("p (t n) -> p t n", n=512)
            m.dma(X, tokm["sx"].rearrange("(t p) n -> p t n", p=128), w=['X'], slot='V')
            m.dma(Z, tokm["sz"].rearrange("(t p) n -> p t n", p=128), w=['Z'], slot='G')
            sdt = m.alloc(512)
            m.dma(sdt, sdtok, w=['sdt'], slot='misc0')
            dsk = m.alloc(8)
            m.dma(dsk, IN("d_skip")[l].partition_broadcast(128), w=['dsk'], slot='misc1')
            ngb = m.alloc(512)
            m.dma(ngb, IN("ssm_norm_g")[l].partition_broadcast(128), w=['ngb'], slot='misc2')
            BT = qk[0][0]
            CT = qk[0][1]
            Gbc = qkraw[:, 4096:8192]
            xdt = m.allocb(NT * 64).rearrange("p (t n) -> p t n", n=64)
            ss = m.alloc(NT * 8)
            rstd = m.alloc(NT * 2)
            Ls = [m.alloc(512) for _ in range(3)]
            wts = [m.allocb(512) for _ in range(4)]
            rqb = 0
            ytmp = [m.alloc(64) for _ in range(2)]
            ztmp = [m.alloc(64) for _ in range(2)]
            junk = m.alloc(64)
            sdt3 = sdt.rearrange("p (t e) -> p t e", e=16)
            cnt = 0
            ecnt = 0
            for g in range(2):
                m.dma(BT, qT["sBCT"][g * 128:(g + 1) * 128, :], w=['BT'], slot='misc3')
                m.dma(CT, qT["sBCT"][256 + g * 128:256 + (g + 1) * 128, :], w=['CT'], slot='misc4')
                for r_ in range(4):
                    h = 4 * g + r_
                    m.dma(Gbc, scumT[h:h + 1, :].partition_broadcast(128), w=['Gbc'], slot='misc5')
                    for tt_ in range(NT):
                        m.ts('pool', xdt[:, tt_, :], X[:, tt_, h * 64:(h + 1) * 64], sdt3[:, tt_, h:h + 1], ALU.mult, r=['X', 'sdt'], w=['xdt'])
                    pend = [None]
                    for tb in range(8):
                        t0 = tb * 512
                        by = 6 + (rqb % 2)
                        rqb += 1
                        ns_ = 4 * tb + 4
                        for st in range(ns_):
                            mdg = st - 4 * tb
                            c0 = 128 * max(mdg, 0)
                            b = m.bank6()
                            m.mm(m.ps[:, b, c0:512], BT[:, st * 128:(st + 1) * 128], CT[:, t0 + c0:t0 + 512], True, True, r=['BT', 'CT'], w=[('ps', b)])
                            L = Ls[cnt % 3]
                            lk = ('L', cnt % 3)
                            wt = wts[cnt % 4]
                            wk_ = ('wt', cnt % 4)
                            cnt += 1
                            m.act(L[:, c0:512], Gbc[:, t0 + c0:t0 + 512], AF.Exp, r=['Gbc', 'sdt'], w=[lk], bias=sdt3[:, st, 8 + h:9 + h])
                            if mdg >= 0:
                                diag_mask(L[:, c0:c0 + 128], r=[lk], w=[lk], strict=False)
                            m.tt('dve', wt[:, c0:512], L[:, c0:512], m.ps[:, b, c0:512], ALU.mult, r=[lk, ('ps', b)], w=[wk_])

                            def back(wt=wt, wk_=wk_, st=st, mdg=mdg, by=by, tb=tb, last=(st == ns_ - 1), h=h):
                                nonlocal ecnt
                                for qt in range(max(mdg, 0), 4):
                                    m.mm(m.ps[:, by, qt * 64:(qt + 1) * 64], wt[:, qt * 128:(qt + 1) * 128], xdt[:, st, :], (st == 0 and qt == 0), False,
                                         r=[wk_, 'xdt'], w=[('ps', by)])
                                if not last:
                                    return
                                for qt in range(4):
                                    tt_ = 4 * tb + qt
                                    e_ = ecnt % 2
                                    ecnt += 1
                                    yk = ('ytmp', e_)
                                    m.stt(ytmp[e_], X[:, tt_, h * 64:(h + 1) * 64], dsk[:, h:h + 1], m.ps[:, by, qt * 64:(qt + 1) * 64], ALU.mult, ALU.add,
                                          r=['X', 'dsk', ('ps', by)], w=[yk])
                                    m.act(ztmp[e_], Z[:, tt_, h * 64:(h + 1) * 64], AF.Silu, r=['Z'], w=[('ztmp', e_)])
                                    m.tt('dve', ytmp[e_], ytmp[e_], ztmp[e_], ALU.mult, r=[yk, ('ztmp', e_)], w=[yk])
                                    m.op('act', lambda e, e_=e_, tt_=tt_, h=h: e.activation(out=junk, in_=ytmp[e_], func=AF.Square, accum_out=ss[:, tt_ * 8 + h:tt_ * 8 + h + 1]),
                                         r=[yk], w=['junk', ('ss', tt_)])
                                    m.cp('pool', O_all[:, tt_, h * 64:(h + 1) * 64], ytmp[e_], r=[yk], w=[('O', tt_)])
                            prev = pend[0]
                            pend[0] = back
                            if prev is not None:
                                prev()
                    pend[0]()
                    pend[0] = None
                ssv = ss.rearrange("p (t e) -> p t e", e=8)[:, :, 4 * g:4 * g + 4]
                rg_ = rstd[:, g * NT:(g + 1) * NT]
                m.op('dve', lambda e, ssv=ssv, rg_=rg_: e.tensor_reduce(out=rg_, in_=ssv, axis=mybir.AxisListType.X, op=ALU.add),
                     r=[('ss', t) for t in range(NT)], w=[('rstd', g)])
                m.ts('dve', rg_, rg_, 1.0 / 256.0, ALU.mult, r=[('rstd', g)], w=[('rstd', g)], s2=LN_EPS, op1=ALU.add)
                m.act(rg_, rg_, AF.Sqrt, r=[('rstd', g)], w=[('rstd', g)])
                m.op('dve', lambda e, rg_=rg_: e.reciprocal(out=rg_, in_=rg_), r=[('rstd', g)], w=[('rstd', g)])
                for tt_ in range(NT):
                    o_ = O_all[:, tt_, g * 256:(g + 1) * 256]
                    m.stt(o_, o_, rg_[:, tt_:tt_ + 1], ngb[:, g * 256:(g + 1) * 256], ALU.mult, ALU.mult, r=[('O', tt_), ('rstd', g), 'ngb'], w=[('O', tt_)])
            finalize_branch(2)

        if 'd' in MIX:
            m.aoff = mix_base
            V = m.allocb(NT * 512).rearrange("p (t n) -> p t n", n=512)
            m.dma(V, tokm["bv"].rearrange("(t p) n -> p t n", p=128), w=['V'], slot='V')
            triU = m.allocb(128)
            triL = m.allocb(128)
            onesb = m.allocb(128)
            m.memset('pool', onesb, 1.0, w=['onesb'])
            m.asel(triU, onesb, [[-1, 128]], ALU.is_ge, 0.0, -1, 1, r=['onesb'], w=['triU'])
            m.asel(triL, onesb, [[1, 128]], ALU.is_ge, 0.0, 0, -1, r=['onesb'], w=['triL'])
            NS = 3
            bufs = []
            for hd in range(2):
                bufs.append(([m.alloc(512) for _ in range(NS)], [m.allocb(512) for _ in range(NS)],
                             [m.alloc(512) for _ in range(NS)], [m.allocb(512) for _ in range(NS)]))
            cntA = [0, 0]
            zc = [0, 0]

            def sb_head_qb(hd, h, qb, qh, kh, qkk):
                q0 = qb * 512
                kts = list(range(4 * qb + 3, -1, -1))
                n_ = len(kts)
                BACC, BO = 4 + hd, 6 + hd
                es_, spb_, ts__, wt_ = bufs[hd]
                info = {}

                def A(j):
                    kt = kts[j]
                    i = cntA[hd] % NS
                    cntA[hd] += 1
                    mdg = kt - 4 * qb
                    c0 = 128 * max(mdg, 0)
                    b = 2 * hd + (zc[hd] % 2)
                    zc[hd] += 1
                    ek, sk, tk, = ('es', hd, i), ('spb', hd, i), ('ts', hd, i)
                    m.mm(m.ps[:, b, c0:512], kh[:, kt * 128:(kt + 1) * 128], qh[:, q0 + c0:q0 + 512], True, True, r=[qkk], w=[('ps', b)])
                    m.act(es_[i][:, c0:512], m.ps[:, b, c0:512], AF.Exp, r=[('ps', b)], w=[ek], scale=0.125)
                    m.act(spb_[i][:, c0:512], es_[i][:, c0:512], AF.Ln, r=[ek], w=[sk], bias=1.0)
                    info[j] = (i, c0, mdg, kt, b)

                def A2(j):
                    i, c0, mdg, kt, b = info[j]
                    sk, tk = ('spb', hd, i), ('ts', hd, i)
                    m.stt(ts__[i][:, c0:512], m.ps[:, b, c0:512], 0.125, spb_[i][:, c0:512], ALU.mult, ALU.subtract, r=[('ps', b), sk], w=[tk])
                    if mdg >= 0:
                        diag_mask(spb_[i][:, c0:c0 + 128], r=[sk], w=[sk], strict=True)

                def B1(j):
                    i, c0, mdg, kt, b = info[j]
                    sk = ('spb', hd, i)
                    m.mm(m.ps[:, BACC, c0:512], triU, spb_[i][:, c0:512], j == 0, False, r=['triU', sk], w=[('ps', BACC)])

                def B2(j):
                    i, c0, mdg, kt, b = info[j]
                    tk = ('ts', hd, i)
                    m.tt('dve', ts__[i][:, c0:512], ts__[i][:, c0:512], m.ps[:, BACC, c0:512], ALU.subtract, r=[tk, ('ps', BACC)], w=[tk])

                def B3(j):
                    i, c0, mdg, kt, b = info[j]
                    sk = ('spb', hd, i)
                    m.mm(m.ps[:, BACC, c0:512], triL, spb_[i][:, c0:512], False, False, r=['triL', sk], w=[('ps', BACC)])

                def B4(j):
                    i, c0, mdg, kt, b = info[j]
                    tk, wk_ = ('ts', hd, i), ('wt', hd, i)
                    m.act(wt_[i][:, c0:512], ts__[i][:, c0:512], AF.Exp, r=[tk], w=[wk_])
                    if mdg >= 0:
                        diag_mask(wt_[i][:, c0:c0 + 128], r=[wk_], w=[wk_], strict=True)

                def C(j):
                    i, c0, mdg, kt, b = info[j]
                    wk_ = ('wt', hd, i)
                    for qt in range(max(mdg, 0), 4):
                        m.mm(m.ps[:, BO, qt * 64:(qt + 1) * 64], wt_[i][:, qt * 128:(qt + 1) * 128], V[:, kt, h * 64:(h + 1) * 64], j == 0 and qt == max(mdg, 0), False,
                             r=[wk_, 'V'], w=[('ps', BO)])

                def E():
                    for qt in range(4):
                        tt_ = 4 * qb + qt
                        m.cp('act', O_all[:, tt_, h * 64:(h + 1) * 64], m.ps[:, BO, qt * 64:(qt + 1) * 64], r=[('ps', BO)], w=[('O', tt_)])
                return dict(n=n_, A1=A, A2=A2, B1=B1, B2=B2, B3=B3, B4=B4, C=C, E=E)
            import os
            for hp in range(int(os.environ.get('SB_H', 8)) // 2):
                hs = (2 * hp, 2 * hp + 1)
                lq = [load_qk("bqT", h_ * 64, "bkT", h_ * 64) for h_ in hs]
                for qb in range(int(os.environ.get('SB_QB', 8))):
                    S_ = [sb_head_qb(hd, hs[hd], qb, lq[hd][0], lq[hd][1], lq[hd][2]) for hd in range(2)]
                    n_ = S_[0]['n']
                    for st_name in ('A1', 'A2'):
                        for hd in range(2):
                            S_[hd][st_name](0)
                    for j in range(n_):
                        if j + 1 < n_:
                            for hd in range(2):
                                S_[hd]['A1'](j + 1)
                        for st_name in ('B1', 'B2', 'B3', 'B4'):
                            for hd in range(2):
                                S_[hd][st_name](j)
                        if j + 1 < n_:
                            for hd in range(2):
                                S_[hd]['A2'](j + 1)
                        if j >= 1:
                            for hd in range(2):
                                S_[hd]['C'](j - 1)
                    for hd in range(2):
                        S_[hd]['C'](n_ - 1)
                        S_[hd]['E']()
            finalize_branch(3)
        m.barrier()
        if stop_after == ('mix', l):
            break

        m.aoff = persist_end
        hT = _r3(m.allocb(8 * S), 8)
        p1_end = m.aoff
        xts = [m.alloc(D) for _ in range(3)]
        make_hT(hT, x_src, scp[:, 0:8], modT[:, 0:8], range(NT))
        m.barrier()
        m.aoff = p1_end
        wst4 = [m.alloc(8 * 512)] * 2
        wg4 = [m.allocb(8 * 512) for _ in range(2)]
        wb4 = [m.allocb(4 * 512) for _ in range(2)]
        obs = [m.allocb(16 * 512) for _ in range(2)]
        rowbs = [m.allocb(S) for _ in range(2)]
        sigs = [m.alloc(512) for _ in range(2)]
        accs4 = [m.alloc(512) for _ in range(2)]
        tmps4 = [m.alloc(512) for _ in range(2)]
        oc = 0
        sc_ = 0
        for dc in range(8):
            i = dc % 2
            st_g = wst4[0].rearrange("p (c n j) -> p c n j", c=8, n=4)
            for n in range(4):
                c0 = 6152 + n * 1024 + dc * 128
                m.dma(st_g[:, :, n, :], IN("w_in")[l][:, c0:c0 + 128].rearrange("(c p) j -> p c j", p=128), w=[('wst4', 0)], r=[('wst4', 0)], slot=('wst4', 0, n))
            wg = wg4[i].rearrange("p (c n j) -> p c n j", c=8, n=4)
            m.cp('pool', wg4[i], wst4[i], r=[('wst4', 0)], w=[('wg4', i)])
            st_b = wst4[i][:, 0:2048].rearrange("p (n c j) -> p n c j", n=4, c=4)
            for n in range(4):
                m.dma(st_b[:, n, :, :], IN("w_br")[l][n][:, dc * 128:(dc + 1) * 128].rearrange("(c p) j -> p c j", p=128), w=[('wst4', 0)], r=[('wst4', 0)], slot=('wst4', 0, n))
            wbr = wb4[i].rearrange("p (n c j) -> p n c j", n=4, c=4)
            m.cp('pool', wb4[i], wst4[i][:, 0:2048], r=[('wst4', 0)], w=[('wb4', i)])
            rb = rowbs[dc % 2]
            rk = ('rowb4', dc % 2)
            for tb in range(8):
                o_i = oc % 2
                oc += 1
                ob = obs[o_i].rearrange("p (n c t) -> p n c t", n=4, c=4)
                for n in range(4):
                    m.dma(ob[:, n, :, :], oT[n][:, tb * 512:(tb + 1) * 512].rearrange("(c p) t -> p c t", p=128), w=[('ob', o_i, n)], slot=('ob', o_i, n))
                a_i = tb % 2
                for n in range(4):
                    bg = m.bank()
                    for k in range(8):
                        m.mm(m.ps[:, bg, :], wg[:, k, n, :], hT[:, k, tb * 512:(tb + 1) * 512], k == 0, k == 7, r=[('wg4', i)] + HK[tb * 4:tb * 4 + 4], w=[('ps', bg)])
                    by = m.bank()
                    for c in range(4):
                        m.mm(m.ps[:, by, :], wbr[:, n, c, :], ob[:, n, c, :], c == 0, c == 3, r=[('wb4', i), ('ob', o_i, n)], w=[('ps', by)])
                    s_i = sc_ % 2
                    sc_ += 1
                    m.act(sigs[s_i], m.ps[:, bg, :], AF.Sigmoid, r=[('ps', bg)], w=[('sig', s_i)])
                    if n == 0:
                        m.tt('dve', accs4[a_i], sigs[s_i], m.ps[:, by, :], ALU.mult, r=[('sig', s_i), ('ps', by)], w=[('acc4', a_i)])
                    else:
                        m.tt('dve', tmps4[s_i], sigs[s_i], m.ps[:, by, :], ALU.mult, r=[('sig', s_i), ('ps', by)], w=[('tmp4', s_i)])
                        if n < 3:
                            m.tt('pool', accs4[a_i], accs4[a_i], tmps4[s_i], ALU.add, r=[('acc4', a_i), ('tmp4', s_i)], w=[('acc4', a_i)])
                        else:
                            m.tt('pool', rb[:, tb * 512:(tb + 1) * 512], accs4[a_i], tmps4[s_i], ALU.add, r=[('acc4', a_i), ('tmp4', s_i)], w=[rk])
            m.dma(mergedT[dc * 128:(dc + 1) * 128, :], rb, r=[rk], slot=rk)
        m.barrier()
        if stop_after == ('merge', l):
            break

        m.aoff = persist_end
        m.init_wbufs(2, 8 * 512)
        wo = [m.load_w(IN("w_out")[l][:, hh * 512:(hh + 1) * 512], 8, 512) for hh in range(2)]
        mts = [m.allocb(8 * 128) for _ in range(2)]
        xts = [m.alloc(D) for _ in range(3)]
        tmp5 = [m.alloc(D) for _ in range(2)]
        out5 = [m.alloc(D) for _ in range(2)]
        for tt_ in range(NT):
            i = tt_ % 2
            mt = _r3(mts[i], 8)
            m.dma(mt, mergedT[:, tt_ * 128:(tt_ + 1) * 128].rearrange("(c p) t -> p c t", p=128), w=[('mt', i)], slot=('mt', i))
            xs = tt_ % 3
            m.dma(xts[xs], x_src[tt_ * 128:(tt_ + 1) * 128, :], w=[('xt', xs)], slot=('xt', xs))
            b2 = m.bank2()
            for hh in range(2):
                for k in range(8):
                    m.mm(m.ps[:, b2 + hh, :], mt[:, k, :], wo[hh][0][:, k, :], k == 0, k == 7, r=[('mt', i), wo[hh][1]], w=[('ps', b2 + hh)])
            ps2 = (m.ps[:, b2:b2 + 2, :].rearrange("p a n -> p (a n)"), [('ps', b2), ('ps', b2 + 1)])
            ln_epilogue(ps2, xts[xs], ('xt', xs), gbc[:, 0:D], lnbc[:, 0:D], lnbc[:, D:2 * D], out5[i], ('out5', i), tmp5[i], ('tmp5', i), i)
            m.dma(x1_d[tt_ * 128:(tt_ + 1) * 128, :], out5[i], r=[('out5', i)], slot=('out5', i))
        m.barrier()
        if stop_after == ('ln1', l):
            break

        m.aoff = persist_end
        moe = (l % 2 == 1)
        TBK = 1024
        h2T = _r3(m.allocb(8 * TBK), 8)
        yacc = _r3(m.alloc(8 * TBK), 8)
        aTs = [m.allocb(4 * TBK).rearrange("p (c t) -> p c t", c=4) for _ in range(2)]
        wst6 = [m.alloc(8 * 512) for _ in range(2)]
        wbf6 = [m.allocb(8 * 512) for _ in range(4)]
        xts = [m.alloc(D) for _ in range(2)]
        tmp6 = [m.alloc(D) for _ in range(1)]
        out6 = [m.alloc(D) for _ in range(2)]
        sil = [m.alloc(512) for _ in range(2)]
        tmy = [m.alloc(512) for _ in range(2)]
        wsi = [0]
        wbi = [0]

        def load6(src_r3, kc, n):
            si = wsi[0] % 2
            wsi[0] += 1
            bi = wbi[0] % 4
            wbi[0] += 1
            st = _r3(wst6[si][:, 0:kc * n], kc)
            m.dma(st, src_r3, w=[('wst6', si)], slot=('wst6', si))
            bf = _r3(wbf6[bi][:, 0:kc * n], kc)
            m.cp('act', bf, st, r=[('wst6', si)], w=[('wbf6', bi)])
            return bf, ('wbf6', bi)
        if moe:
            rw = m.alloc(64)
            rbb = m.alloc(8)
            selE = m.alloc(8 * 128)
            combT = m.alloc(TBK)
            cbt_ = m.alloc(TBK)
            hf = [m.alloc(8 * 128)] * 2
            rs = m.alloc(64)
            cpad = m.alloc(128)
            m.memset('pool', cpad, 0.0, w=['cpad'])
            m.dma(_r3(rw, 8), IN("router_w")[0].rearrange("(c p) e -> p c e", p=128), w=['rw'], slot='misc0')
            m.dma(rbb, IN("router_b")[0].partition_broadcast(128), w=['rbb'], slot='misc1')
            m.memset('pool', selE[0:8, :], 1.0, w=['selE'])
            for e_ in range(8):
                m.ts('dve', selE[0:8, e_ * 128:(e_ + 1) * 128], selE[0:8, e_ * 128:(e_ + 1) * 128], ident[0:8, e_:e_ + 1], ALU.mult, r=['selE', 'ident'], w=['selE'])
            experts = [(IN("expert_w_gu")[0][e], IN("expert_w_down")[0][e], D_FFE, e) for e in range(8)]
        else:
            experts = [(IN("ffn_w_gu")[l // 2], IN("ffn_w_down")[l // 2], D_FF, None)]
        for tbk in range(S // TBK):
            tts = list(range(tbk * 8, tbk * 8 + 8))
            for tt_ in tts:
                s_ = tt_ % 2
                m.dma(xts[s_], x1_d[tt_ * 128:(tt_ + 1) * 128, :], w=[('xt', s_)], slot=('xt', s_))
                b2 = m.bank2()
                for c in range(8):
                    bb, cc = b2 + c // 4, (c % 4) * 128
                    m.tr(m.ps[:, bb, cc:cc + 128], xts[s_][:, c * 128:(c + 1) * 128], ident, r=[('xt', s_), 'ident'], w=[('ps', bb)])
                o = (tt_ - tbk * 8) * 128
                import os
                MA = int(os.environ.get('MOE_A', 9))
                if not moe:
                    for c in range(8):
                        bb, cc = b2 + c // 4, (c % 4) * 128
                        m.act(h2T[:, c, o:o + 128], m.ps[:, bb, cc:cc + 128], AF.Identity, r=[('ps', bb), 'scp', 'modT'],
                              w=[('h2T', tt_ % 8)], scale=scp[:, 8 + c:9 + c], bias=modT[:, 24 + c:25 + c])
                else:
                    hfi = hf[0]
                    hk = ('hf', 0)
                    for c in range(8):
                        bb, cc = b2 + c // 4, (c % 4) * 128
                        m.act(hfi[:, c * 128:(c + 1) * 128], m.ps[:, bb, cc:cc + 128], AF.Identity, r=[('ps', bb), 'scp', 'modT'],
                              w=[hk], scale=scp[:, 8 + c:9 + c], bias=modT[:, 24 + c:25 + c])
                    m.cp('pool', h2T[:, :, o:o + 128], _r3(hfi, 8), r=[hk], w=[('h2T', tt_ % 8)])
                if moe and MA >= 1:
                    bl = m.bank()
                    for c in range(8):
                        m.mm(m.ps[:, bl, 0:8], hfi[:, c * 128:(c + 1) * 128], rw[:, c * 8:(c + 1) * 8], c == 0, c == 7, r=[hk, 'rw'], w=[('ps', bl)])
                    m.tt('dve', rs[:, 0:8], m.ps[:, bl, 0:8], rbb, ALU.add, r=[('ps', bl), 'rbb'], w=['rs'])
                if moe and MA >= 2:
                    m.op('dve', lambda e: e.max(out=rs[:, 8:16], in_=rs[:, 0:8]), r=['rs'], w=['rs'])
                    m.ts('dve', rs[:, 16:24], rs[:, 0:8], rs[:, 9:10], ALU.is_ge, r=['rs'], w=['rs'])
                    m.ts('dve', rs[:, 24:25], rs[:, 8:9], -1.0, ALU.mult, r=['rs'], w=['rs'])
                    m.act(rs[:, 32:40], rs[:, 0:8], AF.Exp, r=['rs'], w=['rs'], bias=rs[:, 24:25])
                    m.act(rs[:, 25:26], rs[:, 9:10], AF.Exp, r=['rs'], w=['rs'], bias=rs[:, 24:25])
                    m.ts('dve', rs[:, 25:26], rs[:, 25:26], 1.0, ALU.add, r=['rs'], w=['rs'])
                    m.op('dve', lambda e: e.reciprocal(out=rs[:, 26:27], in_=rs[:, 25:26]), r=['rs'], w=['rs'])
                    m.tt('dve', rs[:, 40:48], rs[:, 32:40], rs[:, 16:24], ALU.mult, r=['rs'], w=['rs'])
                    m.ts('dve', rs[:, 40:48], rs[:, 40:48], rs[:, 26:27], ALU.mult, r=['rs'], w=['rs'])
                if moe and MA >= 3:
                    bt = m.bank()
                    m.cp('dve', cpad[:, 0:8], rs[:, 40:48], r=['rs'], w=['cpad'])
                    m.tr(m.ps[:, bt, 0:128], cpad, ident, r=['cpad', 'ident'], w=[('ps', bt)])
                    m.cp('dve', combT[0:8, o:o + 128], m.ps[0:8, bt, 0:128], r=[('ps', bt)], w=['combT'])
            import os
            MD = os.environ.get('MOE_DBG', 'ABC')
            m.memset('pool', yacc, 0.0, w=['yacc'])
            H2K = [('h2T', i_) for i_ in range(8)]
            gi = 0
            for (wgu, wdn, F, e) in (experts if 'B' in MD else []):
                if e is not None:
                    for half in range(2):
                        bc_ = m.bank()
                        m.mm(m.ps[:, bc_, :], selE[0:8, e * 128:(e + 1) * 128], combT[0:8, half * 512:(half + 1) * 512], True, True, r=['selE', 'combT'], w=[('ps', bc_)])
                        m.cp('act', cbt_[:, half * 512:(half + 1) * 512], m.ps[:, bc_, :], r=[('ps', bc_)], w=['cbt_'])
                nfc = F // 128
                for f0 in range(0, nfc, 4):
                    ncq = min(4, nfc - f0)
                    ncol = ncq * 128
                    gw, gk = load6(wgu[:, f0 * 128:f0 * 128 + ncol].rearrange("(c p) n -> p c n", p=128), 8, ncol)
                    uw, uk = load6(wgu[:, F + f0 * 128:F + f0 * 128 + ncol].rearrange("(c p) n -> p c n", p=128), 8, ncol)
                    dw, dk = load6(wdn[f0 * 128:f0 * 128 + ncol, :].rearrange("(c p) n -> p c n", p=128), ncq, D)
                    aT = aTs[gi % 2]
                    ak = ('aT', gi % 2)
                    gi += 1
                    for fc in range(ncq):
                        for half in range(2):
                            pg = m.bank()
                            for k in range(8):
                                m.mm(m.ps[:, pg, :], gw[:, k, fc * 128:(fc + 1) * 128], h2T[:, k, half * 512:(half + 1) * 512], k == 0, k == 7,
                                     r=[gk] + H2K[half * 4:half * 4 + 4], w=[('ps', pg)])
                            pu = m.bank()
                            for k in range(8):
                                m.mm(m.ps[:, pu, :], uw[:, k, fc * 128:(fc + 1) * 128], h2T[:, k, half * 512:(half + 1) * 512], k == 0, k == 7,
                                     r=[uk] + H2K[half * 4:half * 4 + 4], w=[('ps', pu)])
                            s_i = (fc * 2 + half) % 2
                            m.act(sil[s_i], m.ps[:, pg, :], AF.Silu, r=[('ps', pg)], w=[('sil', s_i)])
                            m.tt('dve', aT[:, fc, half * 512:(half + 1) * 512], sil[s_i], m.ps[:, pu, :], ALU.mult, r=[('sil', s_i), ('ps', pu)], w=[ak])
                    for dc in range(8):
                        for half in range(2):
                            py = m.bank()
                            for fc in range(ncq):
                                m.mm(m.ps[:, py, :], dw[:, fc, dc * 128:(dc + 1) * 128], aT[:, fc, half * 512:(half + 1) * 512], fc == 0, fc == ncq - 1,
                                     r=[dk, ak], w=[('ps', py)])
                            ya = yacc[:, dc, half * 512:(half + 1) * 512]
                            yk = ('yacc', dc, half)
                            if e is not None:
                                t_i = (dc * 2 + half) % 2
                                m.tt('dve', tmy[t_i], m.ps[:, py, :], cbt_[:, half * 512:(half + 1) * 512], ALU.mult, r=[('ps', py), 'cbt_'], w=[('tmy', t_i)])
                                m.tt('dve', ya, ya, tmy[t_i], ALU.add, r=['yacc', yk, ('tmy', t_i)], w=[yk])
                            else:
                                m.tt('dve', ya, ya, m.ps[:, py, :], ALU.add, r=['yacc', yk, ('ps', py)], w=[yk])
            YK = [('yacc', dc, half) for dc in range(8) for half in range(2)] + ['yacc']
            for tt_ in (tts if 'C' in MD else []):
                s_ = tt_ % 2
                o = (tt_ - tbk * 8) * 128
                m.dma(xts[s_], x1_d[tt_ * 128:(tt_ + 1) * 128, :], w=[('xt', s_)], slot=('xt', s_))
                b2 = m.bank2()
                for c in range(8):
                    bb, cc = b2 + c // 4, (c % 4) * 128
                    m.tr(m.ps[:, bb, cc:cc + 128], yacc[:, c, o:o + 128], ident, r=YK + ['ident'], w=[('ps', bb)])
                ps2 = (m.ps[:, b2:b2 + 2, :].rearrange("p a n -> p (a n)"), [('ps', b2), ('ps', b2 + 1)])
                ln_epilogue(ps2, xts[s_], ('xt', s_), gbc[:, D:2 * D], lnbc[:, 2 * D:3 * D], lnbc[:, 3 * D:4 * D], out6[s_], ('out6', s_), tmp6[0], ('tmp6', 0), s_)
                m.dma(x_dst[tt_ * 128:(tt_ + 1) * 128, :], out6[s_], r=[('out6', s_)], slot=('out6', s_))
        m.barrier()
        if stop_after == ('ffn', l):
            break
    m.barrier()
    nc_ = m.finish()
    nc_.used_inputs = list(_ins.keys())
    return nc_


_CACHE = {}


def _prep_inputs(inputs, b):
    f = lambda a: np.ascontiguousarray(a, dtype=np.float32)
    R = {
        "x": lambda a: a[b],
        "c": lambda a: np.asarray(a[b]).reshape(8, 128).T,
        "b_ada": lambda a: np.asarray(a).reshape(2, 1, 6 * D),
        "conv_w": lambda a: np.asarray(a).reshape(2, 4, 8, 128).transpose(0, 3, 2, 1),
        "conv_b": lambda a: np.asarray(a).reshape(2, 8, 128).transpose(0, 2, 1),
        "dt_bias": lambda a: np.asarray(a).reshape(2, 8, 1),
        "a_log": lambda a: np.asarray(a).reshape(2, 8, 1),
        "d_skip": lambda a: np.asarray(a).reshape(2, 1, 8),
        "ssm_norm_g": lambda a: np.asarray(a).reshape(2, 1, 512),
        "ret_gn_g": lambda a: np.asarray(a).reshape(2, 1, 512),
        "ret_gn_b": lambda a: np.asarray(a).reshape(2, 1, 512),
        "ln1_g": lambda a: np.asarray(a).reshape(2, 1, D),
        "ln1_b": lambda a: np.asarray(a).reshape(2, 1, D),
        "ln2_g": lambda a: np.asarray(a).reshape(2, 1, D),
        "ln2_b": lambda a: np.asarray(a).reshape(2, 1, D),
        "router_b": lambda a: np.asarray(a).reshape(1, 1, 8),
    }
    return {k: f(R[k](v) if k in R else v) for k, v in inputs.items()}


def kernel(**inputs):
    nc = build()
    shared = _prep_inputs(inputs, 0)
    in_maps = []
    for b in range(8):
        d = dict(shared)
        d["x"] = np.ascontiguousarray(inputs["x"][b], dtype=np.float32)
        d["c"] = np.ascontiguousarray(np.asarray(inputs["c"][b]).reshape(8, 128).T, dtype=np.float32)
        in_maps.append(d)
    res = run_bass_kernel_spmd(nc, in_maps, core_ids=list(range(8)))
    return np.stack([np.asarray(r["y"], dtype=np.float32) for r in res.results], axis=0)
```

```python
import contextlib
import math
import numpy as np
import concourse.bass as bass
import concourse.mybir as mybir
from concourse.bass_utils import run_bass_kernel_spmd

F32 = mybir.dt.float32
BF16 = mybir.dt.bfloat16
AF = mybir.ActivationFunctionType
ALU = mybir.AluOpType

S = 4096
D = 1024
NT = 32
IN_W = 10248
ALPHA = 4.0 ** 0.25
LN_EPS = 1e-5
NORM_EPS = 1e-6
D_FF = 2816
D_FFE = 3584


class Builder:
    ENGS = ('pe', 'act', 'dve', 'pool', 'sp')

    def __init__(self):
        self.nc = bass.Bass("TRN2", target_bir_lowering=False)
        self.stack = contextlib.ExitStack()
        self.q = {e: [] for e in self.ENGS}
        self.seq = {e: 0 for e in self.ENGS}
        self.known = {e: {} for e in self.ENGS}
        self.last_w = {}
        self.readers = {}
        self.slots = {}
        self.sems = {}
        for e in self.ENGS:
            self.sems[('e', e)] = self.stack.enter_context(self.nc.semaphore("c_" + e))
        self._uid = 0

    def sbuf(self, shape, dtype, name=None):
        self._uid += 1
        return self.stack.enter_context(self.nc.sbuf_tensor(name or f"sb{self._uid}", list(shape), dtype))

    def psum(self, shape, dtype, name=None):
        self._uid += 1
        return self.stack.enter_context(self.nc.psum_tensor(name or f"ps{self._uid}", list(shape), dtype))

    def dram(self, name, shape, dtype, kind="Internal"):
        return self.nc.dram_tensor(name, list(shape), dtype, kind=kind).ap()

    def _deps(self, eng, r, w, skip_same_w=False):
        deps = {}

        def add(tok):
            s, v = tok
            if deps.get(s, 0) < v:
                deps[s] = v
        for k in r:
            t = self.last_w.get(k)
            if t is not None:
                add(t)
        for k in w:
            t = self.last_w.get(k)
            if t is not None and not (skip_same_w and t[0] == ('e', eng)):
                add(t)
            for t in self.readers.get(k, ()):
                add(t)
        waits = []
        kn = self.known[eng]
        for s, v in deps.items():
            if kn.get(s, 0) >= v:
                continue
            kn[s] = v
            waits.append((s, v))
        return waits

    def _record(self, tok, r, w):
        for k in w:
            self.last_w[k] = tok
            self.readers[k] = []
        for k in r:
            self.readers.setdefault(k, []).append(tok)

    def op(self, eng, fn, r=(), w=()):
        waits = self._deps(eng, r, w, skip_same_w=(eng == 'pe'))
        self.seq[eng] += 1
        tok = (('e', eng), self.seq[eng])
        self.q[eng].append((fn, waits, (('e', eng), 1)))
        self._record(tok, r, w)
        return tok

    def dma(self, out, in_, r=(), w=(), slot=None, q='sp'):
        if slot not in self.slots:
            self.sems[('d', slot)] = self.stack.enter_context(self.nc.semaphore("d%d" % len(self.slots)))
            self.slots[slot] = [('d', slot), 0]
        waits = self._deps(q, r, w)
        ent = self.slots[slot]
        ent[1] += 16
        tok = (ent[0], ent[1])
        self.q[q].append((lambda e, o=out, i=in_: e.dma_start(out=o, in_=i), waits, (ent[0], 16)))
        self._record(tok, r, w)
        return tok

    def barrier(self):
        toks = []
        for e in self.ENGS:
            if self.seq[e] > 0:
                toks.append((('e', e), self.seq[e]))
        for k, ent in self.slots.items():
            if ent[1] > 0:
                toks.append((ent[0], ent[1]))
        for e in self.ENGS:
            kn = self.known[e]
            waits = []
            for s, v in toks:
                if kn.get(s, 0) >= v:
                    continue
                kn[s] = v
                waits.append((s, v))
            if waits:
                self.q[e].append((None, waits, None))
        self.last_w.clear()
        self.readers.clear()

    def finish(self):
        self.barrier()
        nc = self.nc
        sems = self.sems

        def run(eng_obj, lst):
            for fn, waits, inc in lst:
                for s, v in waits:
                    eng_obj.wait_ge(sems[s], v)
                if fn is not None:
                    fn(eng_obj).then_inc(sems[inc[0]], inc[1])
        with nc.Block() as block:
            @block.tensor
            def _(e):
                run(e, self.q['pe'])

            @block.scalar
            def _(e):
                run(e, self.q['act'])

            @block.vector
            def _(e):
                run(e, self.q['dve'])

            @block.gpsimd
            def _(e):
                run(e, self.q['pool'])

            @block.sync
            def _(e):
                run(e, self.q['sp'])
        self.stack.close()
        return nc


def _r3(ap, c):
    return ap.rearrange("p (c n) -> p c n", c=c)


class MK(Builder):
    AW = 50 * 1024

    def __init__(self, dbg=()):
        super().__init__()
        self.dbg = set(dbg)
        self.arena = self.sbuf([128, self.AW], F32, "arena")
        self.aoff = 0
        self.ps = self.psum([128, 8, 512], F32, "psum")
        self.pcur = 0
        self.ktag = 0

    def alloc(self, words):
        a = self.aoff
        self.aoff += words
        assert self.aoff <= self.AW, (self.aoff, self.AW)
        return self.arena[:, a:a + words]

    def allocb(self, n):
        assert n % 2 == 0
        return self.alloc(n // 2).bitcast(BF16)

    def key(self, name):
        self.ktag += 1
        return (name, self.ktag)

    def bank(self):
        i = self.pcur
        self.pcur = (self.pcur + 1) % 8
        return i

    def bank6(self):
        i = getattr(self, 'p6', 0)
        self.p6 = (i + 1) % 6
        return i

    def bank2(self):
        if self.pcur % 2:
            self.pcur = (self.pcur + 1) % 8
        i = self.pcur
        self.pcur = (self.pcur + 2) % 8
        return i

    def scr(self, name, shape, dtype):
        return self.dram(name, shape, dtype, kind="ExternalOutput" if name in self.dbg else "Internal")

    def mm(self, out, lhsT, rhs, start, stop, r, w):
        self.op('pe', lambda e: e.matmul(out, lhsT=lhsT, rhs=rhs, start=start, stop=stop), r=r, w=w)

    def tr(self, out, in_, ident, r, w):
        self.op('pe', lambda e: e.transpose(out=out, in_=in_, identity=ident), r=r, w=w)

    def act(self, out, in_, func, r, w, bias=None, scale=None):
        kw = {}
        if bias is not None:
            kw['bias'] = bias
        if scale is not None:
            kw['scale'] = scale
        self.op('act', lambda e: e.activation(out=out, in_=in_, func=func, **kw), r=r, w=w)

    def tt(self, eng, out, in0, in1, op, r, w):
        self.op(eng, lambda e: e.tensor_tensor(out=out, in0=in0, in1=in1, op=op), r=r, w=w)

    def ts(self, eng, out, in0, s1, op0, r, w, s2=None, op1=None):
        if op1 is None:
            self.op(eng, lambda e: e.tensor_scalar(out=out, in0=in0, scalar1=s1, scalar2=None, op0=op0), r=r, w=w)
        else:
            self.op(eng, lambda e: e.tensor_scalar(out=out, in0=in0, scalar1=s1, scalar2=s2, op0=op0, op1=op1), r=r, w=w)

    def stt(self, out, in0, scalar, in1, op0, op1, r, w):
        self.op('dve', lambda e: e.scalar_tensor_tensor(out=out, in0=in0, scalar=scalar, in1=in1, op0=op0, op1=op1), r=r, w=w)

    def cp(self, eng, out, in_, r, w):
        if eng == 'act':
            self.op('act', lambda e: e.copy(out=out, in_=in_), r=r, w=w)
        else:
            self.op(eng, lambda e: e.tensor_copy(out=out, in_=in_), r=r, w=w)

    def memset(self, eng, ap, val, w):
        self.op(eng, lambda e: e.memset(ap, val), w=w)

    def asel(self, out, in_, pattern, cmp, fill, base, cm, r, w):
        self.op('pool', lambda e: e.affine_select(out=out, in_=in_, pattern=pattern, compare_op=cmp, fill=fill,
                                                  base=base, channel_multiplier=cm), r=r, w=w)

    def init_wbufs(self, nslots, words):
        self.wst = [self.alloc(words) for _ in range(nslots)]
        self.wbf = [self.alloc(words // 2).bitcast(BF16) for _ in range(nslots)]
        self.wi = 0
        self.wn = nslots

    def load_w(self, src, kc, n, cast=True):
        i = self.wi
        self.wi = (self.wi + 1) % self.wn
        st = _r3(self.wst[i][:, 0:kc * n], kc)
        self.dma(st, src.rearrange("(c p) n -> p c n", p=128), w=[('wst', i)], slot=('wst', i))
        if not cast:
            return st, ('wst', i)
        bf = _r3(self.wbf[i][:, 0:kc * n], kc)
        self.cp('pool', bf, st, r=[('wst', i)], w=[('wbf', i)])
        return bf, ('wbf', i)


def build(dbg=(), stop_after=None, layers=(0, 1), MIX='abcd', skip=(), x_first=False):
    m = MK(dbg)
    nc = m.nc

    SHAPES = dict(x=[S, D], c=[128, 8], w_ada=[2, D, 6 * D], b_ada=[2, 1, 6 * D], w_in=[2, D, IN_W],
                  conv_w=[2, 128, 8, 4], conv_b=[2, 128, 8], dt_bias=[2, 8, 1], a_log=[2, 8, 1], d_skip=[2, 1, 8],
                  ssm_norm_g=[2, 1, 512], ret_gn_g=[2, 1, 512], ret_gn_b=[2, 1, 512], w_br=[2, 4, 512, D],
                  w_out=[2, D, D], ln1_g=[2, 1, D], ln1_b=[2, 1, D], ln2_g=[2, 1, D], ln2_b=[2, 1, D],
                  ffn_w_gu=[1, D, 2 * D_FF], ffn_w_down=[1, D_FF, D], router_w=[1, D, 8], router_b=[1, 1, 8],
                  expert_w_gu=[1, 8, D, 2 * D_FFE], expert_w_down=[1, 8, D_FFE, D])
    _ins = {}

    def IN(name):
        if name not in _ins:
            _ins[name] = m.dram(name, SHAPES[name], F32, kind="ExternalInput")
        return _ins[name]
    m.used_inputs = _ins
    y_out = m.dram("y", [S, D], F32, kind="ExternalOutput")

    qT = {n: m.scr(n, [512, S], BF16) for n in ("mqT", "mkT", "rqkT", "bqT", "bkT", "sBCT")}
    mv65 = m.scr("mv65", [S, 520], BF16)
    tokm = {n: m.scr(n, [S, 512], BF16) for n in ("rv", "rg", "sz", "bv", "sx")}
    scumT = m.scr("scumT", [8, S], F32)
    sdtok = m.scr("sdtok", [128, NT * 16], F32)
    oT = m.scr("oT", [4, 512, S], BF16)
    mergedT = m.scr("mergedT", [D, S], BF16)
    x1_d = m.scr("x1", [S, D], F32)
    x2_d = m.scr("x2", [S, D], F32)

    ident = m.alloc(128)
    identb = m.allocb(128)
    ones_row = m.alloc(128)
    one11 = ones_row[0:1, 0:1]
    modT = m.alloc(48)
    scp = m.alloc(16)
    gbc = m.alloc(2 * D)
    lnbc = m.alloc(4 * D)
    small = m.alloc(256)
    persist_end = m.aoff

    m.memset('pool', ident, 1.0, w=['ident'])
    m.asel(ident, ident, [[-1, 128]], ALU.is_equal, 0.0, 0, 1, r=['ident'], w=['ident'])
    m.cp('pool', identb, ident, r=['ident'], w=['identb'])
    m.memset('pool', ones_row, 1.0, w=['ones'])
    m.barrier()

    def ln_epilogue(ps2, xt, xkey, g_ap, lg, lb, out_t, okey, tmp, tkey, st):
        pk, stk = ps2[1], ('st', st)
        m.tt('dve', tmp, ps2[0], g_ap, ALU.mult, r=list(pk) + ['gbc'], w=[tkey])
        m.stt(tmp, xt, ALPHA, tmp, ALU.mult, ALU.add, r=[xkey, tkey], w=[tkey])
        sm = small[:, st * 32:(st + 1) * 32]
        m.op('dve', lambda e: e.bn_stats(out=sm[:, 0:6], in_=tmp[:, 0:512]), r=[tkey], w=[stk])
        m.op('dve', lambda e: e.bn_stats(out=sm[:, 6:12], in_=tmp[:, 512:1024]), r=[tkey], w=[stk])
        m.op('dve', lambda e: e.bn_aggr(out=sm[:, 12:14], in_=sm[:, 0:12]), r=[stk], w=[stk])
        m.ts('dve', sm[:, 14:15], sm[:, 13:14], LN_EPS, ALU.add, r=[stk], w=[stk])
        m.act(sm[:, 15:16], sm[:, 14:15], AF.Sqrt, r=[stk], w=[stk])
        m.op('dve', lambda e: e.reciprocal(out=sm[:, 16:17], in_=sm[:, 15:16]), r=[stk], w=[stk])
        m.ts('dve', tmp, tmp, sm[:, 12:13], ALU.subtract, r=[tkey, stk], w=[tkey], s2=sm[:, 16:17], op1=ALU.mult)
        m.tt('pool', tmp, tmp, lg, ALU.mult, r=[tkey, 'lnbc'], w=[tkey])
        m.tt('pool', out_t, tmp, lb, ALU.add, r=[tkey, 'lnbc'], w=[okey])

    for l in layers:
        x_src = IN("x") if (l == 0 or x_first) else x2_d
        x_dst = x2_d if l == 0 else y_out
        m.aoff = persist_end
        cT = m.alloc(8)
        cact = m.alloc(8)
        modrow = m.alloc(6 * D)
        brow = m.alloc(6 * D)
        m.init_wbufs(2, 8 * 512)
        m.dma(cT, IN("c"), w=['cT'], slot='misc0')
        m.dma(brow[0:1, :], IN("b_ada")[l], w=['brow'], slot='misc1')
        for i, k in enumerate(("ln1_g", "ln1_b", "ln2_g", "ln2_b")):
            m.dma(lnbc[:, i * D:(i + 1) * D], IN(k)[l].partition_broadcast(128), w=['lnbc'], slot=('lnbc', i))
        m.act(cact, cT, AF.Silu, r=['cT'], w=['cact'])
        for j in range(12):
            wb, wk = m.load_w(IN("w_ada")[l][:, j * 512:(j + 1) * 512], 8, 512, cast=False)
            b = m.bank()
            for k in range(8):
                m.mm(m.ps[0:1, b, :], cact[:, k:k + 1], wb[:, k, :], k == 0, k == 7, r=['cact', wk], w=[('ps', b)])
            m.tt('dve', modrow[0:1, j * 512:(j + 1) * 512], m.ps[0:1, b, :], brow[0:1, j * 512:(j + 1) * 512], ALU.add,
                 r=[('ps', b), 'brow'], w=['modrow'])
        b = m.bank()
        for j in range(48):
            m.mm(m.ps[:, b, j:j + 1], modrow[0:1, j * 128:(j + 1) * 128], one11, True, True, r=['modrow', 'ones'], w=[('ps', b)])
        m.cp('dve', modT, m.ps[:, b, 0:48], r=[('ps', b)], w=['modT'])
        m.ts('dve', scp[:, 0:8], modT[:, 8:16], 1.0, ALU.add, r=['modT'], w=['scp'])
        m.ts('dve', scp[:, 8:16], modT[:, 32:40], 1.0, ALU.add, r=['modT'], w=['scp'])
        for gi, c0 in enumerate((2 * D, 5 * D)):
            for hh in range(2):
                b = m.bank()
                m.mm(m.ps[:, b, :], ones_row[0:1, 0:128], modrow[0:1, c0 + hh * 512:c0 + (hh + 1) * 512], True, True,
                     r=['modrow', 'ones'], w=[('ps', b)])
                m.cp('act', gbc[:, gi * D + hh * 512:gi * D + (hh + 1) * 512], m.ps[:, b, :], r=[('ps', b)], w=['gbc'])
        m.barrier()
        if 'modT' in m.dbg:
            dd = m.scr("modT", [128, 48], F32)
            m.dma(dd, modT, r=['modT'], slot='dbg')
        if stop_after == ('mod', l):
            break

        m.aoff = persist_end
        hT = _r3(m.allocb(8 * S), 8)
        p1_end = m.aoff
        xts = [m.alloc(D) for _ in range(3)]

        def make_hT(dst, src_d, sc_ap, sh_ap, tts, dst_off=0):
            for tt_ in tts:
                s_ = tt_ % 3
                m.dma(xts[s_], src_d[tt_ * 128:(tt_ + 1) * 128, :], w=[('xt', s_)], slot=('xt', s_))
                b2 = m.bank2()
                for c in range(8):
                    bb, cc = b2 + c // 4, (c % 4) * 128
                    m.tr(m.ps[:, bb, cc:cc + 128], xts[s_][:, c * 128:(c + 1) * 128], ident, r=[('xt', s_), 'ident'], w=[('ps', bb)])
                for c in range(8):
                    bb, cc = b2 + c // 4, (c % 4) * 128
                    o = (tt_ - dst_off) * 128
                    m.act(dst[:, c, o:o + 128], m.ps[:, bb, cc:cc + 128], AF.Identity, r=[('ps', bb), 'scp', 'modT'],
                          w=[('hT', tt_)], scale=sc_ap[:, c:c + 1], bias=sh_ap[:, c:c + 1])
        make_hT(hT, x_src, scp[:, 0:8], modT[:, 0:8], range(NT))
        m.barrier()
        if 'hT' in m.dbg:
            dd = m.scr("hT", [128, 8 * S], BF16)
            m.dma(dd, hT.rearrange("p c n -> p (c n)"), slot='dbg')
            m.barrier()
        if stop_after == ('hT', l):
            break

        m.aoff = p1_end
        m.init_wbufs(2, 8 * 512)
        rowbuf = [m.allocb(S) for _ in range(2)]
        tokbuf = [m.allocb(8 * 520) for _ in range(2)]
        ri = [0]
        ti = [0]
        for tb_ in tokbuf:
            m.memset('pool', tb_, 1.0, w=[])
        m.barrier()
        HK = [('hT', t) for t in range(NT)]

        def proj_T(col0, dst_rows_list, evac_eng=('act', 'dve')):
            wb, wk = m.load_w(IN("w_in")[l][:, col0:col0 + 512], 8, 512)
            for j in range(4):
                if dst_rows_list[j] is None:
                    continue
                rb = rowbuf[ri[0] % 2]
                rk = ('row', ri[0] % 2)
                ri[0] += 1
                for tb in range(8):
                    b = m.bank()
                    for k in range(8):
                        m.mm(m.ps[:, b, :], wb[:, k, j * 128:(j + 1) * 128], hT[:, k, tb * 512:(tb + 1) * 512], k == 0, k == 7,
                             r=[wk] + HK[tb * 4:tb * 4 + 4], w=[('ps', b)])
                    m.cp(evac_eng[tb % 2], rb[:, tb * 512:(tb + 1) * 512], m.ps[:, b, :], r=[('ps', b)], w=[rk])
                m.dma(dst_rows_list[j], rb, r=[rk], slot=rk)

        def proj_N(col0, dst, width=512):
            wb, wk = m.load_w(IN("w_in")[l][:, col0:col0 + 512], 8, 512)
            for g in range(4):
                tb_ = tokbuf[ti[0] % 2]
                tk = ('tok', ti[0] % 2)
                ti[0] += 1
                t3 = tb_.rearrange("p (t n) -> p t n", t=8)
                for q in range(8):
                    tt_ = g * 8 + q
                    b = m.bank()
                    for k in range(8):
                        m.mm(m.ps[:, b, :], hT[:, k, tt_ * 128:(tt_ + 1) * 128], wb[:, k, :], k == 0, k == 7,
                             r=[wk, HK[tt_]], w=[('ps', b)])
                    if width == 520:
                        o = t3[:, q, :].rearrange("p (h e) -> p h e", e=65)[:, :, 0:64]
                        i_ = m.ps[:, b, :].rearrange("p (h e) -> p h e", e=64)
                    else:
                        o = t3[:, q, 0:512]
                        i_ = m.ps[:, b, :]
                    m.cp(('act', 'dve')[q % 2], o, i_, r=[('ps', b)], w=[tk])
                m.dma(dst[g * 1024:(g + 1) * 1024, :].rearrange("(t p) n -> p t n", p=128), t3[:, :, 0:width], r=[tk], slot=tk)

        def rows(name, j):
            return qT[name][j * 128:(j + 1) * 128, :]
        proj_T(0, [rows("mqT", j) for j in range(4)])
        proj_T(512, [rows("mkT", j) for j in range(4)])
        proj_N(1024, mv65, 520)
        proj_T(1536, [rows("rqkT", j) for j in range(4)])
        proj_N(2048, tokm["rv"])
        proj_N(2560, tokm["rg"])
        proj_N(3072, tokm["sz"])
        proj_T(4616, [rows("bqT", j) for j in range(4)])
        proj_T(5128, [rows("bkT", j) for j in range(4)])
        proj_N(5640, tokm["bv"])
        m.barrier()
        if stop_after == ('proj', l):
            break

        m.aoff = p1_end
        m.init_wbufs(1, 8 * 512)
        xc = m.alloc(S + 4)
        accb = m.alloc(S)
        sc2 = m.alloc(S)
        outb = sc2[:, 0:S // 2].bitcast(BF16)
        xtok = sc2[:, S // 2:S].bitcast(BF16)
        cwt = m.alloc(32)
        cbt = m.alloc(8)
        dtb = m.alloc(1)
        acol = m.alloc(1)
        m.dma(cwt, IN("conv_w")[l].rearrange("p c j -> p (c j)"), w=['cwt'], slot='misc0')
        m.dma(cbt, IN("conv_b")[l], w=['cbt'], slot='misc1')
        m.dma(dtb[0:8, :], IN("dt_bias")[l], w=['dtb'], slot='misc2')
        m.dma(acol[0:8, :], IN("a_log")[l], w=['acol'], slot='misc3')
        m.act(acol[0:8, :], acol[0:8, :], AF.Exp, r=['acol'], w=['acol'])
        m.ts('dve', acol[0:8, :], acol[0:8, :], -1.0, ALU.mult, r=['acol'], w=['acol'])
        m.memset('pool', accb[0:8, :], 1.0, w=['accb'])
        m.memset('pool', xc[:, 0:3], 0.0, w=['xc'])
        wb, wk = m.load_w(IN("w_in")[l][:, 4608:4616], 8, 8)
        dtT = xc[0:8, 4:4 + S]
        cum = sc2[0:8, :]
        for tb in range(8):
            b = m.bank()
            for k in range(8):
                m.mm(m.ps[0:8, b, :], wb[:, k, 0:8], hT[:, k, tb * 512:(tb + 1) * 512], k == 0, k == 7,
                     r=[wk] + HK[tb * 4:tb * 4 + 4], w=[('ps', b)])
            m.act(dtT[:, tb * 512:(tb + 1) * 512], m.ps[0:8, b, :], AF.Exp, r=[('ps', b), 'dtb'], w=['dtT'], bias=dtb[0:8, :])
        m.act(dtT, dtT, AF.Ln, r=['dtT'], w=['dtT'], bias=1.0)
        m.ts('dve', cum, dtT, acol[0:8, :], ALU.mult, r=['dtT', 'acol'], w=['cum'])
        m.op('dve', lambda e: e.tensor_tensor_scan(out=cum, data0=accb[0:8, :], data1=cum, initial=0.0, op0=ALU.mult, op1=ALU.add),
             r=['cum', 'accb'], w=['cum'])
        m.dma(scumT, cum, r=['cum'], slot='misc4')
        b = m.bank()
        for tt_ in range(NT):
            m.tr(m.ps[:, b, tt_ * 16:tt_ * 16 + 8], dtT[:, tt_ * 128:(tt_ + 1) * 128], ident[0:8, 0:8], r=['dtT', 'ident'], w=[('ps', b)])
            m.tr(m.ps[:, b, tt_ * 16 + 8:tt_ * 16 + 16], cum[:, tt_ * 128:(tt_ + 1) * 128], ident[0:8, 0:8], r=['cum', 'ident'], w=[('ps', b)])
        sdt_sb = accb[:, 512:1024]
        m.cp('dve', sdt_sb, m.ps[:, b, :], r=[('ps', b), 'accb'], w=['sdt_sb'])
        ncv = sdt_sb.rearrange("p (t e) -> p t e", e=16)[:, :, 8:16]
        m.ts('dve', ncv, ncv, -1.0, ALU.mult, r=['sdt_sb'], w=['sdt_sb'])
        m.dma(sdtok, sdt_sb, r=['sdt_sb'], slot='misc5')
        m.barrier()
        for blk in range(2):
            wb, wk = m.load_w(IN("w_in")[l][:, 3584 + blk * 512:3584 + (blk + 1) * 512], 8, 512)
            for jj in range(4):
                j = blk * 4 + jj
                for tb in range(8):
                    b = m.bank()
                    for k in range(8):
                        m.mm(m.ps[:, b, :], wb[:, k, jj * 128:(jj + 1) * 128], hT[:, k, tb * 512:(tb + 1) * 512], k == 0, k == 7,
                             r=[wk] + HK[tb * 4:tb * 4 + 4], w=[('ps', b)])
                    m.cp(('act', 'dve')[tb % 2], xc[:, 3 + tb * 512:3 + (tb + 1) * 512], m.ps[:, b, :], r=[('ps', b)], w=['xc'])
                m.act(accb, xc[:, 3:3 + S], AF.Identity, r=['xc', 'cwt', 'cbt'], w=['accb'], scale=cwt[:, j * 4 + 3:j * 4 + 4], bias=cbt[:, j:j + 1])
                for tap in (2, 1, 0):
                    m.stt(accb, xc[:, tap:tap + S], cwt[:, j * 4 + tap:j * 4 + tap + 1], accb, ALU.mult, ALU.add, r=['xc', 'cwt', 'accb'], w=['accb'])
                m.act(outb, accb, AF.Silu, r=['accb'], w=['outb'])
                if j >= 4:
                    m.dma(qT["sBCT"][(j - 4) * 128:(j - 3) * 128, :], outb, r=['outb'], slot='misc6')
                else:
                    for g in range(4):
                        b = m.bank()
                        pb = m.ps[:, b, :].bitcast(BF16)
                        for q in range(8):
                            tt_ = g * 8 + q
                            m.tr(pb[:, q * 128:(q + 1) * 128], outb[:, tt_ * 128:(tt_ + 1) * 128], identb, r=['outb', 'identb'], w=[('ps', b)])
                        m.cp(('act', 'dve')[g % 2], xtok[:, g * 1024:(g + 1) * 1024], pb, r=[('ps', b)], w=['xtok'])
                    m.dma(tokm["sx"][:, j * 128:(j + 1) * 128].rearrange("(t p) n -> p t n", p=128),
                          xtok.rearrange("p (t n) -> p t n", n=128), r=['xtok'], slot='misc7')
        m.barrier()
        if stop_after == ('ssdpre', l):
            break

        m.aoff = persist_end
        O_all = m.allocb(NT * 512).rearrange("p (t n) -> p t n", n=512)
        qkraw = m.alloc(4 * S // 2)
        qk = [[qkraw[:, (2 * s_ + i_) * 2048:(2 * s_ + i_ + 1) * 2048].bitcast(BF16) for i_ in range(2)] for s_ in range(2)]
        rowb = m.allocb(S)
        Ei = m.alloc(512)
        Ef = m.alloc(512)
        mix_base = m.aoff
        m.op('pool', lambda e: e.iota(Ei.bitcast(mybir.dt.int32), pattern=[[1, 512]], base=0, channel_multiplier=-1), w=['Ei'])
        m.cp('dve', Ef, Ei.bitcast(mybir.dt.int32), r=['Ei'], w=['Ef'])
        qki = [0]

        def load_qk(qname, qrow, kname, krow):
            s_ = qki[0] % 2
            qki[0] += 1
            m.dma(qk[s_][0][0:64, :], qT[qname][qrow:qrow + 64, :], w=[('qk', s_)], slot=('qk', s_, 0))
            m.dma(qk[s_][1][0:64, :], qT[kname][krow:krow + 64, :], r=[('qk', s_)], w=[('qk', s_)], slot=('qk', s_, 1))
            return qk[s_][0][0:64, :], qk[s_][1][0:64, :], ('qk', s_)

        def finalize_branch(n):
            for c in range(4):
                for g in range(4):
                    b = m.bank()
                    pb = m.ps[:, b, :].bitcast(BF16)
                    for q in range(8):
                        tt_ = g * 8 + q
                        m.tr(pb[:, q * 128:(q + 1) * 128], O_all[:, tt_, c * 128:(c + 1) * 128], identb, r=[('O', tt_), 'identb'], w=[('ps', b)])
                    m.cp(('act', 'dve')[g % 2], rowb[:, g * 1024:(g + 1) * 1024], pb, r=[('ps', b)], w=['rowb'])
                m.dma(oT[n][c * 128:(c + 1) * 128, :], rowb, r=['rowb'], slot='rowb')
            m.barrier()

        def mk_decay(dst_full, dst_mask, coef, width, strict=False):
            m.act(dst_full[:, 0:width], Ef[:, 0:width], AF.Exp, r=['Ef'], w=['dfull'], scale=coef)
            m.asel(dst_mask[:, 0:width], dst_full[:, 0:width], [[1, width]], ALU.is_ge, 0.0, -1 if strict else 0, -1, r=['dfull'], w=['dmask'])

        def diag_mask(ap, r, w, strict):
            m.asel(ap, ap, [[1, 128]], ALU.is_ge, 0.0, -1 if strict else 0, -1, r=r, w=w)

        if 'a' in MIX:
            m.aoff = mix_base
            V = m.allocb(NT * 520).rearrange("p (t h e) -> p t h e", h=8, e=65)
            m.dma(V.rearrange("p t h e -> p t (h e)"), mv65.rearrange("(t p) n -> p t n", p=128), w=['V'], slot='V')
            Eiw = m.alloc(4352)
            Efw = m.alloc(4352)
            m.op('pool', lambda e: e.iota(Eiw.bitcast(mybir.dt.int32), pattern=[[1, 4352]], base=0, channel_multiplier=-1), w=['Eiw'])
            m.cp('dve', Efw, Eiw.bitcast(mybir.dt.int32), r=['Eiw'], w=['Efw'])
            ksum = m.alloc(16)
            ktmp = m.alloc(16)
            khl = m.allocb(32)
            gate = m.alloc(512)
            sel = m.alloc(512)
            m8 = m.alloc(8)
            accs = [m.alloc(130) for _ in range(4)]
            rec = m.alloc(4)
            pes = [m.alloc(256) for _ in range(4)]
            pts = [m.allocb(256) for _ in range(6)]
            cnt = 0
            for h in range(8):
                slope = 2.0 ** (-(h + 1))
                qh, kh, qkk = load_qk("mqT", h * 64, "mkT", h * 64)
                m.op('dve', lambda e, kh=kh: e.tensor_reduce(out=ksum[0:64, :], in_=kh.rearrange("p (n j) -> p n j", j=256), axis=mybir.AxisListType.X, op=ALU.add),
                     r=[qkk], w=['ksum'])
                m.cp('dve', khl[0:64, 0:16], ksum[0:64, :], r=['ksum'], w=['khl'])
                m.tt('dve', ktmp[0:64, :], ksum[0:64, :], khl[0:64, 0:16], ALU.subtract, r=['ksum', 'khl'], w=['ktmp'])
                m.cp('dve', khl[0:64, 16:32], ktmp[0:64, :], r=['ktmp'], w=['khl'])
                b = m.bank()
                for tt_ in range(NT):
                    m.mm(m.ps[:, b, tt_ * 16:(tt_ + 1) * 16], qh[:, tt_ * 128:(tt_ + 1) * 128], khl[0:64, 0:16], True, False, r=[qkk, 'khl'], w=[('ps', b)])
                    m.mm(m.ps[:, b, tt_ * 16:(tt_ + 1) * 16], qh[:, tt_ * 128:(tt_ + 1) * 128], khl[0:64, 16:32], False, True, r=[qkk, 'khl'], w=[('ps', b)])
                m.cp('dve', gate, m.ps[:, b, :], r=[('ps', b)], w=['gate'])
                m.asel(gate.rearrange("p (t n) -> p t n", n=16), gate.rearrange("p (t n) -> p t n", n=16), [[1, 32], [-2, 16]], ALU.is_ge, -1e30, -2, 0,
                       r=['gate'], w=['gate'])
                for tt_ in range(NT):
                    own = tt_ // 2
                    g_ = gate[:, tt_ * 16:(tt_ + 1) * 16]
                    if own >= 4:
                        m.op('dve', lambda e, g_=g_: e.max(out=m8, in_=g_), r=['gate'], w=['m8'])
                        m.ts('dve', sel[:, tt_ * 16:(tt_ + 1) * 16], g_, m8[:, 2:3], ALU.is_ge, r=['gate', 'm8'], w=['sel'])
                    else:
                        m.ts('dve', sel[:, tt_ * 16:(tt_ + 1) * 16], g_, -1e29, ALU.is_gt, r=['gate'], w=['sel'])
                pend = [None]

                def flush():
                    if pend[0] is not None:
                        pend[0]()
                        pend[0] = None
                for qb in range(16):
                    q0 = qb * 256
                    acq = accs[(qb % 2) * 2:(qb % 2) * 2 + 2]
                    for a_ in acq:
                        m.memset('pool', a_, 0.0, w=[('acc', id(a_))])
                    for n in range(qb + 1):
                        diag = (n == qb)
                        ptl = []
                        for kt2 in range(2):
                            kt = 2 * n + kt2
                            c0 = 128 if (diag and kt2 == 1) else 0
                            b = m.bank()
                            m.mm(m.ps[:, b, c0:256], kh[:, kt * 128:(kt + 1) * 128], qh[:, q0 + c0:q0 + 256], True, True, r=[qkk], w=[('ps', b)])
                            pe_ = pes[cnt % 4]
                            pek = ('pe', cnt % 4)
                            pt = pts[cnt % 6]
                            ptk = ('pt', cnt % 6)
                            cnt += 1
                            off = q0 - kt * 128
                            m.stt(pe_[:, c0:256], Efw[:, off + c0:off + 256], -8.0 * slope, m.ps[:, b, c0:256], ALU.mult, ALU.add, r=[('ps', b), 'Efw'], w=[pek])
                            m.act(pt[:, c0:256], pe_[:, c0:256], AF.Exp, r=[pek], w=[ptk], scale=0.125)
                            if diag:
                                diag_mask(pt[:, c0:c0 + 128], r=[ptk], w=[ptk], strict=False)
                            ptl.append((pt, ptk, kt, c0))

                        def back(ptl=ptl, qb=qb, n=n, diag=diag, acq=acq):
                            for qt2 in range(2):
                                tt_ = 2 * qb + qt2
                                use = [(pt, ptk, kt) for (pt, ptk, kt, c0) in ptl if c0 <= qt2 * 128]
                                b = m.bank()
                                for i_, (pt, ptk, kt) in enumerate(use):
                                    m.mm(m.ps[:, b, 0:65], pt[:, qt2 * 128:(qt2 + 1) * 128], V[:, kt, h, :], i_ == 0, i_ == len(use) - 1,
                                         r=[ptk, 'V'], w=[('ps', b)])
                                ak = ('acc', id(acq[qt2]))
                                scal = 1.0 if diag else sel[:, tt_ * 16 + n:tt_ * 16 + n + 1]
                                m.stt(acq[qt2][:, 0:65], m.ps[:, b, 0:65], scal, acq[qt2][:, 0:65], ALU.mult, ALU.add, r=[('ps', b), 'sel', ak], w=[ak])
                            if diag:
                                for qt2 in range(2):
                                    tt_ = 2 * qb + qt2
                                    ak = ('acc', id(acq[qt2]))
                                    rc = rec[:, (qb % 2) * 2 + qt2:(qb % 2) * 2 + qt2 + 1]
                                    rk_ = ('rec', (qb % 2) * 2 + qt2)
                                    m.op('dve', lambda e, rc=rc, a_=acq[qt2]: e.reciprocal(out=rc, in_=a_[:, 64:65]), r=[ak], w=[rk_])
                                    m.ts('dve', O_all[:, tt_, h * 64:(h + 1) * 64], acq[qt2][:, 0:64], rc, ALU.mult, r=[ak, rk_], w=[('O', tt_)])
                        prev = pend[0]
                        pend[0] = back
                        if prev is not None:
                            prev()
                flush()
            finalize_branch(0)

        if 'b' in MIX:
            m.aoff = mix_base
            V = m.allocb(NT * 512).rearrange("p (t n) -> p t n", n=512)
            G = m.allocb(NT * 512).rearrange("p (t n) -> p t n", n=512)
            m.dma(V, tokm["rv"].rearrange("(t p) n -> p t n", p=128), w=['V'], slot='V')
            m.dma(G, tokm["rg"].rearrange("(t p) n -> p t n", p=128), w=['G'], slot='G')
            gng = m.alloc(512)
            gnb = m.alloc(512)
            m.dma(gng, IN("ret_gn_g")[l].partition_broadcast(128), w=['gng'], slot='misc0')
            m.dma(gnb, IN("ret_gn_b")[l].partition_broadcast(128), w=['gnb'], slot='misc1')
            Dfull = m.alloc(512)
            Dmask = m.alloc(512)
            pts = [m.allocb(512) for _ in range(4)]
            tmpo = [m.alloc(128) for _ in range(2)]
            tmps = [m.alloc(128) for _ in range(2)]
            cnt = 0
            ecnt = 0
            rqb = 0
            for h in range(4):
                lg = math.log(1.0 - 2.0 ** (-5.0 - h))
                qh, kh, qkk = load_qk("rqkT", h * 64, "rqkT", 256 + h * 64)
                mk_decay(Dfull, Dmask, lg, 512)
                pend = [None]
                for qb in range(8):
                    q0 = qb * 512
                    bo = 6 + (rqb % 2)
                    rqb += 1
                    nk = 4 * qb + 4
                    for kt in range(nk):
                        mdg = kt - 4 * qb
                        c0 = 128 * max(mdg, 0)
                        b = m.bank6()
                        m.mm(m.ps[:, b, c0:512], kh[:, kt * 128:(kt + 1) * 128], qh[:, q0 + c0:q0 + 512], True, True, r=[qkk], w=[('ps', b)])
                        pt = pts[cnt % 4]
                        ptk = ('pt', cnt % 4)
                        cnt += 1
                        if mdg >= 0:
                            m.stt(pt[:, c0:512], m.ps[:, b, c0:512], 0.125, Dmask[:, 0:512 - c0], ALU.mult, ALU.mult, r=[('ps', b), 'dmask'], w=[ptk])
                        else:
                            sc = 0.125 * math.exp(lg * (q0 - kt * 128))
                            m.stt(pt, m.ps[:, b, :], sc, Dfull, ALU.mult, ALU.mult, r=[('ps', b), 'dfull'], w=[ptk])

                        def back(pt=pt, ptk=ptk, kt=kt, mdg=mdg, bo=bo, qb=qb, last=(kt == nk - 1), h=h):
                            nonlocal ecnt
                            for qt in range(max(mdg, 0), 4):
                                m.mm(m.ps[:, bo, qt * 128:(qt + 1) * 128], pt[:, qt * 128:(qt + 1) * 128], V[:, kt, h * 128:(h + 1) * 128], (kt == 0 and qt == 0), False,
                                     r=[ptk, 'V'], w=[('ps', bo)])
                            if not last:
                                return
                            for qt in range(4):
                                tt_ = 4 * qb + qt
                                e_ = ecnt % 2
                                ecnt += 1
                                sm = small[:, 64 + e_ * 32:64 + (e_ + 1) * 32]
                                smk = ('sm', e_)
                                o_ = m.ps[:, bo, qt * 128:(qt + 1) * 128]
                                m.op('dve', lambda e, sm=sm, o_=o_: e.bn_stats(out=sm[:, 0:6], in_=o_), r=[('ps', bo)], w=[smk])
                                m.op('dve', lambda e, sm=sm: e.bn_aggr(out=sm[:, 6:8], in_=sm[:, 0:6]), r=[smk], w=[smk])
                                m.ts('dve', sm[:, 8:9], sm[:, 7:8], NORM_EPS, ALU.add, r=[smk], w=[smk])
                                m.act(sm[:, 9:10], sm[:, 8:9], AF.Sqrt, r=[smk], w=[smk])
                                m.op('dve', lambda e, sm=sm: e.reciprocal(out=sm[:, 10:11], in_=sm[:, 9:10]), r=[smk], w=[smk])
                                to = tmpo[e_]
                                tk = ('tmpo', e_)
                                m.ts('dve', to, o_, sm[:, 6:7], ALU.subtract, r=[('ps', bo), smk], w=[tk], s2=sm[:, 10:11], op1=ALU.mult)
                                m.tt('pool', to, to, gng[:, h * 128:(h + 1) * 128], ALU.mult, r=[tk, 'gng'], w=[tk])
                                m.tt('pool', to, to, gnb[:, h * 128:(h + 1) * 128], ALU.add, r=[tk, 'gnb'], w=[tk])
                                m.act(tmps[e_], G[:, tt_, h * 128:(h + 1) * 128], AF.Silu, r=['G'], w=[('tmps', e_)])
                                m.tt('pool', O_all[:, tt_, h * 128:(h + 1) * 128], to, tmps[e_], ALU.mult, r=[tk, ('tmps', e_)], w=[('O', tt_)])
                        prev = pend[0]
                        pend[0] = back
                        if prev is not None:
                            prev()
                pend[0]()
                pend[0] = None
            finalize_branch(1)

        if 'c' in MIX:
            m.aoff = mix_base
            X = m.allocb(NT * 512).rearrange("p (t n) -> p t n", n=512)
            Z = m.allocb(NT * 512).rearrange("p (t n) -> p t n", n=512)
            m.dma(X, tokm["sx"].rearrange("(t p) n -> p t n", p=128), w=['X'], slot='V')
            m.dma(Z, tokm["sz"].rearrange("(t p) n -> p t n", p=128), w=['Z'], slot='G')
            sdt = m.alloc(512)
            m.dma(sdt, sdtok, w=['sdt'], slot='misc0')
            dsk = m.alloc(8)
            m.dma(dsk, IN("d_skip")[l].partition_broadcast(128), w=['dsk'], slot='misc1')
            ngb = m.alloc(512)
            m.dma(ngb, IN("ssm_norm_g")[l].partition_broadcast(128), w=['ngb'], slot='misc2')
            BT = qk[0][0]
            CT = qk[0][1]
            Gbc = qkraw[:, 4096:8192]
            xdt = m.allocb(NT * 64).rearrange("p (t n) -> p t n", n=64)
            ss = m.alloc(NT * 8)
            rstd = m.alloc(NT * 2)
            Ls = [m.alloc(512) for _ in range(3)]
            wts = [m.allocb(512) for _ in range(4)]
            rqb = 0
            ytmp = [m.alloc(64) for _ in range(2)]
            ztmp = [m.alloc(64) for _ in range(2)]
            junk = m.alloc(64)
            sdt3 = sdt.rearrange("p (t e) -> p t e", e=16)
            cnt = 0
            ecnt = 0
            for g in range(2):
                m.dma(BT, qT["sBCT"][g * 128:(g + 1) * 128, :], w=['BT'], slot='misc3')
                m.dma(CT, qT["sBCT"][256 + g * 128:256 + (g + 1) * 128, :], w=['CT'], slot='misc4')
                for r_ in range(4):
                    h = 4 * g + r_
                    m.dma(Gbc, scumT[h:h + 1, :].partition_broadcast(128), w=['Gbc'], slot='misc5')
                    for tt_ in range(NT):
                        m.ts('pool', xdt[:, tt_, :], X[:, tt_, h * 64:(h + 1) * 64], sdt3[:, tt_, h:h + 1], ALU.mult, r=['X', 'sdt'], w=['xdt'])
                    pend = [None]
                    for tb in range(8):
                        t0 = tb * 512
                        by = 6 + (rqb % 2)
                        rqb += 1
                        ns_ = 4 * tb + 4
                        for st in range(ns_):
                            mdg = st - 4 * tb
                            c0 = 128 * max(mdg, 0)
                            b = m.bank6()
                            m.mm(m.ps[:, b, c0:512], BT[:, st * 128:(st + 1) * 128], CT[:, t0 + c0:t0 + 512], True, True, r=['BT', 'CT'], w=[('ps', b)])
                            L = Ls[cnt % 3]
                            lk = ('L', cnt % 3)
                            wt = wts[cnt % 4]
                            wk_ = ('wt', cnt % 4)
                            cnt += 1
                            m.act(L[:, c0:512], Gbc[:, t0 + c0:t0 + 512], AF.Exp, r=['Gbc', 'sdt'], w=[lk], bias=sdt3[:, st, 8 + h:9 + h])
                            if mdg >= 0:
                                diag_mask(L[:, c0:c0 + 128], r=[lk], w=[lk], strict=False)
                            m.tt('dve', wt[:, c0:512], L[:, c0:512], m.ps[:, b, c0:512], ALU.mult, r=[lk, ('ps', b)], w=[wk_])

                            def back(wt=wt, wk_=wk_, st=st, mdg=mdg, by=by, tb=tb, last=(st == ns_ - 1), h=h):
                                nonlocal ecnt
                                for qt in range(max(mdg, 0), 4):
                                    m.mm(m.ps[:, by, qt * 64:(qt + 1) * 64], wt[:, qt * 128:(qt + 1) * 128], xdt[:, st, :], (st == 0 and qt == 0), False,
                                         r=[wk_, 'xdt'], w=[('ps', by)])
                                if not last:
                                    return
                                for qt in range(4):
                                    tt_ = 4 * tb + qt
                                    e_ = ecnt % 2
                                    ecnt += 1
                                    yk = ('ytmp', e_)
                                    m.stt(ytmp[e_], X[:, tt_, h * 64:(h + 1) * 64], dsk[:, h:h + 1], m.ps[:, by, qt * 64:(qt + 1) * 64], ALU.mult, ALU.add,
                                          r=['X', 'dsk', ('ps', by)], w=[yk])
                                    m.act(ztmp[e_], Z[:, tt_, h * 64:(h + 1) * 64], AF.Silu, r=['Z'], w=[('ztmp', e_)])
                                    m.tt('dve', ytmp[e_], ytmp[e_], ztmp[e_], ALU.mult, r=[yk, ('ztmp', e_)], w=[yk])
                                    m.op('act', lambda e, e_=e_, tt_=tt_, h=h: e.activation(out=junk, in_=ytmp[e_], func=AF.Square, accum_out=ss[:, tt_ * 8 + h:tt_ * 8 + h + 1]),
                                         r=[yk], w=['junk', ('ss', tt_)])
                                    m.cp('pool', O_all[:, tt_, h * 64:(h + 1) * 64], ytmp[e_], r=[yk], w=[('O', tt_)])
                            prev = pend[0]
                            pend[0] = back
                            if prev is not None:
                                prev()
                    pend[0]()
                    pend[0] = None
                ssv = ss.rearrange("p (t e) -> p t e", e=8)[:, :, 4 * g:4 * g + 4]
                rg_ = rstd[:, g * NT:(g + 1) * NT]
                m.op('dve', lambda e, ssv=ssv, rg_=rg_: e.tensor_reduce(out=rg_, in_=ssv, axis=mybir.AxisListType.X, op=ALU.add),
                     r=[('ss', t) for t in range(NT)], w=[('rstd', g)])
                m.ts('dve', rg_, rg_, 1.0 / 256.0, ALU.mult, r=[('rstd', g)], w=[('rstd', g)], s2=LN_EPS, op1=ALU.add)
                m.act(rg_, rg_, AF.Sqrt, r=[('rstd', g)], w=[('rstd', g)])
                m.op('dve', lambda e, rg_=rg_: e.reciprocal(out=rg_, in_=rg_), r=[('rstd', g)], w=[('rstd', g)])
                for tt_ in range(NT):
                    o_ = O_all[:, tt_, g * 256:(g + 1) * 256]
                    m.stt(o_, o_, rg_[:, tt_:tt_ + 1], ngb[:, g * 256:(g + 1) * 256], ALU.mult, ALU.mult, r=[('O', tt_), ('rstd', g), 'ngb'], w=[('O', tt_)])
            finalize_branch(2)

        if 'd' in MIX:
            m.aoff = mix_base
            V = m.allocb(NT * 512).rearrange("p (t n) -> p t n", n=512)
            m.dma(V, tokm["bv"].rearrange("(t p) n -> p t n", p=128), w=['V'], slot='V')
            triU = m.allocb(128)
            triL = m.allocb(128)
            onesb = m.allocb(128)
            m.memset('pool', onesb, 1.0, w=['onesb'])
            m.asel(triU, onesb, [[-1, 128]], ALU.is_ge, 0.0, -1, 1, r=['onesb'], w=['triU'])
            m.asel(triL, onesb, [[1, 128]], ALU.is_ge, 0.0, 0, -1, r=['onesb'], w=['triL'])
            NS = 3
            bufs = []
            for hd in range(2):
                bufs.append(([m.alloc(512) for _ in range(NS)], [m.allocb(512) for _ in range(NS)],
                             [m.alloc(512) for _ in range(NS)], [m.allocb(512) for _ in range(NS)]))
            cntA = [0, 0]
            zc = [0, 0]

            def sb_head_qb(hd, h, qb, qh, kh, qkk):
                q0 = qb * 512
                kts = list(range(4 * qb + 3, -1, -1))
                n_ = len(kts)
                BACC, BO = 4 + hd, 6 + hd
                es_, spb_, ts__, wt_ = bufs[hd]
                info = {}

                def A(j):
                    kt = kts[j]
                    i = cntA[hd] % NS
                    cntA[hd] += 1
                    mdg = kt - 4 * qb
                    c0 = 128 * max(mdg, 0)
                    b = 2 * hd + (zc[hd] % 2)
                    zc[hd] += 1
                    ek, sk, tk, = ('es', hd, i), ('spb', hd, i), ('ts', hd, i)
                    m.mm(m.ps[:, b, c0:512], kh[:, kt * 128:(kt + 1) * 128], qh[:, q0 + c0:q0 + 512], True, True, r=[qkk], w=[('ps', b)])
                    m.act(es_[i][:, c0:512], m.ps[:, b, c0:512], AF.Exp, r=[('ps', b)], w=[ek], scale=0.125)
                    m.act(spb_[i][:, c0:512], es_[i][:, c0:512], AF.Ln, r=[ek], w=[sk], bias=1.0)
                    info[j] = (i, c0, mdg, kt, b)

                def A2(j):
                    i, c0, mdg, kt, b = info[j]
                    sk, tk = ('spb', hd, i), ('ts', hd, i)
                    m.stt(ts__[i][:, c0:512], m.ps[:, b, c0:512], 0.125, spb_[i][:, c0:512], ALU.mult, ALU.subtract, r=[('ps', b), sk], w=[tk])
                    if mdg >= 0:
                        diag_mask(spb_[i][:, c0:c0 + 128], r=[sk], w=[sk], strict=True)

                def B1(j):
                    i, c0, mdg, kt, b = info[j]
                    sk = ('spb', hd, i)
                    m.mm(m.ps[:, BACC, c0:512], triU, spb_[i][:, c0:512], j == 0, False, r=['triU', sk], w=[('ps', BACC)])

                def B2(j):
                    i, c0, mdg, kt, b = info[j]
                    tk = ('ts', hd, i)
                    m.tt('dve', ts__[i][:, c0:512], ts__[i][:, c0:512], m.ps[:, BACC, c0:512], ALU.subtract, r=[tk, ('ps', BACC)], w=[tk])

                def B3(j):
                    i, c0, mdg, kt, b = info[j]
                    sk = ('spb', hd, i)
                    m.mm(m.ps[:, BACC, c0:512], triL, spb_[i][:, c0:512], False, False, r=['triL', sk], w=[('ps', BACC)])

                def B4(j):
                    i, c0, mdg, kt, b = info[j]
                    tk, wk_ = ('ts', hd, i), ('wt', hd, i)
                    m.act(wt_[i][:, c0:512], ts__[i][:, c0:512], AF.Exp, r=[tk], w=[wk_])
                    if mdg >= 0:
                        diag_mask(wt_[i][:, c0:c0 + 128], r=[wk_], w=[wk_], strict=True)

                def C(j):
                    i, c0, mdg, kt, b = info[j]
                    wk_ = ('wt', hd, i)
                    for qt in range(max(mdg, 0), 4):
                        m.mm(m.ps[:, BO, qt * 64:(qt + 1) * 64], wt_[i][:, qt * 128:(qt + 1) * 128], V[:, kt, h * 64:(h + 1) * 64], j == 0 and qt == max(mdg, 0), False,
                             r=[wk_, 'V'], w=[('ps', BO)])

                def E():
                    for qt in range(4):
                        tt_ = 4 * qb + qt
                        m.cp('act', O_all[:, tt_, h * 64:(h + 1) * 64], m.ps[:, BO, qt * 64:(qt + 1) * 64], r=[('ps', BO)], w=[('O', tt_)])
                return dict(n=n_, A1=A, A2=A2, B1=B1, B2=B2, B3=B3, B4=B4, C=C, E=E)
            import os
            for hp in range(int(os.environ.get('SB_H', 8)) // 2):
                hs = (2 * hp, 2 * hp + 1)
                lq = [load_qk("bqT", h_ * 64, "bkT", h_ * 64) for h_ in hs]
                for qb in range(int(os.environ.get('SB_QB', 8))):
                    S_ = [sb_head_qb(hd, hs[hd], qb, lq[hd][0], lq[hd][1], lq[hd][2]) for hd in range(2)]
                    n_ = S_[0]['n']
                    for st_name in ('A1', 'A2'):
                        for hd in range(2):
                            S_[hd][st_name](0)
                    for j in range(n_):
                        if j + 1 < n_:
                            for hd in range(2):
                                S_[hd]['A1'](j + 1)
                        for st_name in ('B1', 'B2', 'B3', 'B4'):
                            for hd in range(2):
                                S_[hd][st_name](j)
                        if j + 1 < n_:
                            for hd in range(2):
                                S_[hd]['A2'](j + 1)
                        if j >= 1:
                            for hd in range(2):
                                S_[hd]['C'](j - 1)
                    for hd in range(2):
                        S_[hd]['C'](n_ - 1)
                        S_[hd]['E']()
            finalize_branch(3)
        m.barrier()
        if stop_after == ('mix', l):
            break

        m.aoff = persist_end
        hT = _r3(m.allocb(8 * S), 8)
        p1_end = m.aoff
        xts = [m.alloc(D) for _ in range(3)]
        make_hT(hT, x_src, scp[:, 0:8], modT[:, 0:8], range(NT))
        m.barrier()
        m.aoff = p1_end
        wst4 = [m.alloc(8 * 512)] * 2
        wg4 = [m.allocb(8 * 512) for _ in range(2)]
        wb4 = [m.allocb(4 * 512) for _ in range(2)]
        obs = [m.allocb(16 * 512) for _ in range(2)]
        rowbs = [m.allocb(S) for _ in range(2)]
        sigs = [m.alloc(512) for _ in range(2)]
        accs4 = [m.alloc(512) for _ in range(2)]
        tmps4 = [m.alloc(512) for _ in range(2)]
        oc = 0
        sc_ = 0
        for dp in range(4):
            for i in range(2):
                dc = 2 * dp + i
                st_g = wst4[0].rearrange("p (c n j) -> p c n j", c=8, n=4)
                for n in range(4):
                    c0 = 6152 + n * 1024 + dc * 128
                    m.dma(st_g[:, :, n, :], IN("w_in")[l][:, c0:c0 + 128].rearrange("(c p) j -> p c j", p=128), w=[('wst4', 0)], r=[('wst4', 0)], slot=('wst4', 0, n))
                m.cp('pool', wg4[i], wst4[0], r=[('wst4', 0)], w=[('wg4', i)])
                st_b = wst4[0][:, 0:2048].rearrange("p (n c j) -> p n c j", n=4, c=4)
                for n in range(4):
                    m.dma(st_b[:, n, :, :], IN("w_br")[l][n][:, dc * 128:(dc + 1) * 128].rearrange("(c p) j -> p c j", p=128), w=[('wst4', 0)], r=[('wst4', 0)], slot=('wst4', 0, n))
                m.cp('pool', wb4[i], wst4[0][:, 0:2048], r=[('wst4', 0)], w=[('wb4', i)])
            for tb in range(8):
                o_i = oc % 2
                oc += 1
                ob = obs[o_i].rearrange("p (n c t) -> p n c t", n=4, c=4)
                for n in range(4):
                    m.dma(ob[:, n, :, :], oT[n][:, tb * 512:(tb + 1) * 512].rearrange("(c p) t -> p c t", p=128), w=[('ob', o_i, n)], slot=('ob', o_i, n))
                for i in range(2):
                    wg = wg4[i].rearrange("p (c n j) -> p c n j", c=8, n=4)
                    wbr = wb4[i].rearrange("p (n c j) -> p n c j", n=4, c=4)
                    rb = rowbs[i]
                    rk = ('rowb4', i)
                    a_i = i
                    for n in range(4):
                        bg = m.bank()
                        for k in range(8):
                            m.mm(m.ps[:, bg, :], wg[:, k, n, :], hT[:, k, tb * 512:(tb + 1) * 512], k == 0, k == 7, r=[('wg4', i)] + HK[tb * 4:tb * 4 + 4], w=[('ps', bg)])
                        by = m.bank()
                        for c in range(4):
                            m.mm(m.ps[:, by, :], wbr[:, n, c, :], ob[:, n, c, :], c == 0, c == 3, r=[('wb4', i), ('ob', o_i, n)], w=[('ps', by)])
                        s_i = sc_ % 2
                        sc_ += 1
                        m.act(sigs[s_i], m.ps[:, bg, :], AF.Sigmoid, r=[('ps', bg)], w=[('sig', s_i)])
                        if n == 0:
                            m.tt('dve', accs4[a_i], sigs[s_i], m.ps[:, by, :], ALU.mult, r=[('sig', s_i), ('ps', by)], w=[('acc4', a_i)])
                        else:
                            m.tt('dve', tmps4[s_i], sigs[s_i], m.ps[:, by, :], ALU.mult, r=[('sig', s_i), ('ps', by)], w=[('tmp4', s_i)])
                            if n < 3:
                                m.tt('pool', accs4[a_i], accs4[a_i], tmps4[s_i], ALU.add, r=[('acc4', a_i), ('tmp4', s_i)], w=[('acc4', a_i)])
                            else:
                                m.tt('pool', rb[:, tb * 512:(tb + 1) * 512], accs4[a_i], tmps4[s_i], ALU.add, r=[('acc4', a_i), ('tmp4', s_i)], w=[rk])
            for i in range(2):
                dc = 2 * dp + i
                m.dma(mergedT[dc * 128:(dc + 1) * 128, :], rowbs[i], r=[('rowb4', i)], slot=('rowb4', i))
        m.barrier()
        if stop_after == ('merge', l):
            break

        m.aoff = persist_end
        m.init_wbufs(2, 8 * 512)
        wo = [m.load_w(IN("w_out")[l][:, hh * 512:(hh + 1) * 512], 8, 512) for hh in range(2)]
        mts = [m.allocb(8 * 128) for _ in range(2)]
        xts = [m.alloc(D) for _ in range(3)]
        tmp5 = [m.alloc(D) for _ in range(2)]
        out5 = [m.alloc(D) for _ in range(2)]
        for tt_ in range(NT):
            i = tt_ % 2
            mt = _r3(mts[i], 8)
            m.dma(mt, mergedT[:, tt_ * 128:(tt_ + 1) * 128].rearrange("(c p) t -> p c t", p=128), w=[('mt', i)], slot=('mt', i))
            xs = tt_ % 3
            m.dma(xts[xs], x_src[tt_ * 128:(tt_ + 1) * 128, :], w=[('xt', xs)], slot=('xt', xs))
            b2 = m.bank2()
            for hh in range(2):
                for k in range(8):
                    m.mm(m.ps[:, b2 + hh, :], mt[:, k, :], wo[hh][0][:, k, :], k == 0, k == 7, r=[('mt', i), wo[hh][1]], w=[('ps', b2 + hh)])
            ps2 = (m.ps[:, b2:b2 + 2, :].rearrange("p a n -> p (a n)"), [('ps', b2), ('ps', b2 + 1)])
            ln_epilogue(ps2, xts[xs], ('xt', xs), gbc[:, 0:D], lnbc[:, 0:D], lnbc[:, D:2 * D], out5[i], ('out5', i), tmp5[i], ('tmp5', i), i)
            m.dma(x1_d[tt_ * 128:(tt_ + 1) * 128, :], out5[i], r=[('out5', i)], slot=('out5', i))
        m.barrier()
        if stop_after == ('ln1', l):
            break

        m.aoff = persist_end
        moe = (l % 2 == 1)
        TBK = 1024
        h2T = _r3(m.allocb(8 * TBK), 8)
        yacc = _r3(m.alloc(8 * TBK), 8)
        aTs = [m.allocb(4 * TBK).rearrange("p (c t) -> p c t", c=4) for _ in range(2)]
        wst6 = [m.alloc(8 * 512) for _ in range(2)]
        wbf6 = [m.allocb(8 * 512) for _ in range(4)]
        xts = [m.alloc(D) for _ in range(2)]
        tmp6 = [m.alloc(D) for _ in range(1)]
        out6 = [m.alloc(D) for _ in range(2)]
        sil = [m.alloc(512) for _ in range(2)]
        tmy = [m.alloc(512) for _ in range(2)]
        wsi = [0]
        wbi = [0]

        def load6(src_r3, kc, n):
            si = wsi[0] % 2
            wsi[0] += 1
            bi = wbi[0] % 4
            wbi[0] += 1
            st = _r3(wst6[si][:, 0:kc * n], kc)
            m.dma(st, src_r3, w=[('wst6', si)], slot=('wst6', si))
            bf = _r3(wbf6[bi][:, 0:kc * n], kc)
            m.cp('act', bf, st, r=[('wst6', si)], w=[('wbf6', bi)])
            return bf, ('wbf6', bi)
        if moe:
            rw = m.alloc(64)
            rbb = m.alloc(8)
            selE = m.alloc(8 * 128)
            combT = m.alloc(TBK)
            cbt_ = m.alloc(TBK)
            hf = [m.alloc(8 * 128)] * 2
            rs = m.alloc(64)
            cpad = m.alloc(128)
            m.memset('pool', cpad, 0.0, w=['cpad'])
            m.dma(_r3(rw, 8), IN("router_w")[0].rearrange("(c p) e -> p c e", p=128), w=['rw'], slot='misc0')
            m.dma(rbb, IN("router_b")[0].partition_broadcast(128), w=['rbb'], slot='misc1')
            m.memset('pool', selE[0:8, :], 1.0, w=['selE'])
            for e_ in range(8):
                m.ts('dve', selE[0:8, e_ * 128:(e_ + 1) * 128], selE[0:8, e_ * 128:(e_ + 1) * 128], ident[0:8, e_:e_ + 1], ALU.mult, r=['selE', 'ident'], w=['selE'])
            experts = [(IN("expert_w_gu")[0][e], IN("expert_w_down")[0][e], D_FFE, e) for e in range(8)]
        else:
            experts = [(IN("ffn_w_gu")[l // 2], IN("ffn_w_down")[l // 2], D_FF, None)]
        def stageA(tbk):
            tts = list(range(tbk * 8, tbk * 8 + 8))
            for tt_ in tts:
                s_ = tt_ % 2
                m.dma(xts[s_], x1_d[tt_ * 128:(tt_ + 1) * 128, :], w=[('xt', s_)], slot=('xt', s_))
                b2 = m.bank2()
                for c in range(8):
                    bb, cc = b2 + c // 4, (c % 4) * 128
                    m.tr(m.ps[:, bb, cc:cc + 128], xts[s_][:, c * 128:(c + 1) * 128], ident, r=[('xt', s_), 'ident'], w=[('ps', bb)])
                o = (tt_ - tbk * 8) * 128
                import os
                MA = int(os.environ.get('MOE_A', 9))
                if not moe:
                    for c in range(8):
                        bb, cc = b2 + c // 4, (c % 4) * 128
                        m.act(h2T[:, c, o:o + 128], m.ps[:, bb, cc:cc + 128], AF.Identity, r=[('ps', bb), 'scp', 'modT'],
                              w=[('h2T', tt_ % 8)], scale=scp[:, 8 + c:9 + c], bias=modT[:, 24 + c:25 + c])
                else:
                    hfi = hf[0]
                    hk = ('hf', 0)
                    for c in range(8):
                        bb, cc = b2 + c // 4, (c % 4) * 128
                        m.act(hfi[:, c * 128:(c + 1) * 128], m.ps[:, bb, cc:cc + 128], AF.Identity, r=[('ps', bb), 'scp', 'modT'],
                              w=[hk], scale=scp[:, 8 + c:9 + c], bias=modT[:, 24 + c:25 + c])
                    m.cp('pool', h2T[:, :, o:o + 128], _r3(hfi, 8), r=[hk], w=[('h2T', tt_ % 8)])
                if moe and MA >= 1:
                    bl = m.bank()
                    for c in range(8):
                        m.mm(m.ps[:, bl, 0:8], hfi[:, c * 128:(c + 1) * 128], rw[:, c * 8:(c + 1) * 8], c == 0, c == 7, r=[hk, 'rw'], w=[('ps', bl)])
                    m.tt('dve', rs[:, 0:8], m.ps[:, bl, 0:8], rbb, ALU.add, r=[('ps', bl), 'rbb'], w=['rs'])
                if moe and MA >= 2:
                    m.op('dve', lambda e: e.max(out=rs[:, 8:16], in_=rs[:, 0:8]), r=['rs'], w=['rs'])
                    m.ts('dve', rs[:, 16:24], rs[:, 0:8], rs[:, 9:10], ALU.is_ge, r=['rs'], w=['rs'])
                    m.ts('dve', rs[:, 24:25], rs[:, 8:9], -1.0, ALU.mult, r=['rs'], w=['rs'])
                    m.act(rs[:, 32:40], rs[:, 0:8], AF.Exp, r=['rs'], w=['rs'], bias=rs[:, 24:25])
                    m.act(rs[:, 25:26], rs[:, 9:10], AF.Exp, r=['rs'], w=['rs'], bias=rs[:, 24:25])
                    m.ts('dve', rs[:, 25:26], rs[:, 25:26], 1.0, ALU.add, r=['rs'], w=['rs'])
                    m.op('dve', lambda e: e.reciprocal(out=rs[:, 26:27], in_=rs[:, 25:26]), r=['rs'], w=['rs'])
                    m.tt('dve', rs[:, 40:48], rs[:, 32:40], rs[:, 16:24], ALU.mult, r=['rs'], w=['rs'])
                    m.ts('dve', rs[:, 40:48], rs[:, 40:48], rs[:, 26:27], ALU.mult, r=['rs'], w=['rs'])
                if moe and MA >= 3:
                    bt = m.bank()
                    m.cp('dve', cpad[:, 0:8], rs[:, 40:48], r=['rs'], w=['cpad'])
                    m.tr(m.ps[:, bt, 0:128], cpad, ident, r=['cpad', 'ident'], w=[('ps', bt)])
                    m.cp('dve', combT[0:8, o:o + 128], m.ps[0:8, bt, 0:128], r=[('ps', bt)], w=['combT'])

        def stageC(tbk):
            tts = list(range(tbk * 8, tbk * 8 + 8))
            import os
            MD = os.environ.get('MOE_DBG', 'ABC')
            YK = [('yacc', dc, half) for dc in range(8) for half in range(2)] + ['yacc']
            for tt_ in (tts if 'C' in MD else []):
                s_ = tt_ % 2
                o = (tt_ - tbk * 8) * 128
                m.dma(xts[s_], x1_d[tt_ * 128:(tt_ + 1) * 128, :], w=[('xt', s_)], slot=('xt', s_))
                b2 = m.bank2()
                for c in range(8):
                    bb, cc = b2 + c // 4, (c % 4) * 128
                    m.tr(m.ps[:, bb, cc:cc + 128], yacc[:, c, o:o + 128], ident, r=YK + ['ident'], w=[('ps', bb)])
                ps2 = (m.ps[:, b2:b2 + 2, :].rearrange("p a n -> p (a n)"), [('ps', b2), ('ps', b2 + 1)])
                ln_epilogue(ps2, xts[s_], ('xt', s_), gbc[:, D:2 * D], lnbc[:, 2 * D:3 * D], lnbc[:, 3 * D:4 * D], out6[s_], ('out6', s_), tmp6[0], ('tmp6', 0), s_)
                m.dma(x_dst[tt_ * 128:(tt_ + 1) * 128, :], out6[s_], r=[('out6', s_)], slot=('out6', s_))

        stageA(0)
        for tbk in range(S // TBK):
            tts = list(range(tbk * 8, tbk * 8 + 8))
            import os
            MD = os.environ.get('MOE_DBG', 'ABC')
            m.memset('pool', yacc, 0.0, w=['yacc'])
            H2K = [('h2T', i_) for i_ in range(8)]
            gi = 0
            for (wgu, wdn, F, e) in (experts if 'B' in MD else []):
                if e is not None:
                    for half in range(2):
                        bc_ = m.bank()
                        m.mm(m.ps[:, bc_, :], selE[0:8, e * 128:(e + 1) * 128], combT[0:8, half * 512:(half + 1) * 512], True, True, r=['selE', 'combT'], w=[('ps', bc_)])
                        m.cp('act', cbt_[:, half * 512:(half + 1) * 512], m.ps[:, bc_, :], r=[('ps', bc_)], w=['cbt_'])
                nfc = F // 128
                for f0 in range(0, nfc, 4):
                    ncq = min(4, nfc - f0)
                    ncol = ncq * 128
                    gw, gk = load6(wgu[:, f0 * 128:f0 * 128 + ncol].rearrange("(c p) n -> p c n", p=128), 8, ncol)
                    uw, uk = load6(wgu[:, F + f0 * 128:F + f0 * 128 + ncol].rearrange("(c p) n -> p c n", p=128), 8, ncol)
                    dw, dk = load6(wdn[f0 * 128:f0 * 128 + ncol, :].rearrange("(c p) n -> p c n", p=128), ncq, D)
                    aT = aTs[gi % 2]
                    ak = ('aT', gi % 2)
                    gi += 1
                    for fc in range(ncq):
                        for half in range(2):
                            pg = m.bank()
                            for k in range(8):
                                m.mm(m.ps[:, pg, :], gw[:, k, fc * 128:(fc + 1) * 128], h2T[:, k, half * 512:(half + 1) * 512], k == 0, k == 7,
                                     r=[gk] + H2K[half * 4:half * 4 + 4], w=[('ps', pg)])
                            pu = m.bank()
                            for k in range(8):
                                m.mm(m.ps[:, pu, :], uw[:, k, fc * 128:(fc + 1) * 128], h2T[:, k, half * 512:(half + 1) * 512], k == 0, k == 7,
                                     r=[uk] + H2K[half * 4:half * 4 + 4], w=[('ps', pu)])
                            s_i = (fc * 2 + half) % 2
                            m.act(sil[s_i], m.ps[:, pg, :], AF.Silu, r=[('ps', pg)], w=[('sil', s_i)])
                            m.tt('dve', aT[:, fc, half * 512:(half + 1) * 512], sil[s_i], m.ps[:, pu, :], ALU.mult, r=[('sil', s_i), ('ps', pu)], w=[ak])
                    for dc in range(8):
                        for half in range(2):
                            py = m.bank()
                            for fc in range(ncq):
                                m.mm(m.ps[:, py, :], dw[:, fc, dc * 128:(dc + 1) * 128], aT[:, fc, half * 512:(half + 1) * 512], fc == 0, fc == ncq - 1,
                                     r=[dk, ak], w=[('ps', py)])
                            ya = yacc[:, dc, half * 512:(half + 1) * 512]
                            yk = ('yacc', dc, half)
                            if e is not None:
                                t_i = (dc * 2 + half) % 2
                                m.tt('dve', tmy[t_i], m.ps[:, py, :], cbt_[:, half * 512:(half + 1) * 512], ALU.mult, r=[('ps', py), 'cbt_'], w=[('tmy', t_i)])
                                m.tt('dve', ya, ya, tmy[t_i], ALU.add, r=['yacc', yk, ('tmy', t_i)], w=[yk])
                            else:
                                m.tt('dve', ya, ya, m.ps[:, py, :], ALU.add, r=['yacc', yk, ('ps', py)], w=[yk])
            if tbk + 1 < S // TBK:
                stageA(tbk + 1)
            stageC(tbk)
        m.barrier()
        if stop_after == ('ffn', l):
            break
    m.barrier()
    nc_ = m.finish()
    nc_.used_inputs = list(_ins.keys())
    return nc_


_CACHE = {}


def _prep_inputs(inputs, b):
    f = lambda a: np.ascontiguousarray(a, dtype=np.float32)
    R = {
        "x": lambda a: a[b],
        "c": lambda a: np.asarray(a[b]).reshape(8, 128).T,
        "b_ada": lambda a: np.asarray(a).reshape(2, 1, 6 * D),
        "conv_w": lambda a: np.asarray(a).reshape(2, 4, 8, 128).transpose(0, 3, 2, 1),
        "conv_b": lambda a: np.asarray(a).reshape(2, 8, 128).transpose(0, 2, 1),
        "dt_bias": lambda a: np.asarray(a).reshape(2, 8, 1),
        "a_log": lambda a: np.asarray(a).reshape(2, 8, 1),
        "d_skip": lambda a: np.asarray(a).reshape(2, 1, 8),
        "ssm_norm_g": lambda a: np.asarray(a).reshape(2, 1, 512),
        "ret_gn_g": lambda a: np.asarray(a).reshape(2, 1, 512),
        "ret_gn_b": lambda a: np.asarray(a).reshape(2, 1, 512),
        "ln1_g": lambda a: np.asarray(a).reshape(2, 1, D),
        "ln1_b": lambda a: np.asarray(a).reshape(2, 1, D),
        "ln2_g": lambda a: np.asarray(a).reshape(2, 1, D),
        "ln2_b": lambda a: np.asarray(a).reshape(2, 1, D),
        "router_b": lambda a: np.asarray(a).reshape(1, 1, 8),
    }
    return {k: f(R[k](v) if k in R else v) for k, v in inputs.items()}


def kernel(**inputs):
    nc = build()
    shared = _prep_inputs(inputs, 0)
    in_maps = []
    for b in range(8):
        d = dict(shared)
        d["x"] = np.ascontiguousarray(inputs["x"][b], dtype=np.float32)
        d["c"] = np.ascontiguousarray(np.asarray(inputs["c"][b]).reshape(8, 128).T, dtype=np.float32)
        in_maps.append(d)
    res = run_bass_kernel_spmd(nc, in_maps, core_ids=list(range(8)))
    return np.stack([np.asarray(r["y"], dtype=np.float32) for r in res.results], axis=0)
```
